# Optimizing a Trainium2 kernel written in Bass

```python
import jax
import jax.numpy as jnp
from jax import lax
import numpy as np

D_MODEL = 2048
BATCH = 1
SEQ = 16384
DEPTH = 2

RW_HEADS = 16
RW_HEAD = 64
RW_WIDTH = RW_HEADS * RW_HEAD
RW_DECAY_LORA = 96
RW_A_LORA = 96
RW_V_LORA = 64
RW_G_LORA = 256
RW_GN_EPS = 64e-5
RW_BASE_SIZES = (RW_WIDTH, RW_WIDTH, RW_WIDTH, RW_DECAY_LORA, RW_A_LORA, RW_G_LORA)
RW_COLS_FIRST = sum(RW_BASE_SIZES)
RW_COLS_DEEP = RW_COLS_FIRST + RW_V_LORA

NSA_HEADS = 16
NSA_KV_GROUPS = 4
NSA_HPG = NSA_HEADS // NSA_KV_GROUPS
NSA_HEAD = 64
NSA_WIDTH = NSA_HEADS * NSA_HEAD
NSA_KV = NSA_KV_GROUPS * NSA_HEAD
CMP_LEN = 32
CMP_STRIDE = 16
CMP_HIDDEN = 128
SEL_BLOCK = 64
SEL_TOPN = 16
WINDOW = 512
Q_BLOCK = 128
FORCE_BONUS = 1e4
NEG_INF = -1e30
NSA_SIZES = (NSA_WIDTH,) + (NSA_KV,) * 6 + (3 * NSA_HEADS,)
NSA_COLS = sum(NSA_SIZES)

RET_HEADS = 8
RET_HEAD = 128
RET_WIDTH = RET_HEADS * RET_HEAD
RET_CHUNK = 128
RET_THETA = 10000.0
RET_GN_EPS = 1e-5
RET_SIZES = (RET_WIDTH,) * 4
RET_COLS = sum(RET_SIZES)

IN_COLS_FIRST = RW_COLS_FIRST + NSA_COLS + RET_COLS + 3 * D_MODEL
IN_COLS_DEEP = RW_COLS_DEEP + NSA_COLS + RET_COLS + 3 * D_MODEL

N_GROUPS = 4
EXPERTS_PER_GROUP = 8
N_EXPERTS = N_GROUPS * EXPERTS_PER_GROUP
EXPERT_FF = 512
EXPERT_TOPK = 2
MOE_BLOCK = 128

DN_ALPHA = (2 * DEPTH) ** 0.25
DN_BETA = (8 * DEPTH) ** -0.25
LN_EPS = 1e-5

kernel_name = 'hybrid_rwkv7_nsa_retention_hmoe'


def _split(p, sizes):
    idx = [int(i) for i in np.cumsum(sizes)[:-1]]
    return jnp.split(p, idx, axis=-1)


def _layer_norm(x, g, b):
    xf = x.astype(jnp.float32)
    mu = jnp.mean(xf, -1, keepdims=True)
    var = jnp.mean(jnp.square(xf - mu), -1, keepdims=True)
    return ((xf - mu) * lax.rsqrt(var + LN_EPS)).astype(x.dtype) * g + b


def _head_norm(x, eps):
    x = x.astype(jnp.float32)
    mu = jnp.mean(x, -1, keepdims=True)
    var = jnp.mean(jnp.square(x - mu), -1, keepdims=True)
    return (x - mu) * lax.rsqrt(var + eps)


def _token_shift(z, mu):
    prev = jnp.pad(z, ((0, 0), (1, 0), (0, 0)))[:, :-1]
    return z + (prev - z) * mu


def _masked_softmax(s, mask):
    s = jnp.where(mask, s, NEG_INF)
    p = jax.nn.softmax(s, axis=-1)
    return jnp.where(mask, p, 0.0)


def _rwkv7_time_mix(p, mu, w0, w_up, a0, a_up, g_up, k_k, k_a, r_k, lnx_g, lnx_b, v_first, v0, v_up):
    B, S, _ = p.shape
    H, N = RW_HEADS, RW_HEAD
    f32 = jnp.float32
    p = _token_shift(p, mu)
    if v_first is None:
        r, k, v, xw, xa, xg = _split(p, RW_BASE_SIZES)
        v_first = v
    else:
        r, k, v, xw, xa, xg, xv = _split(p, RW_BASE_SIZES + (RW_V_LORA,))
        v = v + (v_first - v) * jax.nn.sigmoid(v0 + xv @ v_up)
    w_log = -jax.nn.softplus(-(w0 + jnp.tanh(xw) @ w_up)) - 0.5
    decay = jnp.exp(-jnp.exp(w_log.astype(f32)))
    a = jax.nn.sigmoid(a0 + xa @ a_up)
    g = jax.nn.sigmoid(xg) @ g_up
    kk = (k * k_k).astype(f32).reshape(B, S, H, N)
    kk = kk / jnp.maximum(jnp.linalg.norm(kk, axis=-1, keepdims=True), 1e-12)
    k = k * (1.0 + (a - 1.0) * k_a)
    heads = lambda t: t.astype(f32).reshape(B, S, H, N)
    r_h, k_h, v_h, a_h, w_h = heads(r), heads(k), heads(v), heads(a), heads(decay)

    def step(state, inp):
        r_t, w_t, k_t, v_t, kk_t, a_t = inp
        s_kk = jnp.einsum('bhij,bhj->bhi', state, kk_t)
        state = (state * w_t[:, :, None, :] - s_kk[..., :, None] * (kk_t * a_t)[..., None, :]
                 + v_t[..., :, None] * k_t[..., None, :])
        return state, jnp.einsum('bhij,bhj->bhi', state, r_t)

    seq_first = lambda t: jnp.moveaxis(t, 1, 0)
    xs = (seq_first(r_h), seq_first(w_h), seq_first(k_h), seq_first(v_h), seq_first(kk), seq_first(a_h))
    _, y = lax.scan(step, jnp.zeros((B, H, N, N), f32), xs)
    y = jnp.moveaxis(y, 0, 1)
    y = _head_norm(y, RW_GN_EPS).reshape(B, S, RW_WIDTH) * lnx_g + lnx_b
    bonus = jnp.sum(r_h * k_h * r_k, -1, keepdims=True) * v_h
    out = (y + bonus.reshape(B, S, RW_WIDTH)) * g.astype(f32)
    return out.astype(p.dtype), v_first


def _nsa_compress(t, pe, w1, b1, w2):
    B, S, G, DH = t.shape
    n_cmp = (S - CMP_LEN) // CMP_STRIDE + 1
    idx = jnp.arange(n_cmp)[:, None] * CMP_STRIDE + jnp.arange(CMP_LEN)[None, :]
    blocks = t[:, idx] + pe[:, None, :]
    blocks = jnp.swapaxes(blocks, 2, 3).reshape(B, n_cmp, G, CMP_LEN * DH)
    return jax.nn.gelu(blocks @ w1 + b1) @ w2


def _nsa(p, cmp_pe, cmp_w1, cmp_b1, cmp_w2):
    B, S, _ = p.shape
    G, HPG, DH = NSA_KV_GROUPS, NSA_HPG, NSA_HEAD
    f32 = jnp.float32
    q, kc, vc, ks, vs, kw, vw, gate = _split(p, NSA_SIZES)
    kv = lambda t: t.reshape(B, S, G, DH)
    q = q.reshape(B, S, G, HPG, DH) * (DH ** -0.5)
    gate = jax.nn.sigmoid(gate).reshape(B, S, G, HPG, 3)
    k_cmp = _nsa_compress(kv(kc), cmp_pe[0], cmp_w1[0], cmp_b1[0], cmp_w2[0])
    v_cmp = _nsa_compress(kv(vc), cmp_pe[1], cmp_w1[1], cmp_b1[1], cmp_w2[1])
    n_cmp = k_cmp.shape[1]
    cmp_start = jnp.arange(n_cmp) * CMP_STRIDE
    cmp_end = cmp_start + CMP_LEN - 1
    n_sel = S // SEL_BLOCK
    top_n = min(SEL_TOPN, n_sel)
    sel_j = jnp.arange(n_sel)
    sel_start = sel_j * SEL_BLOCK
    overlap = ((cmp_start[:, None] < sel_start[None] + SEL_BLOCK)
               & (cmp_start[:, None] + CMP_LEN > sel_start[None])).astype(f32)
    to_blocks = lambda t: kv(t).reshape(B, n_sel, SEL_BLOCK, G, DH).transpose(0, 3, 1, 2, 4)
    ks_b, vs_b = to_blocks(ks), to_blocks(vs)
    pad = lambda t: jnp.pad(kv(t), ((0, 0), (WINDOW, 0), (0, 0), (0, 0)))
    kw_p, vw_p = pad(kw), pad(vw)
    gather_blocks = jax.vmap(jax.vmap(lambda blocks, ids: blocks[ids]))
    win_off = jnp.arange(WINDOW + Q_BLOCK) - WINDOW

    def query_block(qi):
        q0 = qi * Q_BLOCK
        t = q0 + jnp.arange(Q_BLOCK)
        qb = lax.dynamic_slice_in_dim(q, q0, Q_BLOCK, axis=1)
        gb = lax.dynamic_slice_in_dim(gate, q0, Q_BLOCK, axis=1)
        s = jnp.einsum('bqghd,bcgd->bghqc', qb, k_cmp).astype(f32)
        p_cmp = _masked_softmax(s, cmp_end[None, :] <= t[:, None])
        o_cmp = jnp.einsum('bghqc,bcgd->bqghd', p_cmp.astype(v_cmp.dtype), v_cmp)
        imp = jnp.einsum('bghqc,cn->bgqn', p_cmp, overlap)
        cur = (t // SEL_BLOCK)[:, None]
        causal = sel_j[None] <= cur
        forced = (sel_j[None] == 0) | (sel_j[None] == cur) | (sel_j[None] == cur - 1)
        score = jnp.where(causal, imp + jnp.where(forced, FORCE_BONUS, 0.0), NEG_INF)
        top_val, top_idx = lax.top_k(score, top_n)
        k_sel = gather_blocks(ks_b, top_idx).reshape(B, G, Q_BLOCK, top_n * SEL_BLOCK, DH)
        v_sel = gather_blocks(vs_b, top_idx).reshape(B, G, Q_BLOCK, top_n * SEL_BLOCK, DH)
        tok = top_idx[..., None] * SEL_BLOCK + jnp.arange(SEL_BLOCK)
        m_sel = (top_val[..., None] > 0.5 * NEG_INF) & (tok <= t[:, None, None])
        m_sel = m_sel.reshape(B, G, 1, Q_BLOCK, top_n * SEL_BLOCK)
        s = jnp.einsum('bqghd,bgqkd->bghqk', qb, k_sel).astype(f32)
        p_sel = _masked_softmax(s, m_sel)
        o_sel = jnp.einsum('bghqk,bgqkd->bqghd', p_sel.astype(v_sel.dtype), v_sel)
        kwb = lax.dynamic_slice_in_dim(kw_p, q0, WINDOW + Q_BLOCK, axis=1)
        vwb = lax.dynamic_slice_in_dim(vw_p, q0, WINDOW + Q_BLOCK, axis=1)
        s_pos = q0 + win_off
        dist = t[:, None] - s_pos[None]
        m_win = (dist >= 0) & (dist < WINDOW) & (s_pos[None] >= 0)
        s = jnp.einsum('bqghd,bkgd->bghqk', qb, kwb).astype(f32)
        p_win = _masked_softmax(s, m_win)
        o_win = jnp.einsum('bghqk,bkgd->bqghd', p_win.astype(vwb.dtype), vwb)
        return gb[..., 0:1] * o_cmp + gb[..., 1:2] * o_sel + gb[..., 2:3] * o_win

    out = lax.map(query_block, jnp.arange(S // Q_BLOCK))
    return jnp.moveaxis(out, 0, 1).reshape(B, S, NSA_WIDTH)


def _rotate_half(t, cos, sin):
    t1, t2 = jnp.split(t, 2, axis=-1)
    c, s = cos[:, None, :], sin[:, None, :]
    return jnp.concatenate([t1 * c - t2 * s, t1 * s + t2 * c], axis=-1)


def _retention(p):
    B, S, _ = p.shape
    H, DK = RET_HEADS, RET_HEAD
    f32 = jnp.float32
    q, k, v, g = _split(p, RET_SIZES)
    heads = lambda t: t.astype(f32).reshape(B, S, H, DK)
    inv_freq = 1.0 / (RET_THETA ** jnp.linspace(0.0, 1.0, DK // 2))
    ang = jnp.arange(S, dtype=f32)[:, None] * inv_freq[None, :]
    cos, sin = jnp.cos(ang), jnp.sin(ang)
    q = _rotate_half(heads(q), cos, sin)
    k = _rotate_half(heads(k), cos, sin) * (DK ** -0.5)
    v = heads(v)
    log_g = jnp.log1p(-jnp.exp2(-5.0 - jnp.arange(H, dtype=f32)))
    C = RET_CHUNK
    n_chunks = S // C
    i = jnp.arange(C, dtype=f32)
    diff = i[:, None] - i[None, :]
    inner_decay = jnp.where(diff >= 0, jnp.exp(log_g[:, None, None] * jnp.maximum(diff, 0.0)), 0.0)
    q_decay = jnp.exp(log_g[:, None] * (i + 1.0))[..., None]
    k_decay = jnp.exp(log_g[:, None] * (C - 1.0 - i))[..., None]
    c_decay = jnp.exp(log_g * C)[:, None, None]
    chunks = lambda t: t.reshape(B, n_chunks, C, H, DK).transpose(1, 0, 3, 2, 4)

    def step(R, inp):
        qc, kc, vc = inp
        att = jnp.einsum('bhqd,bhkd->bhqk', qc, kc) * inner_decay
        o = jnp.einsum('bhqk,bhkd->bhqd', att, vc) + jnp.einsum('bhqd,bhde->bhqe', qc, R) * q_decay
        R = R * c_decay + jnp.einsum('bhkd,bhke->bhde', kc * k_decay, vc)
        return R, o

    _, o = lax.scan(step, jnp.zeros((B, H, DK, DK), f32), (chunks(q), chunks(k), chunks(v)))
    o = o.transpose(1, 0, 3, 2, 4).reshape(B, S, H, DK)
    o = _head_norm(o, RET_GN_EPS).reshape(B, S, RET_WIDTH)
    return (jax.nn.silu(g.astype(f32)) * o).astype(p.dtype)


def _hier_moe(x, w_grp, b_grp, w_exp, b_exp, w_gate, w_up, w_down):
    B, S, D = x.shape
    T = B * S
    f32 = jnp.float32
    xt = x.reshape(T, D)
    grp_logits = (xt @ w_grp + b_grp).astype(f32)
    grp = jnp.argmax(grp_logits, axis=-1)
    grp_w = jnp.take_along_axis(jax.nn.softmax(grp_logits, -1), grp[:, None], axis=-1)
    exp_logits = (xt @ w_exp + b_exp).astype(f32).reshape(T, N_GROUPS, EXPERTS_PER_GROUP)
    in_grp = jnp.take_along_axis(exp_logits, grp[:, None, None], axis=1)[:, 0]
    top_val, top_idx = lax.top_k(in_grp, EXPERT_TOPK)
    gate_w = (jax.nn.softmax(top_val, -1) * grp_w).reshape(-1)
    expert = (grp[:, None] * EXPERTS_PER_GROUP + top_idx).reshape(-1)
    token = jnp.repeat(jnp.arange(T, dtype=jnp.int32), EXPERT_TOPK)
    n_assign = T * EXPERT_TOPK
    order = jnp.argsort(expert)
    e_s, t_s, w_s = expert[order], token[order], gate_w[order]
    counts = jnp.zeros((N_EXPERTS,), jnp.int32).at[expert].add(1)
    start = jnp.cumsum(counts) - counts
    padded = (counts + MOE_BLOCK - 1) // MOE_BLOCK * MOE_BLOCK
    pad_end = jnp.cumsum(padded)
    slot = (pad_end - padded)[e_s] + jnp.arange(n_assign, dtype=jnp.int32) - start[e_s]
    n_blocks = -(-n_assign // MOE_BLOCK) + N_EXPERTS
    n_slots = n_blocks * MOE_BLOCK
    slot_tok = jnp.full((n_slots,), T, jnp.int32).at[slot].set(t_s)
    slot_w = jnp.zeros((n_slots,), f32).at[slot].set(w_s)
    blk_expert = jnp.minimum(jnp.searchsorted(pad_end, jnp.arange(n_blocks, dtype=jnp.int32) * MOE_BLOCK, side='right'), N_EXPERTS - 1)
    x_slots = jnp.concatenate([xt, jnp.zeros((1, D), xt.dtype)])[slot_tok].reshape(n_blocks, MOE_BLOCK, D)

    def expert_block(args):
        xb, e = args
        h = jax.nn.silu(xb @ w_gate[e]) * (xb @ w_up[e])
        return h @ w_down[e]

    y = lax.map(expert_block, (x_slots, blk_expert)).reshape(n_slots, D)
    out = jax.ops.segment_sum(y * slot_w[:, None].astype(y.dtype), slot_tok, num_segments=T + 1)[:T]
    return out.reshape(B, S, D)


def setup_inputs(seed: int = 0) -> dict:
    key = jax.random.key(seed)
    keys = iter(jax.random.split(key, 48))
    f32 = jnp.float32
    L, D = DEPTH, D_MODEL
    nrm = lambda shape, scale: jax.random.normal(next(keys), shape, f32) * scale
    around = lambda shape, center, scale: center + nrm(shape, scale)
    ratio = (jnp.arange(RW_WIDTH, dtype=f32) / (RW_WIDTH - 1)) ** 0.85
    return {
        'x': nrm((BATCH, SEQ, D), 1.0),
        'w_in_first': nrm((D, IN_COLS_FIRST), D ** -0.5),
        'w_in_deep': nrm((L - 1, D, IN_COLS_DEEP), D ** -0.5),
        'rw_mu_first': jax.random.uniform(next(keys), (RW_COLS_FIRST,), f32),
        'rw_mu_deep': jax.random.uniform(next(keys), (L - 1, RW_COLS_DEEP), f32),
        'rw_w0': -6.0 + 5.0 * ratio + nrm((L, RW_WIDTH), 0.1),
        'rw_w_up': nrm((L, RW_DECAY_LORA, RW_WIDTH), 0.5 * RW_DECAY_LORA ** -0.5),
        'rw_a0': nrm((L, RW_WIDTH), 0.1),
        'rw_a_up': nrm((L, RW_A_LORA, RW_WIDTH), RW_A_LORA ** -0.5),
        'rw_v0': around((L - 1, RW_WIDTH), 1.0, 0.1),
        'rw_v_up': nrm((L - 1, RW_V_LORA, RW_WIDTH), RW_V_LORA ** -0.5),
        'rw_g_up': nrm((L, RW_G_LORA, RW_WIDTH), RW_G_LORA ** -0.5),
        'rw_k_k': around((L, RW_WIDTH), 0.85, 0.05),
        'rw_k_a': around((L, RW_WIDTH), 1.0, 0.05),
        'rw_r_k': nrm((L, RW_HEADS, RW_HEAD), 0.1),
        'rw_lnx_g': around((L, RW_WIDTH), 1.0, 0.05),
        'rw_lnx_b': nrm((L, RW_WIDTH), 0.02),
        'nsa_cmp_pe': nrm((L, 2, CMP_LEN, NSA_HEAD), 0.2),
        'nsa_cmp_w1': nrm((L, 2, CMP_LEN * NSA_HEAD, CMP_HIDDEN), (CMP_LEN * NSA_HEAD) ** -0.5),
        'nsa_cmp_b1': nrm((L, 2, CMP_HIDDEN), 0.02),
        'nsa_cmp_w2': nrm((L, 2, CMP_HIDDEN, NSA_HEAD), CMP_HIDDEN ** -0.5),
        'w_br_rw': nrm((L, RW_WIDTH, D), RW_WIDTH ** -0.5),
        'w_br_nsa': nrm((L, NSA_WIDTH, D), NSA_WIDTH ** -0.5),
        'w_br_ret': nrm((L, RET_WIDTH, D), RET_WIDTH ** -0.5),
        'w_out': nrm((L, D, D), D ** -0.5 * DN_BETA),
        'ln1_g': around((L, D), 1.0, 0.05),
        'ln1_b': nrm((L, D), 0.02),
        'moe_w_grp': nrm((L, D, N_GROUPS), D ** -0.5),
        'moe_b_grp': nrm((L, N_GROUPS), 0.01),
        'moe_w_exp': nrm((L, D, N_EXPERTS), D ** -0.5),
        'moe_b_exp': nrm((L, N_EXPERTS), 0.01),
        'moe_w_gate': nrm((L, N_EXPERTS, D, EXPERT_FF), D ** -0.5),
        'moe_w_up': nrm((L, N_EXPERTS, D, EXPERT_FF), D ** -0.5),
        'moe_w_down': nrm((L, N_EXPERTS, EXPERT_FF, D), EXPERT_FF ** -0.5 * DN_BETA),
        'ln2_g': around((L, D), 1.0, 0.05),
        'ln2_b': nrm((L, D), 0.02),
    }


def reference(x, w_in_first, w_in_deep, rw_mu_first, rw_mu_deep, rw_w0, rw_w_up, rw_a0, rw_a_up, rw_v0, rw_v_up, rw_g_up, rw_k_k, rw_k_a, rw_r_k, rw_lnx_g, rw_lnx_b, nsa_cmp_pe, nsa_cmp_w1, nsa_cmp_b1, nsa_cmp_w2, w_br_rw, w_br_nsa, w_br_ret, w_out, ln1_g, ln1_b, moe_w_grp, moe_b_grp, moe_w_exp, moe_b_exp, moe_w_gate, moe_w_up, moe_w_down, ln2_g, ln2_b):
    v_first = None
    for l in range(DEPTH):
        first = l == 0
        w_in = w_in_first if first else w_in_deep[l - 1]
        mu = rw_mu_first if first else rw_mu_deep[l - 1]
        rw_cols = RW_COLS_FIRST if first else RW_COLS_DEEP
        proj = x @ w_in
        p_rw, p_nsa, p_ret, g_rw, g_nsa, g_ret = _split(proj, (rw_cols, NSA_COLS, RET_COLS, D_MODEL, D_MODEL, D_MODEL))
        y_rw, v_first = _rwkv7_time_mix(p_rw, mu, rw_w0[l], rw_w_up[l], rw_a0[l], rw_a_up[l], rw_g_up[l],
                                        rw_k_k[l], rw_k_a[l], rw_r_k[l], rw_lnx_g[l], rw_lnx_b[l], v_first,
                                        None if first else rw_v0[l - 1], None if first else rw_v_up[l - 1])
        y_nsa = _nsa(p_nsa, nsa_cmp_pe[l], nsa_cmp_w1[l], nsa_cmp_b1[l], nsa_cmp_w2[l])
        y_ret = _retention(p_ret)
        merged = (jax.nn.sigmoid(g_rw) * (y_rw @ w_br_rw[l])
                  + jax.nn.sigmoid(g_nsa) * (y_nsa @ w_br_nsa[l])
                  + jax.nn.sigmoid(g_ret) * (y_ret @ w_br_ret[l]))
        x = _layer_norm(DN_ALPHA * x + merged @ w_out[l], ln1_g[l], ln1_b[l])
        moe = _hier_moe(x, moe_w_grp[l], moe_b_grp[l], moe_w_exp[l], moe_b_exp[l], moe_w_gate[l], moe_w_up[l], moe_w_down[l])
        x = _layer_norm(DN_ALPHA * x + moe, ln2_g[l], ln2_b[l])
    return x
```

```python
import contextlib
import numpy as np
import concourse.bass as bass
import concourse.mybir as mybir
from concourse.bass_utils import run_bass_kernel_spmd

F32 = mybir.dt.float32
BF16 = mybir.dt.bfloat16
ALU = mybir.AluOpType
AF = mybir.ActivationFunctionType
AX = mybir.AxisListType

NDMA_SEM = 8
ARENA_BYTES = 206 * 1024


class Prog:
    def __init__(self):
        self.nc = bass.Bass("TRN2", target_bir_lowering=False)
        self.ops = []
        self.last_w = {}
        self.readers = {}
        self.stack = contextlib.ExitStack()
        self._n = 0
        self.arena = self.stack.enter_context(self.nc.sbuf_tensor("arena", [128, ARENA_BYTES // 2], BF16))
        self.bump = 0
        self.banks = [self.stack.enter_context(self.nc.psum_tensor(f"bank{i}", [128, 512], F32)) for i in range(8)]
        self.nbank = 0

    def dram(self, name, shape, dtype=F32, kind="Internal"):
        return self.nc.dram_tensor(name, list(shape), dtype, kind=kind).ap()

    def sb(self, shape, dtype=F32, name=None):
        esz = 4 if dtype == F32 else 2
        n = int(np.prod(shape[1:]))
        nbytes = (n * esz + 63) // 64 * 64
        off = self.bump
        self.bump += nbytes
        assert self.bump <= ARENA_BYTES, f"SBUF arena overflow: {self.bump}"
        v = self.arena[0:shape[0], off // 2: off // 2 + n * esz // 2]
        if dtype == F32:
            v = v.bitcast(F32)
        if len(shape) == 3:
            v = v.rearrange("p (a b) -> p a b", a=shape[1])
        elif len(shape) == 4:
            v = v.rearrange("p (a b c) -> p a b c", a=shape[1], b=shape[2])
        elif len(shape) == 5:
            v = v.rearrange("p (a b c d) -> p a b c d", a=shape[1], b=shape[2], c=shape[3])
        return v

    def ps(self, shape=None, dtype=F32, name=None):
        b = self.banks[self.nbank % 8]
        self.nbank += 1
        return b

    def phase(self):
        self.barrier()
        self.bump = 0
        self.nbank = 0
        self.last_w = {}
        self.readers = {}

    def barrier(self):
        engs = ["pe", "dve", "act", "pool", "sp"]
        idxs = set()
        for e in engs:
            last = [i for i, o in enumerate(self.ops) if o["eng"] == e]
            if last:
                idxs.add(last[-1])
            dm = [i for i, o in enumerate(self.ops) if o["eng"] == e and o["dma"]]
            idxs.update(dm[-NDMA_SEM:])
        for e in engs:
            self.ops.append(dict(eng=e, fn=lambda en: en.nop(), deps=set(idxs), dma=False, r=(), w=(), barrier=True))

    def op(self, eng, fn, r=(), w=(), dma=False):
        idx = len(self.ops)
        deps = set()
        for k in r:
            if k in self.last_w:
                deps.add(self.last_w[k])
        for k in w:
            if k in self.last_w:
                deps.add(self.last_w[k])
            for rd in self.readers.get(k, ()):
                deps.add(rd)
        for k in r:
            self.readers.setdefault(k, []).append(idx)
        for k in w:
            self.last_w[k] = idx
            self.readers[k] = []
        deps.discard(idx)
        self.ops.append(dict(eng=eng, fn=fn, deps=deps, dma=dma, r=tuple(r), w=tuple(w)))
        return idx

    def pe(self, fn, r=(), w=()):
        return self.op("pe", fn, r, w)

    def dve(self, fn, r=(), w=()):
        return self.op("dve", fn, r, w)

    def act(self, fn, r=(), w=()):
        return self.op("act", fn, r, w)

    def pool(self, fn, r=(), w=()):
        return self.op("pool", fn, r, w)

    def dma(self, eng, out, in_, r=(), w=(), **kw):
        return self.op(eng, lambda e: e.dma_start(out=out, in_=in_, **kw), r, w, dma=True)

    def finalize(self):
        nc = self.nc
        ops = self.ops
        engs = ["pe", "dve", "act", "pool", "sp"]
        needed = [False] * len(ops)
        for i, o in enumerate(ops):
            keep = set()
            for d in o["deps"]:
                od = ops[d]
                same = od["eng"] == o["eng"]
                if same and not od["dma"]:
                    if o["eng"] == "pe" or o.get("barrier"):
                        continue
                    if not (set(od["w"]) & set(o["r"])):
                        continue
                keep.add(d)
                needed[d] = True
            o["deps"] = keep
        sem_c = {e: self.stack.enter_context(nc.semaphore(f"c_{e}")) for e in engs}
        sem_d = {e: [self.stack.enter_context(nc.semaphore(f"d_{e}{j}")) for j in range(NDMA_SEM)]
                 for e in ("sp", "pool", "act")}
        cnt_c = {e: 0 for e in engs}
        cnt_d = {e: 0 for e in engs}
        for i, o in enumerate(ops):
            e = o["eng"]
            if o["dma"]:
                j = cnt_d[e]
                cnt_d[e] += 1
                o["sig"] = (sem_d[e][j % NDMA_SEM], 16 * (j // NDMA_SEM + 1), 16)
                o["dma_idx"] = j
            else:
                if needed[i]:
                    cnt_c[e] += 1
                    o["sig"] = (sem_c[e], cnt_c[e], 1)
                else:
                    o["sig"] = None
        dmas = {e: [o for o in ops if o["dma"] and o["eng"] == e] for e in ("sp", "pool", "act")}
        per_eng = {e: [o for o in ops if o["eng"] == e] for e in engs}

        def emit(e, eng):
            waited = {}

            def wait(sig):
                sem, val, _ = sig
                key = id(sem)
                if waited.get(key, 0) >= val:
                    return
                eng.wait_ge(sem, val)
                waited[key] = val

            dlist = dmas.get(e, [])
            for o in per_eng[e]:
                for d in sorted(o["deps"]):
                    wait(ops[d]["sig"])
                if o["dma"]:
                    j = o["dma_idx"]
                    if j >= NDMA_SEM:
                        wait(dlist[j - NDMA_SEM]["sig"])
                ins = o["fn"](eng)
                if o["sig"] is not None:
                    ins.then_inc(o["sig"][0], o["sig"][2])
            for o in dlist[-NDMA_SEM:]:
                wait(o["sig"])

        with nc.Block() as block:
            @block.tensor
            def _(eng):
                emit("pe", eng)

            @block.vector
            def _(eng):
                emit("dve", eng)

            @block.scalar
            def _(eng):
                emit("act", eng)

            @block.gpsimd
            def _(eng):
                emit("pool", eng)

            @block.sync
            def _(eng):
                emit("sp", eng)
        self.stack.close()
        return nc


def emit_proj(p, S, NF, NT, d, pfx="pj", fm_key=None, tm_key=None):
    KC = 16
    TB = 512
    NB = S // TB
    K = lambda *a: (pfx,) + a
    fm_key = fm_key or (lambda rt, tb: K("ofm", rt, tb))
    tm_key = tm_key or (lambda tb, j: K("otm", tb, j))
    wf = p.sb([128, KC, NF], BF16)
    wt = p.sb([128, KC, NT], BF16)
    for kc in range(KC):
        p.dma("pool", wf[:, kc, :], d["wfm"][kc * 128:(kc + 1) * 128, :], w=[K("wf")])
        p.dma("pool", wt[:, kc, :], d["wtm"][kc * 128:(kc + 1) * 128, :], w=[K("wt")])
    xb = [p.sb([128, KC, TB], BF16) for _ in range(2)]
    of = [p.sb([128, TB]) for _ in range(3)]
    ot = [p.sb([128, NT]) for _ in range(2)]
    pss = [p.ps([128, 512]) for _ in range(4)]
    cnt = {"ps": 0, "of": 0, "ot": 0, "ev": 0}
    rts = [(r0, min(128, NF - r0)) for r0 in range(0, NF, 128)]
    cgs = [(c0, min(512, NT - c0)) for c0 in range(0, NT, 512)]

    def evac(out, in_, r, w):
        if cnt["ev"] % 2 == 0:
            p.act(lambda e: e.copy(out=out, in_=in_), r=r, w=w)
        else:
            p.dve(lambda e: e.tensor_copy(out=out, in_=in_), r=r, w=w)
        cnt["ev"] += 1

    def blk(b):
        q = b % 2
        t0 = b * TB
        X = xb[q]
        p.dma("pool", X[:, :, :], d["xT"][:, t0:t0 + TB].rearrange("(kc p) t -> p kc t", p=128), r=[("xTdram", b)], w=[K("xb", q)])
        for ri, (r0, rn) in enumerate(rts):
            pi = cnt["ps"] % 4; cnt["ps"] += 1
            ps = pss[pi]
            for kc in range(KC):
                p.pe(lambda e, ps=ps, kc=kc, r0=r0, rn=rn: e.matmul(ps[:rn, :], lhsT=wf[:, kc, r0:r0 + rn], rhs=X[:, kc, :], start=(kc == 0), stop=(kc == KC - 1)),
                     r=[K("wf"), K("xb", q)], w=[K("ps", pi)])
            oi = cnt["of"] % 3; cnt["of"] += 1
            evac(of[oi][:rn, :], ps[:rn, :], [K("ps", pi)], [K("of", oi)])
            p.dma("sp", d["ofm"][r0:r0 + rn, t0:t0 + TB], of[oi][:rn, :], r=[K("of", oi)], w=[fm_key(ri, b)])
        for j in range(TB // 128):
            oi = cnt["ot"] % 2; cnt["ot"] += 1
            for (c0, cn) in cgs:
                pi = cnt["ps"] % 4; cnt["ps"] += 1
                ps = pss[pi]
                for kc in range(KC):
                    p.pe(lambda e, ps=ps, kc=kc, c0=c0, cn=cn, j=j: e.matmul(ps[:, :cn], lhsT=X[:, kc, j * 128:(j + 1) * 128], rhs=wt[:, kc, c0:c0 + cn],
                                                                         start=(kc == 0), stop=(kc == KC - 1)), r=[K("wt"), K("xb", q)], w=[K("ps", pi)])
                evac(ot[oi][:, c0:c0 + cn], ps[:, :cn], [K("ps", pi)], [K("ot", oi, c0)])
            p.dma("sp", d["otm"][t0 + j * 128:t0 + (j + 1) * 128, :], ot[oi][:, :], r=[K("ot", oi, c0) for (c0, cn) in cgs], w=[tm_key(b, j)])

    for b in range(NB):
        blk(b)

RW_GN_EPS = 64e-5
CV_NAMES = ["mu_r", "mu_k", "mu_v", "mu_xw", "mu_xa", "mu_xg0", "mu_xg1", "mu_xv",
            "w0", "a0", "v0", "k_k", "k_a", "r_k", "lnx_g", "lnx_b", "eps", "tiny", "fdeep"]
CVI = {n: i for i, n in enumerate(CV_NAMES)}


def emit_rwkv(p, S, d, TC=16, pfx="rw"):
    deep = True
    TB = 512
    NB = S // TB
    K = lambda *a: (pfx,) + a
    cv = p.sb([128, len(CV_NAMES)])
    p.dma("sp", cv[:, :], d["cv"], w=[K("cv")])
    ident = p.sb([128, 128])
    p.dma("sp", ident[:, :], d["ident"], w=[K("ident")])
    bones = p.sb([128, 128])
    p.dma("sp", bones[:, :], d["bones"], w=[K("bones")])
    w_up = p.sb([96, 128]); p.dma("sp", w_up[:, :], d["w_up"], w=[K("w_up")])
    a_up = p.sb([96, 128]); p.dma("sp", a_up[:, :], d["a_up"], w=[K("a_up")])
    g_up = p.sb([128, 2, 128]); p.dma("sp", g_up[:, :, :], d["g_up"].rearrange("(c p) n -> p c n", p=128), w=[K("g_up")])
    if deep:
        v_up = p.sb([64, 128]); p.dma("sp", v_up[:, :], d["v_up"], w=[K("v_up")])
    CONST = [K("cv"), K("ident"), K("bones"), K("w_up"), K("a_up"), K("g_up"), K("v_up")]

    def c(name, rows=128):
        i = CVI[name]
        return cv[:rows, i:i + 1]

    groups = [("r", 128, "pr", "mu_r"), ("k", 128, "pk", "mu_k"), ("v", 128, "pv", "mu_v"),
              ("xw", 96, "pxw", "mu_xw"), ("xa", 96, "pxa", "mu_xa"),
              ("xg0", 128, "pxg", "mu_xg0"), ("xg1", 128, "pxg", "mu_xg1")]
    if deep:
        groups.append(("xv", 64, "pxv", "mu_xv"))
    def mk(n, shape, dt=F32):
        return [p.sb(shape, dt) for _ in range(n)]
    pin = {g[0]: mk(2, [128, TB + 1]) for g in groups}
    sh = {g[0]: mk(1, [128, TB]) * 2 for g in groups}
    names2 = ["tmp", "tmp2", "txw", "dec", "a", "sxg0", "sxg1", "g", "v", "kk", "rn", "nk", "kp", "ka", "bv",
              "yT", "cen", "sq", "o"]
    if deep:
        names2 += ["vf", "vg"]
    DBL = ("v", "yT", "bv", "g")
    T = {n: (mk(2, [128, TB]) if n in DBL else mk(1, [128, TB]) * 2) for n in names2}
    stg = mk(2, [128, 4, 2, 5, 64])
    bc = mk(2, [128, TC, 5, 64])
    St = p.sb([128, 64])
    junk = p.sb([128, 64])
    nskk = p.sb([128, 1])
    p.dve(lambda e: e.memset(St[:, :], 0.0), w=[K("S")])
    NPS = 7
    pss = [p.ps([128, 512]) for _ in range(NPS)]
    psn = [0]

    def nps():
        i = psn[0] % NPS
        psn[0] += 1
        return pss[i], K("ps", i)

    def prep(b):
        q = b % 2
        t0 = b * TB
        kq = lambda *n: K(*n, q if (n[0] in ("v", "yT", "bv", "g", "stg") or n[0].startswith("pin")) else 0)
        for (gn, rows, src, mu) in groups:
            off = 128 if gn == "xg1" else 0
            sap = d[src]
            tile = pin[gn][q]
            if b == 0:
                p.dma("sp", tile[:rows, 1:TB + 1], sap[off:off + rows, t0:t0 + TB], w=[kq("pin_" + gn)])
                p.pool(lambda e, tile=tile, rows=rows: e.memset(tile[:rows, 0:1], 0.0), w=[kq("pin0_" + gn)])
            else:
                p.dma("sp", tile[:rows, 0:TB + 1], sap[off:off + rows, t0 - 1:t0 + TB], w=[kq("pin_" + gn), kq("pin0_" + gn)])
            tmp = T["tmp"][q]
            p.dve(lambda e, tile=tile, rows=rows, tmp=tmp: e.tensor_sub(out=tmp[:rows, :], in0=tile[:rows, 0:TB], in1=tile[:rows, 1:TB + 1]),
                  r=[kq("pin_" + gn), kq("pin0_" + gn)], w=[kq("tmp")])
            o = sh[gn][q]
            p.dve(lambda e, tile=tile, rows=rows, tmp=tmp, o=o, mu=mu: e.scalar_tensor_tensor(
                out=o[:rows, :], in0=tmp[:rows, :], scalar=c(mu, rows), in1=tile[:rows, 1:TB + 1], op0=ALU.mult, op1=ALU.add),
                r=[kq("tmp"), kq("pin_" + gn), K("cv")], w=[kq("sh_" + gn)])
        txw = T["txw"][q]
        p.act(lambda e: e.activation(out=txw[:96, :], in_=sh["xw"][q][:96, :], func=AF.Tanh), r=[kq("sh_xw")], w=[kq("txw")])
        ps, pk_ = nps()
        p.pe(lambda e, ps=ps: e.matmul(ps[:, :], lhsT=w_up[:, :], rhs=txw[:96, :], start=True, stop=True), r=[kq("txw"), K("w_up")], w=[pk_])
        dec = T["dec"][q]
        p.act(lambda e, ps=ps: e.activation(out=dec[:, :], in_=ps[:, :], func=AF.Sigmoid, bias=c("w0"), scale=1.0), r=[pk_, K("cv")], w=[kq("dec")])
        p.act(lambda e: e.activation(out=dec[:, :], in_=dec[:, :], func=AF.Exp, scale=-float(np.exp(-0.5))), r=[kq("dec")], w=[kq("dec")])
        ps, pk_ = nps()
        p.pe(lambda e, ps=ps: e.matmul(ps[:, :], lhsT=a_up[:, :], rhs=sh["xa"][q][:96, :], start=True, stop=True), r=[kq("sh_xa"), K("a_up")], w=[pk_])
        a = T["a"][q]
        p.act(lambda e, ps=ps: e.activation(out=a[:, :], in_=ps[:, :], func=AF.Sigmoid, bias=c("a0"), scale=1.0), r=[pk_, K("cv")], w=[kq("a")])
        for j, gn in enumerate(("xg0", "xg1")):
            sx = T["s" + gn][q]
            p.act(lambda e, sx=sx, gn=gn: e.activation(out=sx[:, :], in_=sh[gn][q][:, :], func=AF.Sigmoid), r=[kq("sh_" + gn)], w=[kq("s" + gn)])
        ps, pk_ = nps()
        for j, gn in enumerate(("xg0", "xg1")):
            p.pe(lambda e, ps=ps, j=j, gn=gn: e.matmul(ps[:, :], lhsT=g_up[:, j, :], rhs=T["s" + gn][q][:, :], start=(j == 0), stop=(j == 1)),
                 r=[kq("s" + gn), K("g_up")], w=[pk_])
        g = T["g"][q]
        p.act(lambda e, ps=ps: e.copy(out=g[:, :], in_=ps[:, :]), r=[pk_], w=[kq("g")])
        v = T["v"][q]
        vf = T["vf"][q]
        p.dma("sp", vf[:, :], d["vf_in"][:, t0:t0 + TB], w=[kq("vf")])
        ps, pk_ = nps()
        p.pe(lambda e, ps=ps: e.matmul(ps[:, :], lhsT=v_up[:, :], rhs=sh["xv"][q][:64, :], start=True, stop=True), r=[kq("sh_xv"), K("v_up")], w=[pk_])
        vg = T["vg"][q]
        p.act(lambda e, ps=ps: e.activation(out=vg[:, :], in_=ps[:, :], func=AF.Sigmoid, bias=c("v0"), scale=1.0), r=[pk_, K("cv")], w=[kq("vg")])
        tmp = T["tmp"][q]
        p.dve(lambda e: e.tensor_sub(out=tmp[:, :], in0=vf[:, :], in1=sh["v"][q][:, :]), r=[kq("vf"), kq("sh_v")], w=[kq("tmp")])
        p.dve(lambda e: e.tensor_mul(out=tmp[:, :], in0=tmp[:, :], in1=vg[:, :]), r=[kq("tmp"), kq("vg")], w=[kq("tmp")])
        p.dve(lambda e: e.scalar_tensor_tensor(out=v[:, :], in0=tmp[:, :], scalar=c("fdeep"), in1=sh["v"][q][:, :], op0=ALU.mult, op1=ALU.add),
              r=[kq("tmp"), kq("sh_v"), K("cv")], w=[kq("v")])
        p.dma("sp", d["vf_out"][:, t0:t0 + TB], v[:, :], r=[kq("v")], w=[K("vfdram", b)])
        kk = T["kk"][q]
        p.dve(lambda e: e.tensor_scalar(out=kk[:, :], in0=sh["k"][q][:, :], scalar1=c("k_k"), scalar2=None, op0=ALU.mult), r=[kq("sh_k"), K("cv")], w=[kq("kk")])
        tmp2 = T["tmp2"][q]
        p.dve(lambda e: e.tensor_mul(out=tmp2[:, :], in0=kk[:, :], in1=kk[:, :]), r=[kq("kk")], w=[kq("tmp2")])
        ps, pk_ = nps()
        p.pe(lambda e, ps=ps: e.matmul(ps[:, :], lhsT=bones[:, :], rhs=tmp2[:, :], start=True, stop=True), r=[kq("tmp2"), K("bones")], w=[pk_])
        rn = T["rn"][q]
        p.act(lambda e, ps=ps: e.activation(out=rn[:, :], in_=ps[:, :], func=AF.Sqrt), r=[pk_], w=[kq("rn")])
        p.dve(lambda e: e.tensor_scalar(out=rn[:, :], in0=rn[:, :], scalar1=1e-12, scalar2=None, op0=ALU.max), r=[kq("rn")], w=[kq("rn")])
        p.dve(lambda e: e.reciprocal(out=rn[:, :], in_=rn[:, :]), r=[kq("rn")], w=[kq("rn")])
        nk = T["nk"][q]
        p.dve(lambda e: e.scalar_tensor_tensor(out=nk[:, :], in0=kk[:, :], scalar=-1.0, in1=rn[:, :], op0=ALU.mult, op1=ALU.mult), r=[kq("kk"), kq("rn")], w=[kq("nk")])
        kp = T["kp"][q]
        p.dve(lambda e: e.tensor_scalar(out=tmp2[:, :], in0=a[:, :], scalar1=-1.0, scalar2=None, op0=ALU.add), r=[kq("a")], w=[kq("tmp2")])
        p.dve(lambda e: e.scalar_tensor_tensor(out=tmp2[:, :], in0=tmp2[:, :], scalar=c("k_a"), in1=sh["k"][q][:, :], op0=ALU.mult, op1=ALU.mult),
              r=[kq("tmp2"), kq("sh_k"), K("cv")], w=[kq("tmp2")])
        p.dve(lambda e: e.tensor_add(out=kp[:, :], in0=tmp2[:, :], in1=sh["k"][q][:, :]), r=[kq("tmp2"), kq("sh_k")], w=[kq("kp")])
        ka = T["ka"][q]
        p.dve(lambda e: e.scalar_tensor_tensor(out=ka[:, :], in0=nk[:, :], scalar=-1.0, in1=a[:, :], op0=ALU.mult, op1=ALU.mult), r=[kq("nk"), kq("a")], w=[kq("ka")])
        p.dve(lambda e: e.scalar_tensor_tensor(out=tmp2[:, :], in0=sh["r"][q][:, :], scalar=c("r_k"), in1=kp[:, :], op0=ALU.mult, op1=ALU.mult),
              r=[kq("sh_r"), kq("kp"), K("cv")], w=[kq("tmp2")])
        ps, pk_ = nps()
        p.pe(lambda e, ps=ps: e.matmul(ps[:, :], lhsT=bones[:, :], rhs=tmp2[:, :], start=True, stop=True), r=[kq("tmp2"), K("bones")], w=[pk_])
        bv = T["bv"][q]
        p.dve(lambda e, ps=ps: e.tensor_mul(out=bv[:, :], in0=ps[:, :], in1=v[:, :]), r=[pk_, kq("v")], w=[kq("bv")])
        ops5 = [(nk, "nk"), (dec, "dec"), (ka, "ka"), (kp, "kp"), (sh["r"][q], "sh_r")]
        sg = stg[q]
        for j in range(4):
            for qi, (X, xn) in enumerate(ops5):
                ps, pk_ = nps()
                p.pe(lambda e, ps=ps, X=X, j=j: e.transpose(out=ps[:, 0:128], in_=X[:, j * 128:(j + 1) * 128], identity=ident[:, :]),
                     r=[kq(xn), K("ident")], w=[pk_])
                p.act(lambda e, ps=ps, j=j, qi=qi: e.copy(out=sg[:, j, :, qi, :], in_=ps[:, 0:128].rearrange("p (h x) -> p h x", h=2)),
                      r=[pk_], w=[kq("stg", j, qi)])
            for h in range(2):
                p.dma("sp", d["scr"][h, t0 + j * 128:t0 + (j + 1) * 128, :, :], sg[:, j, h, :, :],
                      r=[kq("stg", j, qi) for qi in range(5)], w=[K("scr", b, j, h)])

    def scan(b):
        q = b % 2
        t0 = b * TB
        kq = lambda *n: K(*n, q if (n[0] in ("v", "yT", "bv", "g", "stg") or n[0].startswith("pin")) else 0)
        v = T["v"][q]
        yT = T["yT"][q]
        for ci in range(TB // TC):
            tc0 = t0 + ci * TC
            bq = (b * (TB // TC) + ci) % 2
            B = bc[bq]
            j = (ci * TC) // 128
            for h in range(2):
                src = d["scr"][h, tc0:tc0 + TC, :, :].rearrange("t q x -> (t q x)").partition_broadcast(64)
                p.dma("sp", B[h * 64:(h + 1) * 64, :, :, :].rearrange("p t q x -> p (t q x)"), src,
                      r=[K("scr", b, j, h)], w=[K("bc", bq, h)])
            rk = [K("bc", bq, 0), K("bc", bq, 1)]
            for tt in range(TC):
                t = ci * TC + tt
                p.dve(lambda e, B=B, tt=tt: e.scalar_tensor_tensor(out=junk[:, :], in0=St[:, :], scalar=1.0, in1=B[:, tt, 0, :], op0=ALU.mult, op1=ALU.mult,
                                                                   accum_out=nskk[:, 0:1]), r=rk + [K("S")], w=[K("junk"), K("nskk")])
                p.dve(lambda e, B=B, tt=tt: e.tensor_mul(out=St[:, :], in0=St[:, :], in1=B[:, tt, 1, :]), r=rk + [K("S")], w=[K("S")])
                p.dve(lambda e, B=B, tt=tt: e.scalar_tensor_tensor(out=St[:, :], in0=B[:, tt, 2, :], scalar=nskk[:, 0:1], in1=St[:, :], op0=ALU.mult, op1=ALU.add),
                      r=rk + [K("S"), K("nskk")], w=[K("S")])
                p.dve(lambda e, B=B, tt=tt, t=t: e.scalar_tensor_tensor(out=St[:, :], in0=B[:, tt, 3, :], scalar=v[:, t:t + 1], in1=St[:, :], op0=ALU.mult, op1=ALU.add),
                      r=rk + [K("S"), kq("v")], w=[K("S")])
                p.dve(lambda e, B=B, tt=tt, t=t: e.scalar_tensor_tensor(out=junk[:, :], in0=St[:, :], scalar=1.0, in1=B[:, tt, 4, :], op0=ALU.mult, op1=ALU.mult,
                                                                        accum_out=yT[:, t:t + 1]), r=rk + [K("S")], w=[K("junk"), kq("yT")])

    def post(b):
        q = b % 2
        t0 = b * TB
        kq = lambda *n: K(*n, q if (n[0] in ("v", "yT", "bv", "g", "stg") or n[0].startswith("pin")) else 0)
        yT = T["yT"][q]; cen = T["cen"][q]; sq = T["sq"][q]; o = T["o"][q]
        ps, pk_ = nps()
        p.pe(lambda e, ps=ps: e.matmul(ps[:, :], lhsT=bones[:, :], rhs=yT[:, :], start=True, stop=True), r=[kq("yT"), K("bones")], w=[pk_])
        p.dve(lambda e, ps=ps: e.scalar_tensor_tensor(out=cen[:, :], in0=ps[:, :], scalar=-1.0 / 64, in1=yT[:, :], op0=ALU.mult, op1=ALU.add), r=[pk_, kq("yT")], w=[kq("cen")])
        p.dve(lambda e: e.tensor_mul(out=sq[:, :], in0=cen[:, :], in1=cen[:, :]), r=[kq("cen")], w=[kq("sq")])
        ps, pk_ = nps()
        p.pe(lambda e, ps=ps: e.matmul(ps[:, :], lhsT=bones[:, :], rhs=sq[:, :], start=True, stop=True), r=[kq("sq"), K("bones")], w=[pk_])
        p.act(lambda e, ps=ps: e.activation(out=sq[:, :], in_=ps[:, :], func=AF.Sqrt, bias=c("eps"), scale=1.0 / 64), r=[pk_, K("cv")], w=[kq("sq")])
        p.dve(lambda e: e.reciprocal(out=sq[:, :], in_=sq[:, :]), r=[kq("sq")], w=[kq("sq")])
        p.dve(lambda e: e.tensor_mul(out=cen[:, :], in0=cen[:, :], in1=sq[:, :]), r=[kq("cen"), kq("sq")], w=[kq("cen")])
        p.dve(lambda e: e.tensor_scalar(out=cen[:, :], in0=cen[:, :], scalar1=c("lnx_g"), scalar2=c("lnx_b"), op0=ALU.mult, op1=ALU.add), r=[kq("cen"), K("cv")], w=[kq("cen")])
        p.dve(lambda e: e.tensor_add(out=cen[:, :], in0=cen[:, :], in1=T["bv"][q][:, :]), r=[kq("cen"), kq("bv")], w=[kq("cen")])
        p.dve(lambda e: e.tensor_mul(out=o[:, :], in0=cen[:, :], in1=T["g"][q][:, :]), r=[kq("cen"), kq("g")], w=[kq("o")])
        p.dma("sp", d["y"][:, t0:t0 + TB], o[:, :], r=[kq("o")], w=[K("ydram", b)])

    prep(0)
    for b in range(NB):
        if b + 1 < NB:
            prep(b + 1)
        scan(b)
        post(b)


def rwkv_host_inputs(c, l, inp):
    deep = l > 0
    H0 = 2 * c
    cols = slice(H0 * 64, H0 * 64 + 128)
    mu = inp["rw_mu_first"] if not deep else inp["rw_mu_deep"][l - 1]
    offs = np.cumsum([0, 1024, 1024, 1024, 96, 96, 256] + ([64] if deep else []))
    cvm = np.zeros((128, len(CV_NAMES)), np.float32)
    def put(name, vec):
        cvm[:len(vec), CVI[name]] = vec
    put("mu_r", mu[offs[0]:offs[1]][cols]); put("mu_k", mu[offs[1]:offs[2]][cols]); put("mu_v", mu[offs[2]:offs[3]][cols])
    put("mu_xw", mu[offs[3]:offs[4]]); put("mu_xa", mu[offs[4]:offs[5]])
    put("mu_xg0", mu[offs[5]:offs[5] + 128]); put("mu_xg1", mu[offs[5] + 128:offs[6]])
    if deep:
        put("mu_xv", mu[offs[6]:offs[7]])
        put("v0", inp["rw_v0"][l - 1][cols])
        cvm[:, CVI["fdeep"]] = 1.0
    put("w0", inp["rw_w0"][l][cols]); put("a0", inp["rw_a0"][l][cols])
    put("k_k", inp["rw_k_k"][l][cols]); put("k_a", inp["rw_k_a"][l][cols])
    put("r_k", inp["rw_r_k"][l].reshape(-1)[cols]); put("lnx_g", inp["rw_lnx_g"][l][cols]); put("lnx_b", inp["rw_lnx_b"][l][cols])
    cvm[:, CVI["eps"]] = RW_GN_EPS
    cvm[:, CVI["tiny"]] = 1e-12
    bones = np.zeros((128, 128), np.float32)
    bones[:64, :64] = 1; bones[64:, 64:] = 1
    m = {"rw_cv": cvm, "rw_bones": bones,
         "rw_w_up": np.ascontiguousarray(inp["rw_w_up"][l][:, cols]), "rw_a_up": np.ascontiguousarray(inp["rw_a_up"][l][:, cols]),
         "rw_g_up": np.ascontiguousarray(inp["rw_g_up"][l][:, cols])}
    m["rw_v_up"] = np.ascontiguousarray(inp["rw_v_up"][l - 1][:, cols]) if deep else np.zeros((64, 128), np.float32)
    return m

TINY = 1e-30


def emit_nsa(p, S, d, pfx="nsa"):
    NQ = S // 128
    NSEL = S // 64
    NC = S // 16
    NCC = (NC + 127) // 128
    NSC = (NSEL + 127) // 128
    K = lambda *a: (pfx,) + a

    def const(name, shape, dt, src, eng="pool"):
        t = p.sb(shape, dt)
        p.dma(eng, t[tuple(slice(None) for _ in shape)], src, w=[K(name)])
        return t
    ident = const("ident", [128, 128], F32, d["ident"], "sp")
    ptab = const("ptab", [128, 512], F32, d["ptab"], "sp")
    maskc = const("maskc", [128, 16, 128], BF16, d["maskc"])
    maskp = const("maskp", [128, 128], BF16, d["maskp"])
    tri = const("tri", [128, 128], BF16, d["tri"])
    triu = const("triu", [128, 128], BF16, d["triu"])
    ex = const("ex", [128, 64, 128], BF16, d["ex"])
    CK = [K(n) for n in ("ident", "ptab", "maskc", "maskp", "tri", "triu", "ex")]
    ksb = p.sb([64, S], BF16)
    big2 = p.sb([64, S], BF16)
    vsx = p.sb([128, NQ, 65], BF16)
    vwx = p.sb([128, NQ, 65], BF16)
    vcx = p.sb([128, NCC, 65], BF16)
    ovl = p.sb([128, NCC, NSEL], BF16)
    kcmpT = p.sb([64, NCC * 128], BF16)
    p.dma("pool", ksb[:, :], d["ksT"], w=[K("ksb")])
    for (t, src, nm) in ((vsx, "vs", "vsx"), (vwx, "vw", "vwx")):
        for c0 in range(0, NQ, 16):
            p.dma("pool", t[:, c0:c0 + 16, 0:64], d[src][c0 * 128:(c0 + 16) * 128, :].rearrange("(c p) x -> p c x", p=128), w=[K(nm, 0)])
        p.pool(lambda e, t=t: e.memset(t[:, :, 64:65], 1.0), w=[K(nm, 1)])
    if NC >= 128:
        p.dma("pool", ovl[:, :, :], d["ovl"].rearrange("(c p) n -> p c n", p=128), w=[K("ovl")])
    else:
        p.pool(lambda e: e.memset(ovl[:, :, :], 0.0), w=[K("ovl")])
        p.dma("pool", ovl[:NC, 0, :], d["ovl"], w=[K("ovl")])
    p.pool(lambda e: e.memset(vcx[:, :, 64:65], 1.0), w=[K("vcx", 1)])

    NPS = 8
    pss = [p.ps([128, 512]) for _ in range(NPS)]
    PK = lambda i: K("ps", i)
    B_OC, B_IMP0, B_IMP1, B_ST0, B_ST1, B_MK, B_SEL, B_WIN = range(8)

    w1 = p.sb([64, 32, 128], BF16)
    peT = p.sb([64, 32], BF16)
    b1 = p.sb([128, 1])
    w2 = p.sb([128, 64], BF16)
    bias = p.sb([128, 1])
    hx = p.sb([128, 512]); hu = p.sb([128, 512])
    hidT = p.sb([128, NCC * 128], BF16)
    p.dve(lambda e: e.memset(hidT[:, :], 0.0), w=[K("hidT")])
    p.dve(lambda e: e.memset(kcmpT[:, :], 0.0), w=[K("kcmpT")])
    nvalid = NC - 1
    for kv in range(2):
        p.dma("pool", big2[:, :], d["kcT"] if kv == 0 else d["vcT"], w=[K("big2")])
        p.dma("pool", w1[:, :, :], d["w1"][kv], w=[K("w1")])
        p.dma("pool", peT[:, :], d["peT"][kv], w=[K("peT")])
        p.dma("sp", b1[:, :], d["b1"][kv], w=[K("b1")])
        p.dma("pool", w2[:, :], d["w2"][kv], w=[K("w2")])
        for l in range(32):
            p.pe(lambda e, l=l: e.matmul(pss[B_MK][:, 0:1], lhsT=w1[:, l, :], rhs=peT[:, l:l + 1], start=(l == 0), stop=(l == 31)),
                 r=[K("w1"), K("peT")], w=[PK(B_MK)])
        p.dve(lambda e: e.tensor_add(out=bias[:, :], in0=pss[B_MK][:, 0:1], in1=b1[:, :]), r=[PK(B_MK), K("b1")], w=[K("bias")])
        b3 = big2[:, :].rearrange("p (c x) -> p c x", x=16)
        for c0 in range(0, nvalid, 512):
            n = min(512, nvalid - c0)
            ps = pss[B_ST0]
            for l in range(32):
                rhs = b3[:, c0:c0 + n, l] if l < 16 else b3[:, c0 + 1:c0 + 1 + n, l - 16]
                p.pe(lambda e, l=l, rhs=rhs, n=n, ps=ps: e.matmul(ps[:, 0:n], lhsT=w1[:, l, :], rhs=rhs, start=(l == 0), stop=(l == 31)),
                     r=[K("w1"), K("big2")], w=[PK(B_ST0)])
            p.dve(lambda e, n=n, ps=ps: e.tensor_scalar(out=hx[:, 0:n], in0=ps[:, 0:n], scalar1=bias[:, 0:1], scalar2=None, op0=ALU.add), r=[PK(B_ST0), K("bias")], w=[K("hx")])
            p.dve(lambda e, n=n: e.tensor_mul(out=hu[:, 0:n], in0=hx[:, 0:n], in1=hx[:, 0:n]), r=[K("hx")], w=[K("hu")])
            p.dve(lambda e, n=n: e.tensor_scalar(out=hu[:, 0:n], in0=hu[:, 0:n], scalar1=0.044715, scalar2=1.0, op0=ALU.mult, op1=ALU.add), r=[K("hu")], w=[K("hu")])
            p.dve(lambda e, n=n: e.tensor_mul(out=hu[:, 0:n], in0=hu[:, 0:n], in1=hx[:, 0:n]), r=[K("hu"), K("hx")], w=[K("hu")])
            p.act(lambda e, n=n: e.activation(out=hu[:, 0:n], in_=hu[:, 0:n], func=AF.Tanh, scale=0.7978845608028654), r=[K("hu")], w=[K("hu")])
            p.dve(lambda e, n=n: e.tensor_scalar(out=hu[:, 0:n], in0=hu[:, 0:n], scalar1=1.0, scalar2=0.5, op0=ALU.add, op1=ALU.mult), r=[K("hu")], w=[K("hu")])
            p.dve(lambda e, n=n, c0=c0: e.tensor_mul(out=hidT[:, c0:c0 + n], in0=hu[:, 0:n], in1=hx[:, 0:n]), r=[K("hu"), K("hx")], w=[K("hidT")])
            if kv == 0:
                p.pe(lambda e, n=n, c0=c0: e.matmul(pss[B_ST1][0:64, 0:n], lhsT=w2[:, :], rhs=hidT[:, c0:c0 + n], start=True, stop=True), r=[K("w2"), K("hidT")], w=[PK(B_ST1)])
                p.act(lambda e, n=n, c0=c0: e.copy(out=kcmpT[:, c0:c0 + n], in_=pss[B_ST1][0:64, 0:n]), r=[PK(B_ST1)], w=[K("kcmpT")])
        if kv == 1:
            for j in range(NCC):
                p.pe(lambda e, j=j: e.matmul(pss[B_ST1][:, 0:64], lhsT=hidT[:, j * 128:(j + 1) * 128], rhs=w2[:, :], start=True, stop=True), r=[K("w2"), K("hidT")], w=[PK(B_ST1)])
                p.act(lambda e, j=j: e.copy(out=vcx[:, j, 0:64], in_=pss[B_ST1][:, 0:64]), r=[PK(B_ST1)], w=[K("vcx", 0)])
    p.dma("pool", big2[:, :], d["kwT"], w=[K("big2")])
    kwb = big2

    qf = [p.sb([64, 4, 128]) for _ in range(2)]
    qb = [p.sb([64, 4, 128], BF16) for _ in range(2)]
    gt = [p.sb([128, 6]) for _ in range(2)]
    E = [p.sb([128, 512], BF16) for _ in range(3)]
    en = [0]
    mk = [p.sb([128, 128], BF16) for _ in range(3)]
    mn = [0]
    zz = p.sb([128, 16])
    imp = p.sb([128, NSEL]); sc2 = p.sb([128, NSEL]); sel = p.sb([128, NSEL]); selb = p.sb([128, NSEL])
    m8 = p.sb([128, 16])
    selT = p.sb([128, NSC, 128], BF16)
    p.dve(lambda e: e.memset(selT[:, :, :], 0.0), w=[K("selT")])
    outq = p.sb([128, 2, 64])
    yo = [p.sb([128, 512]) for _ in range(2)]

    def qblock(qi):
        q2 = qi % 2
        kq = lambda *n: K(*n, q2)
        t0 = qi * 128
        p.dma("sp", qf[q2][:, :, :], d["qT"][:, :, t0:t0 + 128].rearrange("h x t -> x h t"), w=[kq("qf")])
        p.act(lambda e: e.mul(out=qb[q2][:, :, :], in_=qf[q2][:, :, :], mul=0.125), r=[kq("qf")], w=[kq("qb")])
        p.dma("sp", gt[q2][:, :], d["gate"][t0:t0 + 128, :], w=[kq("gt")])
        p.act(lambda e: e.activation(out=gt[q2][:, :], in_=gt[q2][:, :], func=AF.Sigmoid), r=[kq("gt")], w=[kq("gt")])
        qall = qb[q2][:, :, :].rearrange("x h t -> x (h t)")
        ncc = (8 * qi + 6) // 128 + 1
        OC = pss[B_OC][:, 0:260].rearrange("p (h x) -> p h x", h=4)
        for cc in range(ncc):
            sb_ = B_ST0 if cc % 2 == 0 else B_ST1
            p.pe(lambda e, cc=cc, sb_=sb_: e.matmul(pss[sb_][:, :], lhsT=kcmpT[:, cc * 128:(cc + 1) * 128], rhs=qall, start=True, stop=True),
                 r=[K("kcmpT"), kq("qb")], w=[PK(sb_)])
            ei = en[0] % 3; en[0] += 1
            Et = E[ei]
            p.act(lambda e, sb_=sb_, Et=Et: e.activation(out=Et[:, :], in_=pss[sb_][:, :], func=AF.Exp), r=[PK(sb_)], w=[K("E", ei)])
            if cc == ncc - 1:
                pat = qi % 16
                p.dve(lambda e, Et=Et, pat=pat: e.tensor_mul(out=Et[:, :].rearrange("p (h t) -> p h t", h=4), in0=Et[:, :].rearrange("p (h t) -> p h t", h=4),
                                                             in1=maskc[:, pat, :].unsqueeze(1).to_broadcast([128, 4, 128])), r=[K("E", ei), K("maskc")], w=[K("E", ei)])
            if cc == ncc - 2 and qi % 16 == 0:
                p.dve(lambda e, Et=Et: e.tensor_mul(out=Et[:, :].rearrange("p (h t) -> p h t", h=4), in0=Et[:, :].rearrange("p (h t) -> p h t", h=4),
                                                    in1=maskp[:, :].unsqueeze(1).to_broadcast([128, 4, 128])), r=[K("E", ei), K("maskp")], w=[K("E", ei)])
            for h in range(4):
                p.pe(lambda e, h=h, Et=Et, cc=cc: e.matmul(OC[:, h, :], lhsT=Et[:, h * 128:(h + 1) * 128], rhs=vcx[:, cc, :], start=(cc == 0 and h == 0), stop=(cc == ncc - 1), skip_group_check=True),
                     r=[K("E", ei), K("vcx", 0), K("vcx", 1)], w=[PK(B_OC)])
                bi = B_IMP0 if h < 2 else B_IMP1
                hh = h % 2
                p.pe(lambda e, h=h, Et=Et, cc=cc, bi=bi, hh=hh: e.matmul(pss[bi][:, hh * 256:hh * 256 + NSEL], lhsT=Et[:, h * 128:(h + 1) * 128], rhs=ovl[:, cc, :],
                                                                      start=(cc == 0 and hh == 0), stop=(cc == ncc - 1), skip_group_check=True),
                     r=[K("E", ei), K("ovl")], w=[PK(bi)])
        p.dve(lambda e: e.tensor_scalar(out=zz[:, 0:4], in0=OC[:, :, 64], scalar1=TINY, scalar2=None, op0=ALU.max), r=[PK(B_OC)], w=[K("zz", 0)])
        p.dve(lambda e: e.reciprocal(out=zz[:, 0:4], in_=zz[:, 0:4]), r=[K("zz", 0)], w=[K("zz", 0)])
        for h in range(4):
            bi = B_IMP0 if h < 2 else B_IMP1
            hh = h % 2
            if h == 0:
                p.dve(lambda e, bi=bi, hh=hh: e.tensor_scalar(out=imp[:, :], in0=pss[bi][:, hh * 256:hh * 256 + NSEL], scalar1=zz[:, 0:1], scalar2=None, op0=ALU.mult),
                      r=[PK(bi), K("zz", 0)], w=[K("imp")])
            else:
                p.dve(lambda e, bi=bi, hh=hh, h=h: e.scalar_tensor_tensor(out=imp[:, :], in0=pss[bi][:, hh * 256:hh * 256 + NSEL], scalar=zz[:, h:h + 1], in1=imp[:, :],
                                                                         op0=ALU.mult, op1=ALU.add), r=[PK(bi), K("zz", 0), K("imp")], w=[K("imp")])
        for h in range(2):
            p.dve(lambda e, h=h: e.tensor_mul(out=zz[:, 4 + h:5 + h], in0=zz[:, h:h + 1], in1=gt[q2][:, 3 * h:3 * h + 1]), r=[K("zz", 0), kq("gt")], w=[K("zz", 1, h)])
            p.dve(lambda e, h=h: e.tensor_scalar(out=outq[:, h, :], in0=OC[:, h, 0:64], scalar1=zz[:, 4 + h:5 + h], scalar2=None, op0=ALU.mult),
                  r=[PK(B_OC), K("zz", 1, h)], w=[K("outq", h)])
        off = 256 - 2 * qi
        p.dve(lambda e: e.tensor_add(out=imp[:, :], in0=imp[:, :], in1=ptab[:, off:off + NSEL]), r=[K("imp"), K("ptab")], w=[K("imp")])
        if qi > 0:
            p.dve(lambda e: e.tensor_scalar(out=imp[:, 0:1], in0=imp[:, 0:1], scalar1=1e4, scalar2=None, op0=ALU.add), r=[K("imp")], w=[K("imp")])
        p.dve(lambda e: e.max(out=m8[:, 0:8], in_=imp[:, :]), r=[K("imp")], w=[K("m8", 0)])
        p.dve(lambda e: e.match_replace(out=sc2[:, :], in_to_replace=m8[:, 0:8], in_values=imp[:, :], imm_value=-1e38), r=[K("imp"), K("m8", 0)], w=[K("sc2")])
        p.dve(lambda e: e.max(out=m8[:, 8:16], in_=sc2[:, :]), r=[K("sc2")], w=[K("m8", 1)])
        p.dve(lambda e: e.tensor_scalar(out=sel[:, :], in0=imp[:, :], scalar1=m8[:, 15:16], scalar2=None, op0=ALU.is_ge), r=[K("imp"), K("m8", 1)], w=[K("sel")])
        p.dve(lambda e: e.tensor_scalar(out=sc2[:, :], in0=imp[:, :], scalar1=-5e29, scalar2=None, op0=ALU.is_gt), r=[K("imp"), K("sc2")], w=[K("sc2")])
        p.dve(lambda e: e.tensor_mul(out=selb[:, :], in0=sel[:, :], in1=sc2[:, :]), r=[K("sel"), K("sc2")], w=[K("selb")])
        if "dbg_sel" in d:
            p.dma("sp", d["dbg_sel"][t0:t0 + 128, :], selb[:, :], r=[K("selb")], w=[K("dbgsel", qi)])
        for j in range(NSC):
            w_ = min(128, NSEL - j * 128)
            p.pe(lambda e, j=j, w_=w_: e.transpose(out=pss[B_MK][:w_, 0:128], in_=selb[:, j * 128:j * 128 + w_], identity=ident[:, :]), r=[K("selb"), K("ident")], w=[PK(B_MK)])
            p.act(lambda e, j=j, w_=w_: e.copy(out=selT[:w_, j, :], in_=pss[B_MK][:w_, 0:128]), r=[PK(B_MK)], w=[K("selT")])
        q01 = qb[q2][:, 0:2, :].rearrange("x h t -> x (h t)")
        ACS = pss[B_SEL][:, 0:130].rearrange("p (h x) -> p h x", h=2)
        ACW = pss[B_WIN][:, 0:130].rearrange("p (h x) -> p h x", h=2)
        sti = [0]

        def attend(kc, kbuf, vbuf, vkeys, ACC, bank, first, last, mask_ap, mkey):
            sb_ = B_ST0 if sti[0] % 2 == 0 else B_ST1
            sti[0] += 1
            p.pe(lambda e: e.matmul(pss[sb_][:, 0:256], lhsT=kbuf[:, kc * 128:(kc + 1) * 128], rhs=q01, start=True, stop=True), r=[K("ksb"), K("big2"), kq("qb")], w=[PK(sb_)])
            ei = en[0] % 3; en[0] += 1
            Et = E[ei]
            p.act(lambda e: e.activation(out=Et[:, 0:256], in_=pss[sb_][:, 0:256], func=AF.Exp), r=[PK(sb_)], w=[K("E", ei)])
            if mask_ap is not None:
                p.dve(lambda e: e.tensor_mul(out=Et[:, 0:256].rearrange("p (h t) -> p h t", h=2), in0=Et[:, 0:256].rearrange("p (h t) -> p h t", h=2),
                                             in1=mask_ap.unsqueeze(1).to_broadcast([128, 2, 128])), r=[K("E", ei)] + mkey, w=[K("E", ei)])
            for h in range(2):
                p.pe(lambda e, h=h: e.matmul(ACC[:, h, :], lhsT=Et[:, h * 128:(h + 1) * 128], rhs=vbuf[:, kc, :], start=(first and h == 0), stop=last, skip_group_check=True),
                     r=[K("E", ei)] + vkeys, w=[PK(bank)])

        for kc in range(qi + 1):
            p.pe(lambda e, kc=kc: e.matmul(pss[B_MK][:, 0:128], lhsT=ex[:, kc % 64, :], rhs=selT[:, kc // 64, :], start=True, stop=True), r=[K("ex"), K("selT")], w=[PK(B_MK)])
            mi = mn[0] % 3; mn[0] += 1
            if kc == qi:
                p.dve(lambda e, mi=mi: e.tensor_mul(out=mk[mi][:, :], in0=pss[B_MK][:, 0:128], in1=tri[:, :]), r=[PK(B_MK), K("tri")], w=[K("mk", mi)])
            else:
                p.act(lambda e, mi=mi: e.copy(out=mk[mi][:, :], in_=pss[B_MK][:, 0:128]), r=[PK(B_MK)], w=[K("mk", mi)])
            attend(kc, ksb, vsx, [K("vsx", 0), K("vsx", 1)], ACS, B_SEL, kc == 0, kc == qi, mk[mi][:, :], [K("mk", mi)])
        kcs = [kc for kc in range(qi - 4, qi + 1) if kc >= 0]
        for kc in kcs:
            if kc == qi:
                m_ap, mkk = tri[:, :], [K("tri")]
            elif kc == qi - 4:
                m_ap, mkk = triu[:, :], [K("triu")]
            else:
                m_ap, mkk = None, []
            attend(kc, kwb, vwx, [K("vwx", 0), K("vwx", 1)], ACW, B_WIN, kc == kcs[0], kc == kcs[-1], m_ap, mkk)
        for bi_, (ACC, bank) in enumerate(((ACS, B_SEL), (ACW, B_WIN))):
            zc = 6 + 4 * bi_
            p.dve(lambda e, ACC=ACC, zc=zc: e.tensor_scalar(out=zz[:, zc:zc + 2], in0=ACC[:, :, 64], scalar1=TINY, scalar2=None, op0=ALU.max), r=[PK(bank)], w=[K("zz", 2, bi_)])
            p.dve(lambda e, zc=zc: e.reciprocal(out=zz[:, zc:zc + 2], in_=zz[:, zc:zc + 2]), r=[K("zz", 2, bi_)], w=[K("zz", 2, bi_)])
            for h in range(2):
                gcol = 3 * h + 1 + bi_
                p.dve(lambda e, zc=zc, h=h, gcol=gcol: e.tensor_mul(out=zz[:, zc + 2 + h:zc + 3 + h], in0=zz[:, zc + h:zc + h + 1], in1=gt[q2][:, gcol:gcol + 1]),
                      r=[K("zz", 2, bi_), kq("gt")], w=[K("zz", 3, bi_, h)])
                p.dve(lambda e, ACC=ACC, zc=zc, h=h: e.scalar_tensor_tensor(out=outq[:, h, :], in0=ACC[:, h, 0:64], scalar=zz[:, zc + 2 + h:zc + 3 + h], in1=outq[:, h, :],
                                                                            op0=ALU.mult, op1=ALU.add), r=[PK(bank), K("zz", 3, bi_, h), K("outq", h)], w=[K("outq", h)])
        yb = (qi // 4) % 2
        p.pe(lambda e: e.transpose(out=pss[B_MK][:, 0:128], in_=outq[:, :, :].rearrange("p h x -> p (h x)"), identity=ident[:, :]), r=[K("outq", 0), K("outq", 1), K("ident")], w=[PK(B_MK)])
        p.act(lambda e: e.copy(out=yo[yb][:, (qi % 4) * 128:(qi % 4 + 1) * 128], in_=pss[B_MK][:, 0:128]), r=[PK(B_MK)], w=[K("yo", yb, qi % 4)])
        if qi % 4 == 3:
            p.dma("sp", d["y"][:, (qi - 3) * 128:(qi + 1) * 128], yo[yb][:, :], r=[K("yo", yb, j) for j in range(4)], w=[K("ydram", qi)])

    for qi in range(NQ):
        qblock(qi)


def nsa_tables(S):
    NSEL = S // 64
    NC = S // 16
    c = np.arange(NC)
    cmp_start = c * 16
    sel_start = np.arange(NSEL) * 64
    ovl = ((cmp_start[:, None] < sel_start[None] + 64) & (cmp_start[:, None] + 32 > sel_start[None])).astype(np.float32)
    ovl[NC - 1:] = 0.0
    q = np.arange(128)
    rel = np.arange(512) - 256
    cq = (q // 64)[:, None]
    ptab = np.where(rel[None] > cq, -1e30, np.where((rel[None] == cq) | (rel[None] == cq - 1), 1e4, 0.0)).astype(np.float32)
    cl = np.arange(128)
    maskc = np.zeros((128, 16, 128), np.float32)
    for pat in range(16):
        maskc[:, pat, :] = (16 * (cl[:, None] - 8 * pat) + 31 <= q[None, :])
    maskp = np.ones((128, 128), np.float32)
    maskp[127, :] = (q >= 15)
    tri = (cl[:, None] <= q[None, :]).astype(np.float32)
    triu = (cl[:, None] > q[None, :]).astype(np.float32)
    ex = np.zeros((128, 64, 128), np.float32)
    for j in range(64):
        for key in range(128):
            ex[2 * j + key // 64, j, key] = 1.0
    return dict(ovl=ovl, ptab=ptab, maskc=maskc, maskp=maskp, tri=tri, triu=triu, ex=ex, ident=np.eye(128, dtype=np.float32))


def nsa_weights(l, inp):
    w1 = inp["nsa_cmp_w1"][l].reshape(2, 32, 64, 128).transpose(0, 2, 1, 3)
    peT = inp["nsa_cmp_pe"][l].transpose(0, 2, 1)
    return dict(w1=np.ascontiguousarray(w1), peT=np.ascontiguousarray(peT), b1=np.ascontiguousarray(inp["nsa_cmp_b1"][l][:, :, None]),
                w2=np.ascontiguousarray(inp["nsa_cmp_w2"][l]))

RET_GN_EPS = 1e-5


def emit_ret(p, S, d, pfx="ret"):
    TB = 512
    NB = S // TB
    K = lambda *a: (pfx,) + a
    cvr = p.sb([128, 4]); p.dma("sp", cvr[:, :], d["cvr"], w=[K("cvr")])
    dec = p.sb([128, 128]); p.dma("sp", dec[:, :], d["dec"], w=[K("dec")])
    qdec = p.sb([128, 128]); p.dma("sp", qdec[:, :], d["qdec"], w=[K("qdec")])
    ident = p.sb([128, 128]); p.dma("sp", ident[:, :], d["ident"], w=[K("ident")])
    R = p.sb([128, 128]); Rb = p.sb([128, 128], BF16)
    p.dve(lambda e: e.memset(R[:, :], 0.0), w=[K("R")])
    p.dve(lambda e: e.memset(Rb[:, :], 0.0), w=[K("Rb")])

    def mk(shape, dt=F32, n=2):
        return [p.sb(shape, dt) for _ in range(n)]
    fm = {n: mk([128, TB]) for n in ("q", "qs", "k", "ks", "cos", "sin")}
    tm = {n: mk([128, 4, 128]) for n in ("k", "v", "g", "cos", "sin")}
    qr = mk([128, TB], BF16); kr = mk([128, TB], BF16); qd = mk([128, TB], BF16)
    t1 = p.sb([128, TB]); t2 = p.sb([128, TB])
    krt = mk([128, 4, 128], BF16); vb = mk([128, 4, 128], BF16); sgt = mk([128, 4, 128])
    u1 = p.sb([128, 4, 128]); u2 = p.sb([128, 4, 128])
    AT = mk([128, 128], BF16)
    o_sb = mk([128, 128]); cen = mk([128, 128]); sq = mk([128, 128])
    st = mk([128, 4])
    yo = mk([128, TB])
    NPS = 6
    pss = [p.ps([128, 512]) for _ in range(NPS)]
    psn = [0]

    def nps():
        i = psn[0] % NPS
        psn[0] += 1
        return pss[i], K("ps", i)

    def blk(b):
        q = b % 2
        t0 = b * TB
        kq = lambda *n: K(*n, q)
        p.dma("sp", fm["q"][q][:, :], d["qT"][:, t0:t0 + TB], w=[kq("fq")])
        p.dma("sp", fm["qs"][q][0:64, :], d["qT"][64:128, t0:t0 + TB], w=[kq("fqs0")])
        p.dma("sp", fm["qs"][q][64:128, :], d["qT"][0:64, t0:t0 + TB], w=[kq("fqs1")])
        p.dma("sp", fm["k"][q][:, :], d["kT"][:, t0:t0 + TB], w=[kq("fk")])
        p.dma("sp", fm["ks"][q][0:64, :], d["kT"][64:128, t0:t0 + TB], w=[kq("fks0")])
        p.dma("sp", fm["ks"][q][64:128, :], d["kT"][0:64, t0:t0 + TB], w=[kq("fks1")])
        p.dma("sp", fm["cos"][q][:, :], d["cosF"][:, t0:t0 + TB], w=[kq("fcos")])
        p.dma("sp", fm["sin"][q][:, :], d["sinF"][:, t0:t0 + TB], w=[kq("fsin")])
        for n in ("k", "v", "g", "cos", "sin"):
            src = {"k": "k", "v": "v", "g": "g", "cos": "cosT", "sin": "sinT"}[n]
            p.dma("sp", tm[n][q][:, :, :], d[src][t0:t0 + TB, :].rearrange("(c p) x -> p c x", p=128), w=[kq("t" + n)])
        for (a, as_, o, on) in (("q", "qs", qr, "qr"), ("k", "ks", kr, "kr")):
            p.dve(lambda e, a=a: e.tensor_mul(out=t1[:, :], in0=fm[a][q][:, :], in1=fm["cos"][q][:, :]), r=[kq("f" + a), kq("fcos")], w=[K("t1")])
            p.pool(lambda e, as_=as_: e.tensor_mul(out=t2[:, :], in0=fm[as_][q][:, :], in1=fm["sin"][q][:, :]), r=[kq("f" + as_ + "0"), kq("f" + as_ + "1"), kq("fsin")], w=[K("t2")])
            p.dve(lambda e, o=o: e.tensor_add(out=o[q][:, :], in0=t1[:, :], in1=t2[:, :]), r=[K("t1"), K("t2")], w=[kq(on)])
        p.pool(lambda e: e.tensor_mul(out=qd[q][:, :].rearrange("p (c x) -> p c x", c=4), in0=qr[q][:, :].rearrange("p (c x) -> p c x", c=4),
                                      in1=qdec[:, :].unsqueeze(1).to_broadcast([128, 4, 128])), r=[kq("qr"), K("qdec")], w=[kq("qd")])
        p.dve(lambda e: e.tensor_mul(out=u1[:, :, :], in0=tm["k"][q][:, :, :], in1=tm["cos"][q][:, :, :]), r=[kq("tk"), kq("tcos")], w=[K("u1")])
        p.pool(lambda e: e.tensor_mul(out=u2[:, :, 0:64], in0=tm["k"][q][:, :, 64:128], in1=tm["sin"][q][:, :, 0:64]), r=[kq("tk"), kq("tsin")], w=[K("u2", 0)])
        p.pool(lambda e: e.tensor_mul(out=u2[:, :, 64:128], in0=tm["k"][q][:, :, 0:64], in1=tm["sin"][q][:, :, 64:128]), r=[kq("tk"), kq("tsin")], w=[K("u2", 1)])
        p.dve(lambda e: e.tensor_add(out=u1[:, :, :], in0=u1[:, :, :], in1=u2[:, :, :]), r=[K("u1"), K("u2", 0), K("u2", 1)], w=[K("u1")])
        p.dve(lambda e: e.tensor_scalar(out=krt[q][:, :, :], in0=u1[:, :, :], scalar1=cvr[:, 0:1], scalar2=None, op0=ALU.mult), r=[K("u1"), K("cvr")], w=[kq("krt")])
        p.act(lambda e: e.copy(out=vb[q][:, :, :], in_=tm["v"][q][:, :, :]), r=[kq("tv")], w=[kq("vb")])
        p.act(lambda e: e.activation(out=sgt[q][:, :, :], in_=tm["g"][q][:, :, :], func=AF.Silu), r=[kq("tg")], w=[kq("sg")])
        for ci in range(4):
            cq = (b * 4 + ci) % 2
            cs = slice(ci * 128, (ci + 1) * 128)
            ps, pk_ = nps()
            p.pe(lambda e, ps=ps, cs=cs: e.matmul(ps[:, 0:128], lhsT=kr[q][:, cs], rhs=qr[q][:, cs], start=True, stop=True), r=[kq("kr"), kq("qr")], w=[pk_])
            p.dve(lambda e, ps=ps, cq=cq: e.tensor_mul(out=AT[cq][:, :], in0=ps[:, 0:128], in1=dec[:, :]), r=[pk_, K("dec")], w=[K("AT", cq)])
            ps2, pk2 = nps()
            p.pe(lambda e, ps2=ps2, cq=cq, ci=ci: e.matmul(ps2[:, 0:128], lhsT=AT[cq][:, :], rhs=vb[q][:, ci, :], start=True, stop=False), r=[K("AT", cq), kq("vb")], w=[pk2])
            p.pe(lambda e, ps2=ps2, cs=cs: e.matmul(ps2[:, 0:128], lhsT=qd[q][:, cs], rhs=Rb[:, :], start=False, stop=True), r=[kq("qd"), K("Rb")], w=[pk2])
            ps3, pk3 = nps()
            p.pe(lambda e, ps3=ps3, ci=ci: e.matmul(ps3[:, 0:128], lhsT=krt[q][:, ci, :], rhs=vb[q][:, ci, :], start=True, stop=True), r=[kq("krt"), kq("vb")], w=[pk3])
            p.dve(lambda e, ps3=ps3: e.scalar_tensor_tensor(out=R[:, :], in0=R[:, :], scalar=cvr[:, 1:2], in1=ps3[:, 0:128], op0=ALU.mult, op1=ALU.add),
                  r=[K("R"), pk3, K("cvr")], w=[K("R")])
            p.act(lambda e: e.copy(out=Rb[:, :], in_=R[:, :]), r=[K("R")], w=[K("Rb")])
            o = o_sb[cq]; ce = cen[cq]; s_ = sq[cq]; stt_ = st[cq]
            p.act(lambda e, ps2=ps2, o=o: e.copy(out=o[:, :], in_=ps2[:, 0:128]), r=[pk2], w=[K("o", cq)])
            if "dbg_o" in d:
                p.dma("sp", d["dbg_o"][t0 + ci * 128:t0 + (ci + 1) * 128, :], o[:, :], r=[K("o", cq)], w=[K("dbgo", b, ci)])
                p.dma("sp", d["dbg_sg"][t0 + ci * 128:t0 + (ci + 1) * 128, :], sgt[q][:, ci, :], r=[kq("sg")], w=[K("dbgsg", b, ci)])
            p.dve(lambda e, o=o, stt_=stt_: e.tensor_reduce(out=stt_[:, 0:1], in_=o[:, :], axis=AX.X, op=ALU.add), r=[K("o", cq)], w=[K("st0", cq)])
            p.dve(lambda e, stt_=stt_: e.tensor_scalar(out=stt_[:, 1:2], in0=stt_[:, 0:1], scalar1=-1.0 / 128, scalar2=None, op0=ALU.mult), r=[K("st0", cq)], w=[K("st1", cq)])
            p.dve(lambda e, o=o, ce=ce, stt_=stt_: e.tensor_scalar(out=ce[:, :], in0=o[:, :], scalar1=stt_[:, 1:2], scalar2=None, op0=ALU.add), r=[K("o", cq), K("st1", cq)], w=[K("cen", cq)])
            p.dve(lambda e, ce=ce, s_=s_: e.tensor_mul(out=s_[:, :], in0=ce[:, :], in1=ce[:, :]), r=[K("cen", cq)], w=[K("sq", cq)])
            p.dve(lambda e, s_=s_, stt_=stt_: e.tensor_reduce(out=stt_[:, 2:3], in_=s_[:, :], axis=AX.X, op=ALU.add), r=[K("sq", cq)], w=[K("st2", cq)])
            p.act(lambda e, stt_=stt_: e.activation(out=stt_[:, 3:4], in_=stt_[:, 2:3], func=AF.Sqrt, bias=cvr[:, 2:3], scale=1.0 / 128), r=[K("st2", cq), K("cvr")], w=[K("st3", cq)])
            p.dve(lambda e, stt_=stt_: e.reciprocal(out=stt_[:, 3:4], in_=stt_[:, 3:4]), r=[K("st3", cq)], w=[K("st3", cq)])
            p.dve(lambda e, ce=ce, stt_=stt_, ci=ci: e.scalar_tensor_tensor(out=ce[:, :], in0=ce[:, :], scalar=stt_[:, 3:4], in1=sgt[q][:, ci, :], op0=ALU.mult, op1=ALU.mult),
                  r=[K("cen", cq), K("st3", cq), kq("sg")], w=[K("cen", cq)])
            ps4, pk4 = nps()
            p.pe(lambda e, ps4=ps4, ce=ce: e.transpose(out=ps4[:, 0:128], in_=ce[:, :], identity=ident[:, :]), r=[K("cen", cq), K("ident")], w=[pk4])
            p.act(lambda e, ps4=ps4, cs=cs: e.copy(out=yo[q][:, cs], in_=ps4[:, 0:128]), r=[pk4], w=[kq("yo", ci)])
        p.dma("sp", d["y"][:, t0:t0 + TB], yo[q][:, :], r=[kq("yo", ci) for ci in range(4)], w=[K("ydram", b)])

    for b in range(NB):
        blk(b)


def ret_tables(S):
    DK = 128
    inv_freq = (1.0 / (np.float32(10000.0) ** np.linspace(0.0, 1.0, DK // 2, dtype=np.float32))).astype(np.float32)
    ang = (np.arange(S, dtype=np.float32)[:, None] * inv_freq[None, :]).astype(np.float32)
    cos = np.cos(ang.astype(np.float64)).astype(np.float32); sin = np.sin(ang.astype(np.float64)).astype(np.float32)
    cosT = np.concatenate([cos, cos], 1); sinT = np.concatenate([-sin, sin], 1)
    return dict(cosT=np.ascontiguousarray(cosT), sinT=np.ascontiguousarray(sinT),
                cosF=np.ascontiguousarray(cosT.T), sinF=np.ascontiguousarray(sinT.T))


def ret_head_consts(h):
    C = 128
    log_g = np.log1p(-np.exp2(-5.0 - h))
    i = np.arange(C, dtype=np.float64)
    diff = i[None, :] - i[:, None]
    s = 128 ** -0.5
    dec = np.where(diff >= 0, np.exp(log_g * np.maximum(diff, 0.0)), 0.0) * s
    qdec = np.tile(np.exp(log_g * (i + 1.0))[None, :], (128, 1))
    cvr = np.zeros((128, 4), np.float32)
    cvr[:, 0] = np.exp(log_g * (C - 1.0 - i)) * s
    cvr[:, 1] = np.exp(log_g * C)
    cvr[:, 2] = RET_GN_EPS
    return dict(dec=dec.astype(np.float32), qdec=qdec.astype(np.float32), cvr=cvr, ident=np.eye(128, dtype=np.float32))

DN_ALPHA = 4 ** 0.25
LN_EPS = 1e-5
NEXP = 32


def emit_stageb(p, T, d, pfx="sb"):
    TB = 512
    NP = T // TB
    K = lambda *a: (pfx,) + a
    lnp = p.sb([128, 16, 4]); p.dma("sp", lnp[:, :, :], d["lnp"].rearrange("p (m c) -> p m c", c=4), w=[K("lnp")])
    wr = p.sb([128, 16, 36]); p.dma("sp", wr[:, :, :], d["wrS"].rearrange("p (m c) -> p m c", c=36), w=[K("wr")])
    brb = p.sb([128, 36]); p.dma("sp", brb[:, :], d["brb"], w=[K("brb")])
    selE = p.sb([32, 32, 128], BF16); p.dma("pool", selE[:, :, :], d["selE"].rearrange("k (e m) -> k e m", m=128), w=[K("selE")])
    ones = p.sb([128, 128]); p.dma("sp", ones[:, :], d["ones"], w=[K("ones")])
    ident = p.sb([128, 128]); p.dma("sp", ident[:, :], d["ident"], w=[K("ident")])
    epsc = p.sb([128, 1]); p.dve(lambda e: e.memset(epsc[:, :], LN_EPS), w=[K("epsc")])

    xf = p.sb([128, 16, TB])
    xb = p.sb([128, 16, TB], BF16)
    arena = p.sb([128, 55296], BF16)
    def carve(off, shape):
        n = int(np.prod(shape))
        v = arena[:, off:off + n]
        if len(shape) == 2:
            return v.rearrange("p (a b) -> p a b", a=shape[0])
        if len(shape) == 3:
            return v.rearrange("p (a b c) -> p a b c", a=shape[0], b=shape[1])
        return v
    yb = carve(0, [3, 8, TB])
    merb = carve(12288, [16, TB])
    wgs = [carve(20480, [16, 3, 128]), carve(26624, [16, 3, 128])]
    wbs = [carve(32768, [8, 3, 128]), carve(35840, [8, 3, 128])]
    wos = [carve(38912, [16, 128]), carve(40960, [16, 128])]
    wge = [carve(0, [16, 512]), carve(8192, [16, 512])]
    wue = [carve(16384, [16, 512]), carve(24576, [16, 512])]
    wde = [carve(32768, [4, 2048]), carve(40960, [4, 2048])]
    hT = [carve(49152, [4, TB]), carve(51200, [4, TB])]
    A1 = [K("phB1")]
    A2 = [K("phMoE")]
    mer = p.sb([128, TB]); sg = p.sb([128, TB]); tmp = p.sb([128, TB])
    mean = p.sb([128, TB]); rstd = p.sb([128, TB])
    gwe = [p.sb([128, TB]) for _ in range(2)]
    gwT = p.sb([32, TB], BF16)
    lg = p.sb([128, 36]); rt = p.sb([128, 64]); gw = p.sb([128, 4, 8]); t48 = p.sb([128, 4, 8])
    NPS = 8
    pss = [p.ps([128, 512]) for _ in range(NPS)]
    psn = [0]

    def nps():
        i = psn[0] % NPS
        psn[0] += 1
        return pss[i], K("ps", i)

    def layernorm(which, pi):
        ps_s, ks = nps()
        for m in range(16):
            p.pe(lambda e, m=m: e.matmul(ps_s[:, :], lhsT=ones[:, :], rhs=xf[:, m, :], start=(m == 0), stop=(m == 15)), r=[K("xf", m), K("ones")], w=[ks])
        ps_q, kq_ = nps()
        for m in range(16):
            p.dve(lambda e, m=m: e.tensor_mul(out=tmp[:, :], in0=xf[:, m, :], in1=xf[:, m, :]), r=[K("xf", m)], w=[K("tmp")])
            p.pe(lambda e, m=m: e.matmul(ps_q[:, :], lhsT=ones[:, :], rhs=tmp[:, :], start=(m == 0), stop=(m == 15)), r=[K("tmp"), K("ones")], w=[kq_])
        p.act(lambda e: e.mul(out=mean[:, :], in_=ps_s[:, :], mul=1.0 / 2048), r=[ks], w=[K("mean")])
        p.dve(lambda e: e.tensor_mul(out=tmp[:, :], in0=mean[:, :], in1=mean[:, :]), r=[K("mean")], w=[K("tmp")])
        p.dve(lambda e: e.scalar_tensor_tensor(out=rstd[:, :], in0=ps_q[:, :], scalar=1.0 / 2048, in1=tmp[:, :], op0=ALU.mult, op1=ALU.subtract), r=[kq_, K("tmp")], w=[K("rstd")])
        p.act(lambda e: e.activation(out=rstd[:, :], in_=rstd[:, :], func=AF.Sqrt, bias=epsc[:, 0:1], scale=1.0), r=[K("rstd"), K("epsc")], w=[K("rstd")])
        p.dve(lambda e: e.reciprocal(out=rstd[:, :], in_=rstd[:, :]), r=[K("rstd")], w=[K("rstd")])
        for m in range(16):
            p.dve(lambda e, m=m: e.tensor_sub(out=xf[:, m, :], in0=xf[:, m, :], in1=mean[:, :]), r=[K("xf", m), K("mean")], w=[K("xf", m)])
            p.dve(lambda e, m=m: e.tensor_mul(out=xf[:, m, :], in0=xf[:, m, :], in1=rstd[:, :]), r=[K("xf", m), K("rstd")], w=[K("xf", m)])
            p.dve(lambda e, m=m: e.tensor_scalar(out=xf[:, m, :], in0=xf[:, m, :], scalar1=lnp[:, m, 2 * which:2 * which + 1], scalar2=lnp[:, m, 2 * which + 1:2 * which + 2],
                                                 op0=ALU.mult, op1=ALU.add), r=[K("xf", m), K("lnp")], w=[K("xf", m)])

    def tpass(pi):
        t0 = pi * TB
        p.dma("sp", xf[:, :, :], d["xT"][:, t0:t0 + TB].rearrange("(m p) t -> p m t", p=128), r=[("xTc_dram",)], w=[K("xf", m) for m in range(16)])
        for m in range(16):
            p.act(lambda e, m=m: e.copy(out=xb[:, m, :], in_=xf[:, m, :]), r=[K("xf", m)], w=[K("xb", m)])
        for b in range(3):
            p.dma("pool", yb[:, b, :, :], d["yT"][b, :, t0:t0 + TB].rearrange("(kc p) t -> p kc t", p=128), r=[("yT_dram", b)], w=[K("yb", b)] + (A2 if b == 0 else []))
        for m in range(16):
            q = m % 2
            p.dma("pool", wgs[q][:, :, :, :], d["wgS"][m].rearrange("p (kc b j) -> p kc b j", kc=16, b=3), w=[K("wgs", q)])
            p.dma("pool", wbs[q][:, :, :, :], d["wbrS"][m].rearrange("p (kc b j) -> p kc b j", kc=8, b=3), w=[K("wbs", q)])
            for b in range(3):
                psG, kG = nps()
                for kc in range(16):
                    p.pe(lambda e, psG=psG, kc=kc, b=b, q=q: e.matmul(psG[:, :], lhsT=wgs[q][:, kc, b, :], rhs=xb[:, kc, :], start=(kc == 0), stop=(kc == 15)),
                         r=[K("wgs", q), K("xb", kc)] + A1, w=[kG])
                psY, kY = nps()
                for kc in range(8):
                    p.pe(lambda e, psY=psY, kc=kc, b=b, q=q: e.matmul(psY[:, :], lhsT=wbs[q][:, kc, b, :], rhs=yb[:, b, kc, :], start=(kc == 0), stop=(kc == 7)),
                         r=[K("wbs", q), K("yb", b)] + A1, w=[kY])
                p.act(lambda e, psG=psG: e.activation(out=sg[:, :], in_=psG[:, :], func=AF.Sigmoid), r=[kG], w=[K("sg")])
                if b == 0:
                    p.dve(lambda e, psY=psY: e.tensor_mul(out=mer[:, :], in0=psY[:, :], in1=sg[:, :]), r=[kY, K("sg")], w=[K("mer")])
                else:
                    p.dve(lambda e, psY=psY: e.tensor_mul(out=tmp[:, :], in0=psY[:, :], in1=sg[:, :]), r=[kY, K("sg")], w=[K("tmp")])
                    p.dve(lambda e: e.tensor_add(out=mer[:, :], in0=mer[:, :], in1=tmp[:, :]), r=[K("mer"), K("tmp")], w=[K("mer")])
            p.act(lambda e, m=m: e.copy(out=merb[:, m, :], in_=mer[:, :]), r=[K("mer")], w=[K("merb", m)])
        for m2 in range(16):
            q = m2 % 2
            p.dma("pool", wos[q][:, :, :], d["woutS"][m2].rearrange("p (kc j) -> p kc j", kc=16), w=[K("wos", q)])
            ps, kp = nps()
            for kc in range(16):
                p.pe(lambda e, ps=ps, kc=kc, q=q: e.matmul(ps[:, :], lhsT=wos[q][:, kc, :], rhs=merb[:, kc, :], start=(kc == 0), stop=(kc == 15)),
                     r=[K("wos", q), K("merb", kc)] + A1, w=[kp])
            p.dve(lambda e, ps=ps, m2=m2: e.scalar_tensor_tensor(out=xf[:, m2, :], in0=xf[:, m2, :], scalar=DN_ALPHA, in1=ps[:, :], op0=ALU.mult, op1=ALU.add),
                  r=[K("xf", m2), kp], w=[K("xf", m2)])
        layernorm(0, pi)
        for m in range(16):
            p.act(lambda e, m=m: e.copy(out=xb[:, m, :], in_=xf[:, m, :]), r=[K("xf", m)], w=[K("xb", m)])
        for j in range(TB // 128):
            js = slice(j * 128, (j + 1) * 128)
            ps, kp = nps()
            for m in range(16):
                p.pe(lambda e, ps=ps, m=m, js=js: e.matmul(ps[:, 0:36], lhsT=xf[:, m, js], rhs=wr[:, m, :], start=(m == 0), stop=(m == 15)), r=[K("xf", m), K("wr")], w=[kp])
            R = lambda *a: K("rt", *a)
            p.dve(lambda e, ps=ps: e.tensor_add(out=lg[:, :], in0=ps[:, 0:36], in1=brb[:, :]), r=[kp, K("brb")], w=[K("lg")])
            p.dve(lambda e: e.tensor_reduce(out=rt[:, 0:1], in_=lg[:, 0:4], axis=AX.X, op=ALU.max), r=[K("lg")], w=[R(0)])
            p.dve(lambda e: e.tensor_scalar(out=rt[:, 1:2], in0=rt[:, 0:1], scalar1=-1.0, scalar2=None, op0=ALU.mult), r=[R(0)], w=[R(1)])
            p.dve(lambda e: e.tensor_scalar(out=rt[:, 4:8], in0=lg[:, 0:4], scalar1=rt[:, 0:1], scalar2=None, op0=ALU.is_ge), r=[K("lg"), R(0)], w=[R(4)])
            p.act(lambda e: e.activation(out=rt[:, 8:12], in_=lg[:, 0:4], func=AF.Exp, bias=rt[:, 1:2], scale=1.0), r=[K("lg"), R(1)], w=[R(8)])
            p.dve(lambda e: e.tensor_reduce(out=rt[:, 2:3], in_=rt[:, 8:12], axis=AX.X, op=ALU.add), r=[R(8)], w=[R(2)])
            p.dve(lambda e: e.reciprocal(out=rt[:, 3:4], in_=rt[:, 2:3]), r=[R(2)], w=[R(3)])
            p.dve(lambda e: e.tensor_mul(out=t48[:, :, :], in0=lg[:, 4:36].rearrange("p (g x) -> p g x", g=4), in1=rt[:, 4:8].unsqueeze(2).to_broadcast([128, 4, 8])),
                  r=[K("lg"), R(4)], w=[K("t48")])
            p.dve(lambda e: e.tensor_reduce(out=rt[:, 20:28], in_=t48[:, :, :].rearrange("p g x -> p x g"), axis=AX.X, op=ALU.add), r=[K("t48")], w=[R(20)])
            p.dve(lambda e: e.tensor_reduce(out=rt[:, 12:13], in_=rt[:, 20:28], axis=AX.X, op=ALU.max), r=[R(20)], w=[R(12)])
            p.dve(lambda e: e.tensor_scalar(out=rt[:, 28:36], in0=rt[:, 20:28], scalar1=rt[:, 12:13], scalar2=None, op0=ALU.is_ge), r=[R(20), R(12)], w=[R(28)])
            p.dve(lambda e: e.scalar_tensor_tensor(out=rt[:, 36:44], in0=rt[:, 28:36], scalar=-1e30, in1=rt[:, 20:28], op0=ALU.mult, op1=ALU.add), r=[R(28), R(20)], w=[R(36)])
            p.dve(lambda e: e.tensor_reduce(out=rt[:, 13:14], in_=rt[:, 36:44], axis=AX.X, op=ALU.max), r=[R(36)], w=[R(13)])
            p.dve(lambda e: e.tensor_scalar(out=rt[:, 44:52], in0=rt[:, 36:44], scalar1=rt[:, 13:14], scalar2=None, op0=ALU.is_ge), r=[R(36), R(13)], w=[R(44)])
            p.dve(lambda e: e.tensor_sub(out=rt[:, 14:15], in0=rt[:, 13:14], in1=rt[:, 12:13]), r=[R(12), R(13)], w=[R(14)])
            p.act(lambda e: e.activation(out=rt[:, 15:16], in_=rt[:, 14:15], func=AF.Exp), r=[R(14)], w=[R(15)])
            p.dve(lambda e: e.tensor_scalar(out=rt[:, 16:17], in0=rt[:, 15:16], scalar1=1.0, scalar2=None, op0=ALU.add), r=[R(15)], w=[R(16)])
            p.dve(lambda e: e.reciprocal(out=rt[:, 16:17], in_=rt[:, 16:17]), r=[R(16)], w=[R(16)])
            p.dve(lambda e: e.tensor_mul(out=rt[:, 17:18], in0=rt[:, 15:16], in1=rt[:, 16:17]), r=[R(15), R(16)], w=[R(17)])
            p.dve(lambda e: e.tensor_scalar(out=rt[:, 16:18], in0=rt[:, 16:18], scalar1=rt[:, 3:4], scalar2=None, op0=ALU.mult), r=[R(16), R(17), R(3)], w=[R(16), R(17)])
            p.dve(lambda e: e.tensor_scalar(out=rt[:, 52:60], in0=rt[:, 28:36], scalar1=rt[:, 16:17], scalar2=None, op0=ALU.mult), r=[R(28), R(16)], w=[R(52)])
            p.dve(lambda e: e.scalar_tensor_tensor(out=rt[:, 52:60], in0=rt[:, 44:52], scalar=rt[:, 17:18], in1=rt[:, 52:60], op0=ALU.mult, op1=ALU.add), r=[R(44), R(17), R(52)], w=[R(52)])
            p.dve(lambda e: e.tensor_copy(out=gw[:, :, :], in_=rt[:, 52:60].unsqueeze(1).to_broadcast([128, 4, 8])), r=[R(52)], w=[K("gw")])
            p.dve(lambda e: e.tensor_mul(out=gw[:, :, :], in0=gw[:, :, :], in1=rt[:, 4:8].unsqueeze(2).to_broadcast([128, 4, 8])), r=[K("gw"), R(4)], w=[K("gw")])
            ps2, kp2 = nps()
            p.pe(lambda e, ps2=ps2: e.transpose(out=ps2[0:32, 0:128], in_=gw[:, :, :].rearrange("p g x -> p (g x)"), identity=ident[:, :]), r=[K("gw"), K("ident")], w=[kp2])
            p.act(lambda e, ps2=ps2, js=js: e.copy(out=gwT[:, js], in_=ps2[0:32, 0:128]), r=[kp2], w=[K("gwT", j)])
        if "dbg_gw" in d:
            p.dma("sp", d["dbg_gw"][:, t0:t0 + TB], gwT[:, :], r=[K("gwT", j) for j in range(4)], w=[K("dbggw", pi)])
            p.dma("sp", d["dbg_x1"][:, t0:t0 + TB].rearrange("(m p) t -> p m t", p=128), xf[:, :, :], r=[K("xf", m) for m in range(16)], w=[K("dbgx1", pi)])
        for m in range(16):
            p.act(lambda e, m=m: e.mul(out=xf[:, m, :], in_=xf[:, m, :], mul=DN_ALPHA), r=[K("xf", m)], w=[K("xf", m)])
        for ex in range(NEXP):
            q = ex % 2
            p.dma("pool", wge[q][:, :, :], d["w_gate"][ex].rearrange("(kc p) f -> p kc f", p=128), w=[K("wge", q)] + (A1 if ex == 0 else []))
            p.dma("pool", wue[q][:, :, :], d["w_up"][ex].rearrange("(kc p) f -> p kc f", p=128), w=[K("wue", q)])
            p.dma("pool", wde[q][:, :, :], d["w_down"][ex].rearrange("(f p) n -> p f n", p=128), w=[K("wde", q)])
            ps, kp = nps()
            p.pe(lambda e, ps=ps, ex=ex: e.matmul(ps[:, :], lhsT=selE[:, ex, :], rhs=gwT[:, :], start=True, stop=True), r=[K("selE")] + [K("gwT", j) for j in range(4)], w=[kp])
            p.act(lambda e, ps=ps, q=q: e.copy(out=gwe[q][:, :], in_=ps[:, :]), r=[kp], w=[K("gwe", q)])
            for f in range(4):
                psG, kG = nps()
                for kc in range(16):
                    p.pe(lambda e, psG=psG, kc=kc, f=f, q=q: e.matmul(psG[:, :], lhsT=wge[q][:, kc, f * 128:(f + 1) * 128], rhs=xb[:, kc, :], start=(kc == 0), stop=(kc == 15)),
                         r=[K("wge", q), K("xb", kc)] + A2, w=[kG])
                psU, kU = nps()
                for kc in range(16):
                    p.pe(lambda e, psU=psU, kc=kc, f=f, q=q: e.matmul(psU[:, :], lhsT=wue[q][:, kc, f * 128:(f + 1) * 128], rhs=xb[:, kc, :], start=(kc == 0), stop=(kc == 15)),
                         r=[K("wue", q), K("xb", kc)] + A2, w=[kU])
                p.act(lambda e, psG=psG: e.activation(out=sg[:, :], in_=psG[:, :], func=AF.Silu), r=[kG], w=[K("sg")])
                p.dve(lambda e, psU=psU: e.tensor_mul(out=tmp[:, :], in0=psU[:, :], in1=sg[:, :]), r=[kU, K("sg")], w=[K("tmp")])
                p.dve(lambda e, f=f, q=q: e.tensor_mul(out=hT[q][:, f, :], in0=tmp[:, :], in1=gwe[q][:, :]), r=[K("tmp"), K("gwe", q)], w=[K("hT", q, f)])
            for m in range(16):
                psD, kD = nps()
                for f in range(4):
                    p.pe(lambda e, psD=psD, f=f, m=m, q=q: e.matmul(psD[:, :], lhsT=wde[q][:, f, m * 128:(m + 1) * 128], rhs=hT[q][:, f, :], start=(f == 0), stop=(f == 3)),
                         r=[K("wde", q), K("hT", q, f)] + A2, w=[kD])
                p.dve(lambda e, psD=psD, m=m: e.tensor_add(out=xf[:, m, :], in0=xf[:, m, :], in1=psD[:, :]), r=[K("xf", m), kD], w=[K("xf", m)])
        layernorm(1, pi)
        for m in range(16):
            p.dma("sp", d["out"][m * 128:(m + 1) * 128, t0:t0 + TB], xf[:, m, :], r=[K("xf", m)], w=[("out_dram", pi, m)])

    for pi in range(NP):
        tpass(pi)


def stageb_host(l, inp, first):
    W = inp["w_in_first"] if first else inp["w_in_deep"][l - 1]
    g0 = W.shape[1] - 3 * 2048
    Wg = W[:, g0:].reshape(16, 128, 3, 16, 128)
    wgS = np.ascontiguousarray(Wg.transpose(3, 1, 0, 2, 4)).reshape(16, 128, 16 * 3 * 128)
    wbr = np.stack([inp["w_br_rw"][l], inp["w_br_nsa"][l], inp["w_br_ret"][l]])
    wbrS = np.ascontiguousarray(wbr.reshape(3, 8, 128, 16, 128).transpose(3, 2, 1, 0, 4)).reshape(16, 128, 8 * 3 * 128)
    woutS = np.ascontiguousarray(inp["w_out"][l].reshape(16, 128, 16, 128).transpose(2, 1, 0, 3)).reshape(16, 128, 16 * 128)
    lnp = np.stack([inp["ln1_g"][l], inp["ln1_b"][l], inp["ln2_g"][l], inp["ln2_b"][l]], -1).reshape(16, 128, 4).transpose(1, 0, 2)
    Wr = np.concatenate([inp["moe_w_grp"][l], inp["moe_w_exp"][l]], 1)
    wrS = Wr.reshape(16, 128, 36).transpose(1, 0, 2)
    br = np.concatenate([inp["moe_b_grp"][l], inp["moe_b_exp"][l]])
    selE = np.zeros((32, 32, 128), np.float32)
    for e in range(32):
        selE[e, e, :] = 1.0
    return dict(wgS=wgS, wbrS=wbrS, woutS=woutS, lnp=np.ascontiguousarray(lnp).reshape(128, 64), wrS=np.ascontiguousarray(wrS).reshape(128, 16 * 36),
                brb=np.ascontiguousarray(np.broadcast_to(br[None], (128, 36))), selE=selE.reshape(32, 32 * 128),
                ones=np.ones((128, 128), np.float32), ident=np.eye(128, dtype=np.float32),
                w_gate=inp["moe_w_gate"][l], w_up=inp["moe_w_up"][l], w_down=inp["moe_w_down"][l])

NF_A = 1664
NT_A = 518
_PROG_CACHE = {}


def build_stage_a(S):
    p = Prog()
    E = lambda n, sh, dt=F32: p.dram(n, sh, dt, kind="ExternalInput")
    O = lambda n, sh, dt=F32: p.dram(n, sh, dt, kind="ExternalOutput")
    NSEL = S // 64
    NC = S // 16
    d = dict(xT=E("xT", [2048, S]), wfm=E("wfm", [2048, NF_A]), wtm=E("wtm", [2048, NT_A]),
             ofm=p.dram("ofm", [NF_A, S]), otm=p.dram("otm", [S, NT_A]))
    emit_proj(p, S, NF_A, NT_A, d)
    p.phase()
    ofm, otm = d["ofm"], d["otm"]
    ident = E("ident", [128, 128])
    rw = dict(pr=ofm[0:128, :], pk=ofm[128:256, :], pv=ofm[256:384, :], pxw=ofm[384:480, :], pxa=ofm[480:576, :], pxg=ofm[576:832, :], pxv=ofm[832:896, :],
              cv=E("rw_cv", [128, len(CV_NAMES)]), ident=ident, bones=E("rw_bones", [128, 128]), w_up=E("rw_w_up", [96, 128]), a_up=E("rw_a_up", [96, 128]),
              g_up=E("rw_g_up", [256, 128]), v_up=E("rw_v_up", [64, 128]), vf_in=E("vf_in", [128, S]), vf_out=O("vf_out", [128, S]), y=O("y_rw", [128, S]),
              scr=p.dram("rw_scr", [2, S, 5, 64]))
    emit_rwkv(p, S, rw)
    p.phase()
    nsa = dict(qT=ofm[896:1152, :].rearrange("(h x) s -> h x s", h=4), kcT=ofm[1152:1216, :], vcT=ofm[1216:1280, :], ksT=ofm[1280:1344, :], kwT=ofm[1344:1408, :],
               vs=otm[:, 0:64], vw=otm[:, 64:128], gate=otm[:, 128:134],
               w1=E("nsa_w1", [2, 64, 32, 128]), peT=E("nsa_peT", [2, 64, 32]), b1=E("nsa_b1", [2, 128, 1]), w2=E("nsa_w2", [2, 128, 64]),
               ovl=E("nsa_ovl", [NC, NSEL]), ptab=E("nsa_ptab", [128, 512]), maskc=E("nsa_maskc", [128, 16, 128]), maskp=E("nsa_maskp", [128, 128]),
               tri=E("nsa_tri", [128, 128]), triu=E("nsa_triu", [128, 128]), ex=E("nsa_ex", [128, 64, 128]), ident=ident, y=O("y_nsa", [128, S]))
    emit_nsa(p, S, nsa)
    p.phase()
    ret = dict(qT=ofm[1408:1536, :], kT=ofm[1536:1664, :], k=otm[:, 134:262], v=otm[:, 262:390], g=otm[:, 390:518],
               cosF=E("ret_cosF", [128, S]), sinF=E("ret_sinF", [128, S]), cosT=E("ret_cosT", [S, 128]), sinT=E("ret_sinT", [S, 128]),
               dec=E("ret_dec", [128, 128]), qdec=E("ret_qdec", [128, 128]), cvr=E("ret_cvr", [128, 4]), ident=ident, y=O("y_ret", [128, S]))
    emit_ret(p, S, ret)
    return p.finalize()


def build_stage_b(T):
    p = Prog()
    E = lambda n, sh, dt=F32: p.dram(n, sh, dt, kind="ExternalInput")
    d = dict(xT=E("xTc", [2048, T]), yT=E("yT", [3, 1024, T]), wgS=E("wgS", [16, 128, 6144]), wbrS=E("wbrS", [16, 128, 3072]), woutS=E("woutS", [16, 128, 2048]),
             lnp=E("lnp", [128, 64]), wrS=E("wrS", [128, 576]), brb=E("brb", [128, 36]), selE=E("selE", [32, 4096]), ones=E("ones", [128, 128]), ident=E("ident", [128, 128]),
             w_gate=E("w_gate", [32, 2048, 512]), w_up=E("w_up", [32, 2048, 512]), w_down=E("w_down", [32, 512, 2048]),
             out=p.dram("out", [2048, T], kind="ExternalOutput"))
    emit_stageb(p, T, d)
    return p.finalize()


def stage_a_weights(c, l, inp):
    first = l == 0
    W = inp["w_in_first"] if first else inp["w_in_deep"][l - 1]
    rwc = 3520 if first else 3584
    n0 = rwc
    r0 = rwc + 2608
    hc = slice(c * 128, c * 128 + 128)
    g, hp = c // 2, c % 2
    heads = [g * 4 + 2 * hp, g * 4 + 2 * hp + 1, g * 4 + 2 * (1 - hp), g * 4 + 2 * (1 - hp) + 1]
    gs = slice(g * 64, (g + 1) * 64)
    fm = [W[:, 0:1024][:, hc], W[:, 1024:2048][:, hc], W[:, 2048:3072][:, hc], W[:, 3072:3168], W[:, 3168:3264], W[:, 3264:3520],
          (W[:, 3520:3584] if not first else np.zeros((2048, 64), np.float32))]
    q = W[:, n0:n0 + 1024]
    fm += [q[:, h * 64:(h + 1) * 64] for h in heads]
    kv = [W[:, n0 + 1024 + i * 256:n0 + 1024 + (i + 1) * 256] for i in range(6)]
    fm += [kv[0][:, gs], kv[1][:, gs], kv[2][:, gs], kv[4][:, gs]]
    rq, rk, rv, rg = [W[:, r0 + i * 1024:r0 + (i + 1) * 1024][:, hc] for i in range(4)]
    fm += [rq, rk]
    gate = W[:, n0 + 1024 + 1536:n0 + 1024 + 1536 + 48]
    gc = g * 12 + 2 * hp * 3
    tm = [kv[3][:, gs], kv[5][:, gs], gate[:, gc:gc + 6], rk, rv, rg]
    wfm = np.ascontiguousarray(np.concatenate(fm, 1)); wtm = np.ascontiguousarray(np.concatenate(tm, 1))
    assert wfm.shape[1] == NF_A and wtm.shape[1] == NT_A
    return wfm, wtm


def forward(inp, S):
    n = 8
    T = S // n
    inp = {k: np.asarray(v) for k, v in inp.items()}
    x = inp["x"][0, :S]
    xT = np.ascontiguousarray(x.T)
    if ("A", S) not in _PROG_CACHE:
        _PROG_CACHE[("A", S)] = build_stage_a(S)
        _PROG_CACHE[("B", T)] = build_stage_b(T)
    ncA, ncB = _PROG_CACHE[("A", S)], _PROG_CACHE[("B", T)]
    ntab = nsa_tables(S)
    rtab = ret_tables(S)
    ident = np.eye(128, dtype=np.float32)
    vf = [np.zeros((128, S), np.float32) for _ in range(n)]
    for l in range(2):
        nw = nsa_weights(l, inp)
        maps = []
        for c in range(n):
            wfm, wtm = stage_a_weights(c, l, inp)
            m = dict(xT=xT, wfm=wfm, wtm=wtm, ident=ident, vf_in=vf[c])
            m.update(rwkv_host_inputs(c, l, inp))
            m.update({"nsa_" + k: v for k, v in nw.items()})
            m.update({"nsa_" + k: v for k, v in ntab.items() if k != "ident"})
            m.update({"ret_" + k: v for k, v in rtab.items()})
            m.update({"ret_" + k: v for k, v in ret_head_consts(c).items() if k != "ident"})
            maps.append(m)
        resA = run_bass_kernel_spmd(ncA, maps, core_ids=list(range(n))).results
        vf = [r["vf_out"] for r in resA]
        yT = np.stack([np.concatenate([r[k] for r in resA], 0) for k in ("y_rw", "y_nsa", "y_ret")])
        hb = stageb_host(l, inp, l == 0)
        maps = []
        for c in range(n):
            m = dict(hb)
            m["xTc"] = np.ascontiguousarray(xT[:, c * T:(c + 1) * T])
            m["yT"] = np.ascontiguousarray(yT[:, :, c * T:(c + 1) * T])
            maps.append(m)
        resB = run_bass_kernel_spmd(ncB, maps, core_ids=list(range(n))).results
        xT = np.ascontiguousarray(np.concatenate([r["out"] for r in resB], 1))
    return np.ascontiguousarray(xT.T)[None].astype(np.float32)


def kernel(**inputs):
    return forward(inputs, 16384)
```

```python
import contextlib
import numpy as np
import concourse.bass as bass
import concourse.mybir as mybir
from concourse.bass_utils import run_bass_kernel_spmd

F32 = mybir.dt.float32
BF16 = mybir.dt.bfloat16
ALU = mybir.AluOpType
AF = mybir.ActivationFunctionType
AX = mybir.AxisListType

NDMA_SEM = 8
ARENA_BYTES = 206 * 1024


class Prog:
    def __init__(self):
        self.nc = bass.Bass("TRN2", target_bir_lowering=False)
        self.ops = []
        self.last_w = {}
        self.readers = {}
        self.stack = contextlib.ExitStack()
        self._n = 0
        self.arena = self.stack.enter_context(self.nc.sbuf_tensor("arena", [128, ARENA_BYTES // 2], BF16))
        self.bump = 0
        self.banks = [self.stack.enter_context(self.nc.psum_tensor(f"bank{i}", [128, 512], F32)) for i in range(8)]
        self.nbank = 0

    def dram(self, name, shape, dtype=F32, kind="Internal"):
        return self.nc.dram_tensor(name, list(shape), dtype, kind=kind).ap()

    def sb(self, shape, dtype=F32, name=None):
        esz = 4 if dtype == F32 else 2
        n = int(np.prod(shape[1:]))
        nbytes = (n * esz + 63) // 64 * 64
        off = self.bump
        self.bump += nbytes
        assert self.bump <= ARENA_BYTES, f"SBUF arena overflow: {self.bump}"
        v = self.arena[0:shape[0], off // 2: off // 2 + n * esz // 2]
        if dtype == F32:
            v = v.bitcast(F32)
        if len(shape) == 3:
            v = v.rearrange("p (a b) -> p a b", a=shape[1])
        elif len(shape) == 4:
            v = v.rearrange("p (a b c) -> p a b c", a=shape[1], b=shape[2])
        elif len(shape) == 5:
            v = v.rearrange("p (a b c d) -> p a b c d", a=shape[1], b=shape[2], c=shape[3])
        return v

    def ps(self, shape=None, dtype=F32, name=None):
        b = self.banks[self.nbank % 8]
        self.nbank += 1
        return b

    def phase(self):
        self.barrier()
        self.bump = 0
        self.nbank = 0
        self.last_w = {}
        self.readers = {}

    def barrier(self):
        engs = ["pe", "dve", "act", "pool", "sp"]
        idxs = set()
        for e in engs:
            last = [i for i, o in enumerate(self.ops) if o["eng"] == e]
            if last:
                idxs.add(last[-1])
            dm = [i for i, o in enumerate(self.ops) if o["eng"] == e and o["dma"]]
            idxs.update(dm[-NDMA_SEM:])
        for e in engs:
            self.ops.append(dict(eng=e, fn=lambda en: en.nop(), deps=set(idxs), dma=False, r=(), w=(), barrier=True))

    def op(self, eng, fn, r=(), w=(), dma=False):
        idx = len(self.ops)
        deps = set()
        for k in r:
            if k in self.last_w:
                deps.add(self.last_w[k])
        for k in w:
            if k in self.last_w:
                deps.add(self.last_w[k])
            for rd in self.readers.get(k, ()):
                deps.add(rd)
        for k in r:
            self.readers.setdefault(k, []).append(idx)
        for k in w:
            self.last_w[k] = idx
            self.readers[k] = []
        deps.discard(idx)
        self.ops.append(dict(eng=eng, fn=fn, deps=deps, dma=dma, r=tuple(r), w=tuple(w)))
        return idx

    def pe(self, fn, r=(), w=()):
        return self.op("pe", fn, r, w)

    def dve(self, fn, r=(), w=()):
        return self.op("dve", fn, r, w)

    def act(self, fn, r=(), w=()):
        return self.op("act", fn, r, w)

    def pool(self, fn, r=(), w=()):
        return self.op("pool", fn, r, w)

    def dma(self, eng, out, in_, r=(), w=(), **kw):
        return self.op(eng, lambda e: e.dma_start(out=out, in_=in_, **kw), r, w, dma=True)

    def finalize(self):
        nc = self.nc
        ops = self.ops
        engs = ["pe", "dve", "act", "pool", "sp"]
        needed = [False] * len(ops)
        for i, o in enumerate(ops):
            keep = set()
            for d in o["deps"]:
                od = ops[d]
                same = od["eng"] == o["eng"]
                if same and not od["dma"]:
                    if o["eng"] == "pe" or o.get("barrier"):
                        continue
                    if not (set(od["w"]) & set(o["r"])):
                        continue
                keep.add(d)
                needed[d] = True
            o["deps"] = keep
        sem_c = {e: self.stack.enter_context(nc.semaphore(f"c_{e}")) for e in engs}
        sem_d = {e: [self.stack.enter_context(nc.semaphore(f"d_{e}{j}")) for j in range(NDMA_SEM)]
                 for e in ("sp", "pool", "act")}
        cnt_c = {e: 0 for e in engs}
        cnt_d = {e: 0 for e in engs}
        for i, o in enumerate(ops):
            e = o["eng"]
            if o["dma"]:
                j = cnt_d[e]
                cnt_d[e] += 1
                o["sig"] = (sem_d[e][j % NDMA_SEM], 16 * (j // NDMA_SEM + 1), 16)
                o["dma_idx"] = j
            else:
                if needed[i]:
                    cnt_c[e] += 1
                    o["sig"] = (sem_c[e], cnt_c[e], 1)
                else:
                    o["sig"] = None
        dmas = {e: [o for o in ops if o["dma"] and o["eng"] == e] for e in ("sp", "pool", "act")}
        per_eng = {e: [o for o in ops if o["eng"] == e] for e in engs}

        def emit(e, eng):
            waited = {}

            def wait(sig):
                sem, val, _ = sig
                key = id(sem)
                if waited.get(key, 0) >= val:
                    return
                eng.wait_ge(sem, val)
                waited[key] = val

            dlist = dmas.get(e, [])
            for o in per_eng[e]:
                for d in sorted(o["deps"]):
                    wait(ops[d]["sig"])
                if o["dma"]:
                    j = o["dma_idx"]
                    if j >= NDMA_SEM:
                        wait(dlist[j - NDMA_SEM]["sig"])
                ins = o["fn"](eng)
                if o["sig"] is not None:
                    ins.then_inc(o["sig"][0], o["sig"][2])
            for o in dlist[-NDMA_SEM:]:
                wait(o["sig"])

        with nc.Block() as block:
            @block.tensor
            def _(eng):
                emit("pe", eng)

            @block.vector
            def _(eng):
                emit("dve", eng)

            @block.scalar
            def _(eng):
                emit("act", eng)

            @block.gpsimd
            def _(eng):
                emit("pool", eng)

            @block.sync
            def _(eng):
                emit("sp", eng)
        self.stack.close()
        return nc


def emit_proj(p, S, NF, NT, d, pfx="pj", fm_key=None, tm_key=None):
    KC = 16
    TB = 512
    NB = S // TB
    K = lambda *a: (pfx,) + a
    fm_key = fm_key or (lambda rt, tb: K("ofm", rt, tb))
    tm_key = tm_key or (lambda tb, j: K("otm", tb, j))
    wf = p.sb([128, KC, NF], BF16)
    wt = p.sb([128, KC, NT], BF16)
    for kc in range(KC):
        p.dma("pool", wf[:, kc, :], d["wfm"][kc * 128:(kc + 1) * 128, :], w=[K("wf")])
        p.dma("pool", wt[:, kc, :], d["wtm"][kc * 128:(kc + 1) * 128, :], w=[K("wt")])
    xb = [p.sb([128, KC, TB], BF16) for _ in range(2)]
    of = [p.sb([128, TB]) for _ in range(3)]
    ot = [p.sb([128, NT]) for _ in range(2)]
    pss = [p.ps([128, 512]) for _ in range(4)]
    cnt = {"ps": 0, "of": 0, "ot": 0, "ev": 0}
    rts = [(r0, min(128, NF - r0)) for r0 in range(0, NF, 128)]
    cgs = [(c0, min(512, NT - c0)) for c0 in range(0, NT, 512)]

    def evac(out, in_, r, w):
        if cnt["ev"] % 2 == 0:
            p.act(lambda e: e.copy(out=out, in_=in_), r=r, w=w)
        else:
            p.dve(lambda e: e.tensor_copy(out=out, in_=in_), r=r, w=w)
        cnt["ev"] += 1

    def blk(b):
        q = b % 2
        t0 = b * TB
        X = xb[q]
        p.dma("pool", X[:, :, :], d["xT"][:, t0:t0 + TB].rearrange("(kc p) t -> p kc t", p=128), r=[("xTdram", b)], w=[K("xb", q)])
        for ri, (r0, rn) in enumerate(rts):
            pi = cnt["ps"] % 4; cnt["ps"] += 1
            ps = pss[pi]
            for kc in range(KC):
                p.pe(lambda e, ps=ps, kc=kc, r0=r0, rn=rn: e.matmul(ps[:rn, :], lhsT=wf[:, kc, r0:r0 + rn], rhs=X[:, kc, :], start=(kc == 0), stop=(kc == KC - 1)),
                     r=[K("wf"), K("xb", q)], w=[K("ps", pi)])
            oi = cnt["of"] % 3; cnt["of"] += 1
            evac(of[oi][:rn, :], ps[:rn, :], [K("ps", pi)], [K("of", oi)])
            p.dma("sp", d["ofm"][r0:r0 + rn, t0:t0 + TB], of[oi][:rn, :], r=[K("of", oi)], w=[fm_key(ri, b)])
        for j in range(TB // 128):
            oi = cnt["ot"] % 2; cnt["ot"] += 1
            for (c0, cn) in cgs:
                pi = cnt["ps"] % 4; cnt["ps"] += 1
                ps = pss[pi]
                for kc in range(KC):
                    p.pe(lambda e, ps=ps, kc=kc, c0=c0, cn=cn, j=j: e.matmul(ps[:, :cn], lhsT=X[:, kc, j * 128:(j + 1) * 128], rhs=wt[:, kc, c0:c0 + cn],
                                                                         start=(kc == 0), stop=(kc == KC - 1)), r=[K("wt"), K("xb", q)], w=[K("ps", pi)])
                evac(ot[oi][:, c0:c0 + cn], ps[:, :cn], [K("ps", pi)], [K("ot", oi, c0)])
            p.dma("sp", d["otm"][t0 + j * 128:t0 + (j + 1) * 128, :], ot[oi][:, :], r=[K("ot", oi, c0) for (c0, cn) in cgs], w=[tm_key(b, j)])

    for b in range(NB):
        blk(b)

RW_GN_EPS = 64e-5
CV_NAMES = ["mu_r", "mu_k", "mu_v", "mu_xw", "mu_xa", "mu_xg0", "mu_xg1", "mu_xv",
            "w0", "a0", "v0", "k_k", "k_a", "r_k", "lnx_g", "lnx_b", "eps", "tiny", "fdeep"]
CVI = {n: i for i, n in enumerate(CV_NAMES)}


def emit_rwkv(p, S, d, TC=16, pfx="rw"):
    deep = True
    TB = 512
    NB = S // TB
    K = lambda *a: (pfx,) + a
    cv = p.sb([128, len(CV_NAMES)])
    p.dma("sp", cv[:, :], d["cv"], w=[K("cv")])
    ident = p.sb([128, 128])
    p.dma("sp", ident[:, :], d["ident"], w=[K("ident")])
    bones = p.sb([128, 128])
    p.dma("sp", bones[:, :], d["bones"], w=[K("bones")])
    w_up = p.sb([96, 128]); p.dma("sp", w_up[:, :], d["w_up"], w=[K("w_up")])
    a_up = p.sb([96, 128]); p.dma("sp", a_up[:, :], d["a_up"], w=[K("a_up")])
    g_up = p.sb([128, 2, 128]); p.dma("sp", g_up[:, :, :], d["g_up"].rearrange("(c p) n -> p c n", p=128), w=[K("g_up")])
    if deep:
        v_up = p.sb([64, 128]); p.dma("sp", v_up[:, :], d["v_up"], w=[K("v_up")])
    CONST = [K("cv"), K("ident"), K("bones"), K("w_up"), K("a_up"), K("g_up"), K("v_up")]

    def c(name, rows=128):
        i = CVI[name]
        return cv[:rows, i:i + 1]

    groups = [("r", 128, "pr", "mu_r"), ("k", 128, "pk", "mu_k"), ("v", 128, "pv", "mu_v"),
              ("xw", 96, "pxw", "mu_xw"), ("xa", 96, "pxa", "mu_xa"),
              ("xg0", 128, "pxg", "mu_xg0"), ("xg1", 128, "pxg", "mu_xg1")]
    if deep:
        groups.append(("xv", 64, "pxv", "mu_xv"))
    def mk(n, shape, dt=F32):
        return [p.sb(shape, dt) for _ in range(n)]
    pin = {g[0]: mk(2, [128, TB + 1]) for g in groups}
    sh = {g[0]: mk(1, [128, TB]) * 2 for g in groups}
    names2 = ["tmp", "tmp2", "txw", "dec", "a", "sxg0", "sxg1", "g", "v", "kk", "rn", "nk", "kp", "ka", "bv",
              "yT", "cen", "sq", "o"]
    if deep:
        names2 += ["vf", "vg"]
    DBL = ("v", "yT", "bv", "g")
    T = {n: (mk(2, [128, TB]) if n in DBL else mk(1, [128, TB]) * 2) for n in names2}
    stg = mk(2, [128, 4, 2, 5, 64])
    bc = mk(2, [128, TC, 5, 64])
    St = p.sb([128, 64])
    junk = p.sb([128, 64])
    nskk = p.sb([128, 1])
    p.dve(lambda e: e.memset(St[:, :], 0.0), w=[K("S")])
    NPS = 7
    pss = [p.ps([128, 512]) for _ in range(NPS)]
    psn = [0]

    def nps():
        i = psn[0] % NPS
        psn[0] += 1
        return pss[i], K("ps", i)

    def prep(b):
        q = b % 2
        t0 = b * TB
        kq = lambda *n: K(*n, q if (n[0] in ("v", "yT", "bv", "g", "stg") or n[0].startswith("pin")) else 0)
        for (gn, rows, src, mu) in groups:
            off = 128 if gn == "xg1" else 0
            sap = d[src]
            tile = pin[gn][q]
            if b == 0:
                p.dma("sp", tile[:rows, 1:TB + 1], sap[off:off + rows, t0:t0 + TB], w=[kq("pin_" + gn)])
                p.pool(lambda e, tile=tile, rows=rows: e.memset(tile[:rows, 0:1], 0.0), w=[kq("pin0_" + gn)])
            else:
                p.dma("sp", tile[:rows, 0:TB + 1], sap[off:off + rows, t0 - 1:t0 + TB], w=[kq("pin_" + gn), kq("pin0_" + gn)])
            tmp = T["tmp"][q]
            p.dve(lambda e, tile=tile, rows=rows, tmp=tmp: e.tensor_sub(out=tmp[:rows, :], in0=tile[:rows, 0:TB], in1=tile[:rows, 1:TB + 1]),
                  r=[kq("pin_" + gn), kq("pin0_" + gn)], w=[kq("tmp")])
            o = sh[gn][q]
            p.dve(lambda e, tile=tile, rows=rows, tmp=tmp, o=o, mu=mu: e.scalar_tensor_tensor(
                out=o[:rows, :], in0=tmp[:rows, :], scalar=c(mu, rows), in1=tile[:rows, 1:TB + 1], op0=ALU.mult, op1=ALU.add),
                r=[kq("tmp"), kq("pin_" + gn), K("cv")], w=[kq("sh_" + gn)])
        txw = T["txw"][q]
        p.act(lambda e: e.activation(out=txw[:96, :], in_=sh["xw"][q][:96, :], func=AF.Tanh), r=[kq("sh_xw")], w=[kq("txw")])
        ps, pk_ = nps()
        p.pe(lambda e, ps=ps: e.matmul(ps[:, :], lhsT=w_up[:, :], rhs=txw[:96, :], start=True, stop=True), r=[kq("txw"), K("w_up")], w=[pk_])
        dec = T["dec"][q]
        p.act(lambda e, ps=ps: e.activation(out=dec[:, :], in_=ps[:, :], func=AF.Sigmoid, bias=c("w0"), scale=1.0), r=[pk_, K("cv")], w=[kq("dec")])
        p.act(lambda e: e.activation(out=dec[:, :], in_=dec[:, :], func=AF.Exp, scale=-float(np.exp(-0.5))), r=[kq("dec")], w=[kq("dec")])
        ps, pk_ = nps()
        p.pe(lambda e, ps=ps: e.matmul(ps[:, :], lhsT=a_up[:, :], rhs=sh["xa"][q][:96, :], start=True, stop=True), r=[kq("sh_xa"), K("a_up")], w=[pk_])
        a = T["a"][q]
        p.act(lambda e, ps=ps: e.activation(out=a[:, :], in_=ps[:, :], func=AF.Sigmoid, bias=c("a0"), scale=1.0), r=[pk_, K("cv")], w=[kq("a")])
        for j, gn in enumerate(("xg0", "xg1")):
            sx = T["s" + gn][q]
            p.act(lambda e, sx=sx, gn=gn: e.activation(out=sx[:, :], in_=sh[gn][q][:, :], func=AF.Sigmoid), r=[kq("sh_" + gn)], w=[kq("s" + gn)])
        ps, pk_ = nps()
        for j, gn in enumerate(("xg0", "xg1")):
            p.pe(lambda e, ps=ps, j=j, gn=gn: e.matmul(ps[:, :], lhsT=g_up[:, j, :], rhs=T["s" + gn][q][:, :], start=(j == 0), stop=(j == 1)),
                 r=[kq("s" + gn), K("g_up")], w=[pk_])
        g = T["g"][q]
        p.act(lambda e, ps=ps: e.copy(out=g[:, :], in_=ps[:, :]), r=[pk_], w=[kq("g")])
        v = T["v"][q]
        vf = T["vf"][q]
        p.dma("sp", vf[:, :], d["vf_in"][:, t0:t0 + TB], w=[kq("vf")])
        ps, pk_ = nps()
        p.pe(lambda e, ps=ps: e.matmul(ps[:, :], lhsT=v_up[:, :], rhs=sh["xv"][q][:64, :], start=True, stop=True), r=[kq("sh_xv"), K("v_up")], w=[pk_])
        vg = T["vg"][q]
        p.act(lambda e, ps=ps: e.activation(out=vg[:, :], in_=ps[:, :], func=AF.Sigmoid, bias=c("v0"), scale=1.0), r=[pk_, K("cv")], w=[kq("vg")])
        tmp = T["tmp"][q]
        p.dve(lambda e: e.tensor_sub(out=tmp[:, :], in0=vf[:, :], in1=sh["v"][q][:, :]), r=[kq("vf"), kq("sh_v")], w=[kq("tmp")])
        p.dve(lambda e: e.tensor_mul(out=tmp[:, :], in0=tmp[:, :], in1=vg[:, :]), r=[kq("tmp"), kq("vg")], w=[kq("tmp")])
        p.dve(lambda e: e.scalar_tensor_tensor(out=v[:, :], in0=tmp[:, :], scalar=c("fdeep"), in1=sh["v"][q][:, :], op0=ALU.mult, op1=ALU.add),
              r=[kq("tmp"), kq("sh_v"), K("cv")], w=[kq("v")])
        p.dma("sp", d["vf_out"][:, t0:t0 + TB], v[:, :], r=[kq("v")], w=[K("vfdram", b)])
        kk = T["kk"][q]
        p.dve(lambda e: e.tensor_scalar(out=kk[:, :], in0=sh["k"][q][:, :], scalar1=c("k_k"), scalar2=None, op0=ALU.mult), r=[kq("sh_k"), K("cv")], w=[kq("kk")])
        tmp2 = T["tmp2"][q]
        p.dve(lambda e: e.tensor_mul(out=tmp2[:, :], in0=kk[:, :], in1=kk[:, :]), r=[kq("kk")], w=[kq("tmp2")])
        ps, pk_ = nps()
        p.pe(lambda e, ps=ps: e.matmul(ps[:, :], lhsT=bones[:, :], rhs=tmp2[:, :], start=True, stop=True), r=[kq("tmp2"), K("bones")], w=[pk_])
        rn = T["rn"][q]
        p.act(lambda e, ps=ps: e.activation(out=rn[:, :], in_=ps[:, :], func=AF.Sqrt), r=[pk_], w=[kq("rn")])
        p.dve(lambda e: e.tensor_scalar(out=rn[:, :], in0=rn[:, :], scalar1=1e-12, scalar2=None, op0=ALU.max), r=[kq("rn")], w=[kq("rn")])
        p.dve(lambda e: e.reciprocal(out=rn[:, :], in_=rn[:, :]), r=[kq("rn")], w=[kq("rn")])
        nk = T["nk"][q]
        p.dve(lambda e: e.scalar_tensor_tensor(out=nk[:, :], in0=kk[:, :], scalar=-1.0, in1=rn[:, :], op0=ALU.mult, op1=ALU.mult), r=[kq("kk"), kq("rn")], w=[kq("nk")])
        kp = T["kp"][q]
        p.dve(lambda e: e.tensor_scalar(out=tmp2[:, :], in0=a[:, :], scalar1=-1.0, scalar2=None, op0=ALU.add), r=[kq("a")], w=[kq("tmp2")])
        p.dve(lambda e: e.scalar_tensor_tensor(out=tmp2[:, :], in0=tmp2[:, :], scalar=c("k_a"), in1=sh["k"][q][:, :], op0=ALU.mult, op1=ALU.mult),
              r=[kq("tmp2"), kq("sh_k"), K("cv")], w=[kq("tmp2")])
        p.dve(lambda e: e.tensor_add(out=kp[:, :], in0=tmp2[:, :], in1=sh["k"][q][:, :]), r=[kq("tmp2"), kq("sh_k")], w=[kq("kp")])
        ka = T["ka"][q]
        p.dve(lambda e: e.scalar_tensor_tensor(out=ka[:, :], in0=nk[:, :], scalar=-1.0, in1=a[:, :], op0=ALU.mult, op1=ALU.mult), r=[kq("nk"), kq("a")], w=[kq("ka")])
        p.dve(lambda e: e.scalar_tensor_tensor(out=tmp2[:, :], in0=sh["r"][q][:, :], scalar=c("r_k"), in1=kp[:, :], op0=ALU.mult, op1=ALU.mult),
              r=[kq("sh_r"), kq("kp"), K("cv")], w=[kq("tmp2")])
        ps, pk_ = nps()
        p.pe(lambda e, ps=ps: e.matmul(ps[:, :], lhsT=bones[:, :], rhs=tmp2[:, :], start=True, stop=True), r=[kq("tmp2"), K("bones")], w=[pk_])
        bv = T["bv"][q]
        p.dve(lambda e, ps=ps: e.tensor_mul(out=bv[:, :], in0=ps[:, :], in1=v[:, :]), r=[pk_, kq("v")], w=[kq("bv")])
        ops5 = [(nk, "nk"), (dec, "dec"), (ka, "ka"), (kp, "kp"), (sh["r"][q], "sh_r")]
        sg = stg[q]
        for j in range(4):
            for qi, (X, xn) in enumerate(ops5):
                ps, pk_ = nps()
                p.pe(lambda e, ps=ps, X=X, j=j: e.transpose(out=ps[:, 0:128], in_=X[:, j * 128:(j + 1) * 128], identity=ident[:, :]),
                     r=[kq(xn), K("ident")], w=[pk_])
                p.act(lambda e, ps=ps, j=j, qi=qi: e.copy(out=sg[:, j, :, qi, :], in_=ps[:, 0:128].rearrange("p (h x) -> p h x", h=2)),
                      r=[pk_], w=[kq("stg", j, qi)])
            for h in range(2):
                p.dma("sp", d["scr"][h, t0 + j * 128:t0 + (j + 1) * 128, :, :], sg[:, j, h, :, :],
                      r=[kq("stg", j, qi) for qi in range(5)], w=[K("scr", b, j, h)])

    def scan(b):
        q = b % 2
        t0 = b * TB
        kq = lambda *n: K(*n, q if (n[0] in ("v", "yT", "bv", "g", "stg") or n[0].startswith("pin")) else 0)
        v = T["v"][q]
        yT = T["yT"][q]
        for ci in range(TB // TC):
            tc0 = t0 + ci * TC
            bq = (b * (TB // TC) + ci) % 2
            B = bc[bq]
            j = (ci * TC) // 128
            for h in range(2):
                src = d["scr"][h, tc0:tc0 + TC, :, :].rearrange("t q x -> (t q x)").partition_broadcast(64)
                p.dma("sp", B[h * 64:(h + 1) * 64, :, :, :].rearrange("p t q x -> p (t q x)"), src,
                      r=[K("scr", b, j, h)], w=[K("bc", bq, h)])
            rk = [K("bc", bq, 0), K("bc", bq, 1)]
            for tt in range(TC):
                t = ci * TC + tt
                p.dve(lambda e, B=B, tt=tt: e.scalar_tensor_tensor(out=junk[:, :], in0=St[:, :], scalar=1.0, in1=B[:, tt, 0, :], op0=ALU.mult, op1=ALU.mult,
                                                                   accum_out=nskk[:, 0:1]), r=rk + [K("S")], w=[K("junk"), K("nskk")])
                p.dve(lambda e, B=B, tt=tt: e.tensor_mul(out=St[:, :], in0=St[:, :], in1=B[:, tt, 1, :]), r=rk + [K("S")], w=[K("S")])
                p.dve(lambda e, B=B, tt=tt: e.scalar_tensor_tensor(out=St[:, :], in0=B[:, tt, 2, :], scalar=nskk[:, 0:1], in1=St[:, :], op0=ALU.mult, op1=ALU.add),
                      r=rk + [K("S"), K("nskk")], w=[K("S")])
                p.dve(lambda e, B=B, tt=tt, t=t: e.scalar_tensor_tensor(out=St[:, :], in0=B[:, tt, 3, :], scalar=v[:, t:t + 1], in1=St[:, :], op0=ALU.mult, op1=ALU.add),
                      r=rk + [K("S"), kq("v")], w=[K("S")])
                p.dve(lambda e, B=B, tt=tt, t=t: e.scalar_tensor_tensor(out=junk[:, :], in0=St[:, :], scalar=1.0, in1=B[:, tt, 4, :], op0=ALU.mult, op1=ALU.mult,
                                                                        accum_out=yT[:, t:t + 1]), r=rk + [K("S")], w=[K("junk"), kq("yT")])

    def post(b):
        q = b % 2
        t0 = b * TB
        kq = lambda *n: K(*n, q if (n[0] in ("v", "yT", "bv", "g", "stg") or n[0].startswith("pin")) else 0)
        yT = T["yT"][q]; cen = T["cen"][q]; sq = T["sq"][q]; o = T["o"][q]
        ps, pk_ = nps()
        p.pe(lambda e, ps=ps: e.matmul(ps[:, :], lhsT=bones[:, :], rhs=yT[:, :], start=True, stop=True), r=[kq("yT"), K("bones")], w=[pk_])
        p.dve(lambda e, ps=ps: e.scalar_tensor_tensor(out=cen[:, :], in0=ps[:, :], scalar=-1.0 / 64, in1=yT[:, :], op0=ALU.mult, op1=ALU.add), r=[pk_, kq("yT")], w=[kq("cen")])
        p.dve(lambda e: e.tensor_mul(out=sq[:, :], in0=cen[:, :], in1=cen[:, :]), r=[kq("cen")], w=[kq("sq")])
        ps, pk_ = nps()
        p.pe(lambda e, ps=ps: e.matmul(ps[:, :], lhsT=bones[:, :], rhs=sq[:, :], start=True, stop=True), r=[kq("sq"), K("bones")], w=[pk_])
        p.act(lambda e, ps=ps: e.activation(out=sq[:, :], in_=ps[:, :], func=AF.Sqrt, bias=c("eps"), scale=1.0 / 64), r=[pk_, K("cv")], w=[kq("sq")])
        p.dve(lambda e: e.reciprocal(out=sq[:, :], in_=sq[:, :]), r=[kq("sq")], w=[kq("sq")])
        p.dve(lambda e: e.tensor_mul(out=cen[:, :], in0=cen[:, :], in1=sq[:, :]), r=[kq("cen"), kq("sq")], w=[kq("cen")])
        p.dve(lambda e: e.tensor_scalar(out=cen[:, :], in0=cen[:, :], scalar1=c("lnx_g"), scalar2=c("lnx_b"), op0=ALU.mult, op1=ALU.add), r=[kq("cen"), K("cv")], w=[kq("cen")])
        p.dve(lambda e: e.tensor_add(out=cen[:, :], in0=cen[:, :], in1=T["bv"][q][:, :]), r=[kq("cen"), kq("bv")], w=[kq("cen")])
        p.dve(lambda e: e.tensor_mul(out=o[:, :], in0=cen[:, :], in1=T["g"][q][:, :]), r=[kq("cen"), kq("g")], w=[kq("o")])
        p.dma("sp", d["y"][:, t0:t0 + TB], o[:, :], r=[kq("o")], w=[K("ydram", b)])

    prep(0)
    for b in range(NB):
        if b + 1 < NB:
            prep(b + 1)
        scan(b)
        post(b)


def rwkv_host_inputs(c, l, inp):
    deep = l > 0
    H0 = 2 * c
    cols = slice(H0 * 64, H0 * 64 + 128)
    mu = inp["rw_mu_first"] if not deep else inp["rw_mu_deep"][l - 1]
    offs = np.cumsum([0, 1024, 1024, 1024, 96, 96, 256] + ([64] if deep else []))
    cvm = np.zeros((128, len(CV_NAMES)), np.float32)
    def put(name, vec):
        cvm[:len(vec), CVI[name]] = vec
    put("mu_r", mu[offs[0]:offs[1]][cols]); put("mu_k", mu[offs[1]:offs[2]][cols]); put("mu_v", mu[offs[2]:offs[3]][cols])
    put("mu_xw", mu[offs[3]:offs[4]]); put("mu_xa", mu[offs[4]:offs[5]])
    put("mu_xg0", mu[offs[5]:offs[5] + 128]); put("mu_xg1", mu[offs[5] + 128:offs[6]])
    if deep:
        put("mu_xv", mu[offs[6]:offs[7]])
        put("v0", inp["rw_v0"][l - 1][cols])
        cvm[:, CVI["fdeep"]] = 1.0
    put("w0", inp["rw_w0"][l][cols]); put("a0", inp["rw_a0"][l][cols])
    put("k_k", inp["rw_k_k"][l][cols]); put("k_a", inp["rw_k_a"][l][cols])
    put("r_k", inp["rw_r_k"][l].reshape(-1)[cols]); put("lnx_g", inp["rw_lnx_g"][l][cols]); put("lnx_b", inp["rw_lnx_b"][l][cols])
    cvm[:, CVI["eps"]] = RW_GN_EPS
    cvm[:, CVI["tiny"]] = 1e-12
    bones = np.zeros((128, 128), np.float32)
    bones[:64, :64] = 1; bones[64:, 64:] = 1
    m = {"rw_cv": cvm, "rw_bones": bones,
         "rw_w_up": np.ascontiguousarray(inp["rw_w_up"][l][:, cols]), "rw_a_up": np.ascontiguousarray(inp["rw_a_up"][l][:, cols]),
         "rw_g_up": np.ascontiguousarray(inp["rw_g_up"][l][:, cols])}
    m["rw_v_up"] = np.ascontiguousarray(inp["rw_v_up"][l - 1][:, cols]) if deep else np.zeros((64, 128), np.float32)
    return m

TINY = 1e-30


def emit_nsa(p, S, d, pfx="nsa"):
    NQ = S // 128
    NSEL = S // 64
    NC = S // 16
    NCC = (NC + 127) // 128
    NSC = (NSEL + 127) // 128
    K = lambda *a: (pfx,) + a

    def const(name, shape, dt, src, eng="pool"):
        t = p.sb(shape, dt)
        p.dma(eng, t[tuple(slice(None) for _ in shape)], src, w=[K(name)])
        return t
    ident = const("ident", [128, 128], F32, d["ident"], "sp")
    ptab = const("ptab", [128, 512], F32, d["ptab"], "sp")
    maskc = const("maskc", [128, 16, 128], BF16, d["maskc"])
    maskp = const("maskp", [128, 128], BF16, d["maskp"])
    tri = const("tri", [128, 128], BF16, d["tri"])
    triu = const("triu", [128, 128], BF16, d["triu"])
    ex = const("ex", [128, 64, 128], BF16, d["ex"])
    CK = [K(n) for n in ("ident", "ptab", "maskc", "maskp", "tri", "triu", "ex")]
    ksb = p.sb([64, S], BF16)
    big2 = p.sb([64, S], BF16)
    vsx = p.sb([128, NQ, 65], BF16)
    vwx = p.sb([128, NQ, 65], BF16)
    vcx = p.sb([128, NCC, 65], BF16)
    ovl = p.sb([128, NCC, NSEL], BF16)
    kcmpT = p.sb([64, NCC * 128], BF16)
    p.dma("pool", ksb[:, :], d["ksT"], w=[K("ksb")])
    for (t, src, nm) in ((vsx, "vs", "vsx"), (vwx, "vw", "vwx")):
        for c0 in range(0, NQ, 16):
            p.dma("pool", t[:, c0:c0 + 16, 0:64], d[src][c0 * 128:(c0 + 16) * 128, :].rearrange("(c p) x -> p c x", p=128), w=[K(nm, 0)])
        p.pool(lambda e, t=t: e.memset(t[:, :, 64:65], 1.0), w=[K(nm, 1)])
    if NC >= 128:
        p.dma("pool", ovl[:, :, :], d["ovl"].rearrange("(c p) n -> p c n", p=128), w=[K("ovl")])
    else:
        p.pool(lambda e: e.memset(ovl[:, :, :], 0.0), w=[K("ovl")])
        p.dma("pool", ovl[:NC, 0, :], d["ovl"], w=[K("ovl")])
    p.pool(lambda e: e.memset(vcx[:, :, 64:65], 1.0), w=[K("vcx", 1)])

    NPS = 8
    pss = [p.ps([128, 512]) for _ in range(NPS)]
    PK = lambda i: K("ps", i)
    B_OC, B_IMP0, B_IMP1, B_ST0, B_ST1, B_MK, B_SEL, B_WIN = range(8)

    w1 = p.sb([64, 32, 128], BF16)
    peT = p.sb([64, 32], BF16)
    b1 = p.sb([128, 1])
    w2 = p.sb([128, 64], BF16)
    bias = p.sb([128, 1])
    hx = p.sb([128, 512]); hu = p.sb([128, 512])
    hidT = p.sb([128, NCC * 128], BF16)
    p.dve(lambda e: e.memset(hidT[:, :], 0.0), w=[K("hidT")])
    p.dve(lambda e: e.memset(kcmpT[:, :], 0.0), w=[K("kcmpT")])
    nvalid = NC - 1
    for kv in range(2):
        p.dma("pool", big2[:, :], d["kcT"] if kv == 0 else d["vcT"], w=[K("big2")])
        p.dma("pool", w1[:, :, :], d["w1"][kv], w=[K("w1")])
        p.dma("pool", peT[:, :], d["peT"][kv], w=[K("peT")])
        p.dma("sp", b1[:, :], d["b1"][kv], w=[K("b1")])
        p.dma("pool", w2[:, :], d["w2"][kv], w=[K("w2")])
        for l in range(32):
            p.pe(lambda e, l=l: e.matmul(pss[B_MK][:, 0:1], lhsT=w1[:, l, :], rhs=peT[:, l:l + 1], start=(l == 0), stop=(l == 31)),
                 r=[K("w1"), K("peT")], w=[PK(B_MK)])
        p.dve(lambda e: e.tensor_add(out=bias[:, :], in0=pss[B_MK][:, 0:1], in1=b1[:, :]), r=[PK(B_MK), K("b1")], w=[K("bias")])
        b3 = big2[:, :].rearrange("p (c x) -> p c x", x=16)
        for c0 in range(0, nvalid, 512):
            n = min(512, nvalid - c0)
            ps = pss[B_ST0]
            for l in range(32):
                rhs = b3[:, c0:c0 + n, l] if l < 16 else b3[:, c0 + 1:c0 + 1 + n, l - 16]
                p.pe(lambda e, l=l, rhs=rhs, n=n, ps=ps: e.matmul(ps[:, 0:n], lhsT=w1[:, l, :], rhs=rhs, start=(l == 0), stop=(l == 31)),
                     r=[K("w1"), K("big2")], w=[PK(B_ST0)])
            p.dve(lambda e, n=n, ps=ps: e.tensor_scalar(out=hx[:, 0:n], in0=ps[:, 0:n], scalar1=bias[:, 0:1], scalar2=None, op0=ALU.add), r=[PK(B_ST0), K("bias")], w=[K("hx")])
            p.dve(lambda e, n=n: e.tensor_mul(out=hu[:, 0:n], in0=hx[:, 0:n], in1=hx[:, 0:n]), r=[K("hx")], w=[K("hu")])
            p.dve(lambda e, n=n: e.tensor_scalar(out=hu[:, 0:n], in0=hu[:, 0:n], scalar1=0.044715, scalar2=1.0, op0=ALU.mult, op1=ALU.add), r=[K("hu")], w=[K("hu")])
            p.dve(lambda e, n=n: e.tensor_mul(out=hu[:, 0:n], in0=hu[:, 0:n], in1=hx[:, 0:n]), r=[K("hu"), K("hx")], w=[K("hu")])
            p.act(lambda e, n=n: e.activation(out=hu[:, 0:n], in_=hu[:, 0:n], func=AF.Tanh, scale=0.7978845608028654), r=[K("hu")], w=[K("hu")])
            p.dve(lambda e, n=n: e.tensor_scalar(out=hu[:, 0:n], in0=hu[:, 0:n], scalar1=1.0, scalar2=0.5, op0=ALU.add, op1=ALU.mult), r=[K("hu")], w=[K("hu")])
            p.dve(lambda e, n=n, c0=c0: e.tensor_mul(out=hidT[:, c0:c0 + n], in0=hu[:, 0:n], in1=hx[:, 0:n]), r=[K("hu"), K("hx")], w=[K("hidT")])
            if kv == 0:
                p.pe(lambda e, n=n, c0=c0: e.matmul(pss[B_ST1][0:64, 0:n], lhsT=w2[:, :], rhs=hidT[:, c0:c0 + n], start=True, stop=True), r=[K("w2"), K("hidT")], w=[PK(B_ST1)])
                p.act(lambda e, n=n, c0=c0: e.copy(out=kcmpT[:, c0:c0 + n], in_=pss[B_ST1][0:64, 0:n]), r=[PK(B_ST1)], w=[K("kcmpT")])
        if kv == 1:
            for j in range(NCC):
                p.pe(lambda e, j=j: e.matmul(pss[B_ST1][:, 0:64], lhsT=hidT[:, j * 128:(j + 1) * 128], rhs=w2[:, :], start=True, stop=True), r=[K("w2"), K("hidT")], w=[PK(B_ST1)])
                p.act(lambda e, j=j: e.copy(out=vcx[:, j, 0:64], in_=pss[B_ST1][:, 0:64]), r=[PK(B_ST1)], w=[K("vcx", 0)])
    p.dma("pool", big2[:, :], d["kwT"], w=[K("big2")])
    kwb = big2

    qf = [p.sb([64, 4, 128]) for _ in range(2)]
    qb = [p.sb([64, 4, 128], BF16) for _ in range(2)]
    gt = [p.sb([128, 6]) for _ in range(2)]
    E = [p.sb([128, 512], BF16) for _ in range(4)]
    en = [0]
    mk = [p.sb([128, 128], BF16) for _ in range(4)]
    mn = [0]
    zz = p.sb([128, 16])
    imp = p.sb([128, NSEL]); sc2 = p.sb([128, NSEL]); sel = p.sb([128, NSEL]); selb = p.sb([128, NSEL])
    m8 = p.sb([128, 16])
    selT = p.sb([128, NSC, 128], BF16)
    p.dve(lambda e: e.memset(selT[:, :, :], 0.0), w=[K("selT")])
    outq = p.sb([128, 2, 64])
    yo = [p.sb([128, 512]) for _ in range(2)]

    def qblock(qi):
        q2 = qi % 2
        kq = lambda *n: K(*n, q2)
        t0 = qi * 128
        p.dma("sp", qf[q2][:, :, :], d["qT"][:, :, t0:t0 + 128].rearrange("h x t -> x h t"), w=[kq("qf")])
        p.act(lambda e: e.mul(out=qb[q2][:, :, :], in_=qf[q2][:, :, :], mul=0.125), r=[kq("qf")], w=[kq("qb")])
        p.dma("sp", gt[q2][:, :], d["gate"][t0:t0 + 128, :], w=[kq("gt")])
        p.act(lambda e: e.activation(out=gt[q2][:, :], in_=gt[q2][:, :], func=AF.Sigmoid), r=[kq("gt")], w=[kq("gt")])
        qall = qb[q2][:, :, :].rearrange("x h t -> x (h t)")
        ncc = (8 * qi + 6) // 128 + 1
        OC = pss[B_OC][:, 0:260].rearrange("p (h x) -> p h x", h=4)
        def cfront(cc):
            sb_ = B_ST0 if cc % 2 == 0 else B_ST1
            p.pe(lambda e: e.matmul(pss[sb_][:, :], lhsT=kcmpT[:, cc * 128:(cc + 1) * 128], rhs=qall, start=True, stop=True),
                 r=[K("kcmpT"), kq("qb")], w=[PK(sb_)])
            ei = en[0] % 4; en[0] += 1
            Et = E[ei]
            p.act(lambda e: e.activation(out=Et[:, :], in_=pss[sb_][:, :], func=AF.Exp), r=[PK(sb_)], w=[K("E", ei)])
            if cc == ncc - 1:
                pat = qi % 16
                p.dve(lambda e: e.tensor_mul(out=Et[:, :].rearrange("p (h t) -> p h t", h=4), in0=Et[:, :].rearrange("p (h t) -> p h t", h=4),
                                             in1=maskc[:, pat, :].unsqueeze(1).to_broadcast([128, 4, 128])), r=[K("E", ei), K("maskc")], w=[K("E", ei)])
            if cc == ncc - 2 and qi % 16 == 0:
                p.dve(lambda e: e.tensor_mul(out=Et[:, :].rearrange("p (h t) -> p h t", h=4), in0=Et[:, :].rearrange("p (h t) -> p h t", h=4),
                                             in1=maskp[:, :].unsqueeze(1).to_broadcast([128, 4, 128])), r=[K("E", ei), K("maskp")], w=[K("E", ei)])
            return Et, ei

        def cback(cc, Et, ei):
            for h in range(4):
                p.pe(lambda e, h=h: e.matmul(OC[:, h, :], lhsT=Et[:, h * 128:(h + 1) * 128], rhs=vcx[:, cc, :], start=(cc == 0 and h == 0), stop=(cc == ncc - 1), skip_group_check=True),
                     r=[K("E", ei), K("vcx", 0), K("vcx", 1)], w=[PK(B_OC)])
                bi = B_IMP0 if h < 2 else B_IMP1
                hh = h % 2
                p.pe(lambda e, h=h, bi=bi, hh=hh: e.matmul(pss[bi][:, hh * 256:hh * 256 + NSEL], lhsT=Et[:, h * 128:(h + 1) * 128], rhs=ovl[:, cc, :],
                                                        start=(cc == 0 and hh == 0), stop=(cc == ncc - 1), skip_group_check=True),
                     r=[K("E", ei), K("ovl")], w=[PK(bi)])

        cur = cfront(0)
        for cc in range(ncc):
            nxt = cfront(cc + 1) if cc + 1 < ncc else None
            cback(cc, *cur)
            cur = nxt
        p.dve(lambda e: e.tensor_scalar(out=zz[:, 0:4], in0=OC[:, :, 64], scalar1=TINY, scalar2=None, op0=ALU.max), r=[PK(B_OC)], w=[K("zz", 0)])
        p.dve(lambda e: e.reciprocal(out=zz[:, 0:4], in_=zz[:, 0:4]), r=[K("zz", 0)], w=[K("zz", 0)])
        for h in range(4):
            bi = B_IMP0 if h < 2 else B_IMP1
            hh = h % 2
            if h == 0:
                p.dve(lambda e, bi=bi, hh=hh: e.tensor_scalar(out=imp[:, :], in0=pss[bi][:, hh * 256:hh * 256 + NSEL], scalar1=zz[:, 0:1], scalar2=None, op0=ALU.mult),
                      r=[PK(bi), K("zz", 0)], w=[K("imp")])
            else:
                p.dve(lambda e, bi=bi, hh=hh, h=h: e.scalar_tensor_tensor(out=imp[:, :], in0=pss[bi][:, hh * 256:hh * 256 + NSEL], scalar=zz[:, h:h + 1], in1=imp[:, :],
                                                                         op0=ALU.mult, op1=ALU.add), r=[PK(bi), K("zz", 0), K("imp")], w=[K("imp")])
        for h in range(2):
            p.dve(lambda e, h=h: e.tensor_mul(out=zz[:, 4 + h:5 + h], in0=zz[:, h:h + 1], in1=gt[q2][:, 3 * h:3 * h + 1]), r=[K("zz", 0), kq("gt")], w=[K("zz", 1, h)])
            p.dve(lambda e, h=h: e.tensor_scalar(out=outq[:, h, :], in0=OC[:, h, 0:64], scalar1=zz[:, 4 + h:5 + h], scalar2=None, op0=ALU.mult),
                  r=[PK(B_OC), K("zz", 1, h)], w=[K("outq", h)])
        off = 256 - 2 * qi
        p.dve(lambda e: e.tensor_add(out=imp[:, :], in0=imp[:, :], in1=ptab[:, off:off + NSEL]), r=[K("imp"), K("ptab")], w=[K("imp")])
        if qi > 0:
            p.dve(lambda e: e.tensor_scalar(out=imp[:, 0:1], in0=imp[:, 0:1], scalar1=1e4, scalar2=None, op0=ALU.add), r=[K("imp")], w=[K("imp")])
        p.dve(lambda e: e.max(out=m8[:, 0:8], in_=imp[:, :]), r=[K("imp")], w=[K("m8", 0)])
        p.dve(lambda e: e.match_replace(out=sc2[:, :], in_to_replace=m8[:, 0:8], in_values=imp[:, :], imm_value=-1e38), r=[K("imp"), K("m8", 0)], w=[K("sc2")])
        p.dve(lambda e: e.max(out=m8[:, 8:16], in_=sc2[:, :]), r=[K("sc2")], w=[K("m8", 1)])
        p.dve(lambda e: e.tensor_scalar(out=sel[:, :], in0=imp[:, :], scalar1=m8[:, 15:16], scalar2=None, op0=ALU.is_ge), r=[K("imp"), K("m8", 1)], w=[K("sel")])
        p.dve(lambda e: e.tensor_scalar(out=sc2[:, :], in0=imp[:, :], scalar1=-5e29, scalar2=None, op0=ALU.is_gt), r=[K("imp"), K("sc2")], w=[K("sc2")])
        p.dve(lambda e: e.tensor_mul(out=selb[:, :], in0=sel[:, :], in1=sc2[:, :]), r=[K("sel"), K("sc2")], w=[K("selb")])
        if "dbg_sel" in d:
            p.dma("sp", d["dbg_sel"][t0:t0 + 128, :], selb[:, :], r=[K("selb")], w=[K("dbgsel", qi)])
        for j in range(NSC):
            w_ = min(128, NSEL - j * 128)
            p.pe(lambda e, j=j, w_=w_: e.transpose(out=pss[B_MK][:w_, 0:128], in_=selb[:, j * 128:j * 128 + w_], identity=ident[:, :]), r=[K("selb"), K("ident")], w=[PK(B_MK)])
            p.act(lambda e, j=j, w_=w_: e.copy(out=selT[:w_, j, :], in_=pss[B_MK][:w_, 0:128]), r=[PK(B_MK)], w=[K("selT")])
        q01 = qb[q2][:, 0:2, :].rearrange("x h t -> x (h t)")
        ACS = pss[B_SEL][:, 0:130].rearrange("p (h x) -> p h x", h=2)
        ACW = pss[B_WIN][:, 0:130].rearrange("p (h x) -> p h x", h=2)
        sti = [0]

        def afront(kc, kbuf, selmask, diag):
            mi = None
            if selmask:
                p.pe(lambda e: e.matmul(pss[B_MK][:, 0:128], lhsT=ex[:, kc % 64, :], rhs=selT[:, kc // 64, :], start=True, stop=True), r=[K("ex"), K("selT")], w=[PK(B_MK)])
                mi = mn[0] % 4; mn[0] += 1
                if diag:
                    p.dve(lambda e: e.tensor_mul(out=mk[mi][:, :], in0=pss[B_MK][:, 0:128], in1=tri[:, :]), r=[PK(B_MK), K("tri")], w=[K("mk", mi)])
                else:
                    p.act(lambda e: e.copy(out=mk[mi][:, :], in_=pss[B_MK][:, 0:128]), r=[PK(B_MK)], w=[K("mk", mi)])
            sb_ = B_ST0 if sti[0] % 2 == 0 else B_ST1
            sti[0] += 1
            p.pe(lambda e: e.matmul(pss[sb_][:, 0:256], lhsT=kbuf[:, kc * 128:(kc + 1) * 128], rhs=q01, start=True, stop=True), r=[K("ksb"), K("big2"), kq("qb")], w=[PK(sb_)])
            ei = en[0] % 4; en[0] += 1
            Et = E[ei]
            p.act(lambda e: e.activation(out=Et[:, 0:256], in_=pss[sb_][:, 0:256], func=AF.Exp), r=[PK(sb_)], w=[K("E", ei)])
            return Et, ei, mi

        def aback(kc, st, vbuf, vkeys, ACC, bank, first, last, mask_ap, mkey):
            Et, ei, mi = st
            if mi is not None:
                mask_ap, mkey = mk[mi][:, :], [K("mk", mi)]
            if mask_ap is not None:
                p.dve(lambda e: e.tensor_mul(out=Et[:, 0:256].rearrange("p (h t) -> p h t", h=2), in0=Et[:, 0:256].rearrange("p (h t) -> p h t", h=2),
                                             in1=mask_ap.unsqueeze(1).to_broadcast([128, 2, 128])), r=[K("E", ei)] + mkey, w=[K("E", ei)])
            for h in range(2):
                p.pe(lambda e, h=h: e.matmul(ACC[:, h, :], lhsT=Et[:, h * 128:(h + 1) * 128], rhs=vbuf[:, kc, :], start=(first and h == 0), stop=last, skip_group_check=True),
                     r=[K("E", ei)] + vkeys, w=[PK(bank)])

        kcs = [kc for kc in range(qi - 4, qi + 1) if kc >= 0]
        jobs = []
        for kc in kcs:
            if kc == qi:
                m_ap, mkk = tri[:, :], [K("tri")]
            elif kc == qi - 4:
                m_ap, mkk = triu[:, :], [K("triu")]
            else:
                m_ap, mkk = None, []
            jobs.append(dict(kc=kc, kbuf=kwb, selmask=False, diag=False, vbuf=vwx, vkeys=[K("vwx", 0), K("vwx", 1)], ACC=ACW, bank=B_WIN, first=(kc == kcs[0]), last=(kc == kcs[-1]), m_ap=m_ap, mkk=mkk))
        for kc in range(qi + 1):
            jobs.append(dict(kc=kc, kbuf=ksb, selmask=True, diag=(kc == qi), vbuf=vsx, vkeys=[K("vsx", 0), K("vsx", 1)], ACC=ACS, bank=B_SEL, first=(kc == 0), last=(kc == qi), m_ap=None, mkk=[]))
        LA = 2
        sts = {}
        for i in range(min(LA, len(jobs))):
            j = jobs[i]
            sts[i] = afront(j["kc"], j["kbuf"], j["selmask"], j["diag"])
        for i, j in enumerate(jobs):
            if i + LA < len(jobs):
                jn = jobs[i + LA]
                sts[i + LA] = afront(jn["kc"], jn["kbuf"], jn["selmask"], jn["diag"])
            aback(j["kc"], sts.pop(i), j["vbuf"], j["vkeys"], j["ACC"], j["bank"], j["first"], j["last"], j["m_ap"], j["mkk"])
        for bi_, (ACC, bank) in enumerate(((ACS, B_SEL), (ACW, B_WIN))):
            zc = 6 + 4 * bi_
            p.dve(lambda e, ACC=ACC, zc=zc: e.tensor_scalar(out=zz[:, zc:zc + 2], in0=ACC[:, :, 64], scalar1=TINY, scalar2=None, op0=ALU.max), r=[PK(bank)], w=[K("zz", 2, bi_)])
            p.dve(lambda e, zc=zc: e.reciprocal(out=zz[:, zc:zc + 2], in_=zz[:, zc:zc + 2]), r=[K("zz", 2, bi_)], w=[K("zz", 2, bi_)])
            for h in range(2):
                gcol = 3 * h + 1 + bi_
                p.dve(lambda e, zc=zc, h=h, gcol=gcol: e.tensor_mul(out=zz[:, zc + 2 + h:zc + 3 + h], in0=zz[:, zc + h:zc + h + 1], in1=gt[q2][:, gcol:gcol + 1]),
                      r=[K("zz", 2, bi_), kq("gt")], w=[K("zz", 3, bi_, h)])
                p.dve(lambda e, ACC=ACC, zc=zc, h=h: e.scalar_tensor_tensor(out=outq[:, h, :], in0=ACC[:, h, 0:64], scalar=zz[:, zc + 2 + h:zc + 3 + h], in1=outq[:, h, :],
                                                                            op0=ALU.mult, op1=ALU.add), r=[PK(bank), K("zz", 3, bi_, h), K("outq", h)], w=[K("outq", h)])
        yb = (qi // 4) % 2
        p.pe(lambda e: e.transpose(out=pss[B_MK][:, 0:128], in_=outq[:, :, :].rearrange("p h x -> p (h x)"), identity=ident[:, :]), r=[K("outq", 0), K("outq", 1), K("ident")], w=[PK(B_MK)])
        p.act(lambda e: e.copy(out=yo[yb][:, (qi % 4) * 128:(qi % 4 + 1) * 128], in_=pss[B_MK][:, 0:128]), r=[PK(B_MK)], w=[K("yo", yb, qi % 4)])
        if qi % 4 == 3:
            p.dma("sp", d["y"][:, (qi - 3) * 128:(qi + 1) * 128], yo[yb][:, :], r=[K("yo", yb, j) for j in range(4)], w=[K("ydram", qi)])

    for qi in range(NQ):
        qblock(qi)


def nsa_tables(S):
    NSEL = S // 64
    NC = S // 16
    c = np.arange(NC)
    cmp_start = c * 16
    sel_start = np.arange(NSEL) * 64
    ovl = ((cmp_start[:, None] < sel_start[None] + 64) & (cmp_start[:, None] + 32 > sel_start[None])).astype(np.float32)
    ovl[NC - 1:] = 0.0
    q = np.arange(128)
    rel = np.arange(512) - 256
    cq = (q // 64)[:, None]
    ptab = np.where(rel[None] > cq, -1e30, np.where((rel[None] == cq) | (rel[None] == cq - 1), 1e4, 0.0)).astype(np.float32)
    cl = np.arange(128)
    maskc = np.zeros((128, 16, 128), np.float32)
    for pat in range(16):
        maskc[:, pat, :] = (16 * (cl[:, None] - 8 * pat) + 31 <= q[None, :])
    maskp = np.ones((128, 128), np.float32)
    maskp[127, :] = (q >= 15)
    tri = (cl[:, None] <= q[None, :]).astype(np.float32)
    triu = (cl[:, None] > q[None, :]).astype(np.float32)
    ex = np.zeros((128, 64, 128), np.float32)
    for j in range(64):
        for key in range(128):
            ex[2 * j + key // 64, j, key] = 1.0
    return dict(ovl=ovl, ptab=ptab, maskc=maskc, maskp=maskp, tri=tri, triu=triu, ex=ex, ident=np.eye(128, dtype=np.float32))


def nsa_weights(l, inp):
    w1 = inp["nsa_cmp_w1"][l].reshape(2, 32, 64, 128).transpose(0, 2, 1, 3)
    peT = inp["nsa_cmp_pe"][l].transpose(0, 2, 1)
    return dict(w1=np.ascontiguousarray(w1), peT=np.ascontiguousarray(peT), b1=np.ascontiguousarray(inp["nsa_cmp_b1"][l][:, :, None]),
                w2=np.ascontiguousarray(inp["nsa_cmp_w2"][l]))

RET_GN_EPS = 1e-5


def emit_ret(p, S, d, pfx="ret"):
    TB = 512
    NB = S // TB
    K = lambda *a: (pfx,) + a
    cvr = p.sb([128, 4]); p.dma("sp", cvr[:, :], d["cvr"], w=[K("cvr")])
    dec = p.sb([128, 128]); p.dma("sp", dec[:, :], d["dec"], w=[K("dec")])
    qdec = p.sb([128, 128]); p.dma("sp", qdec[:, :], d["qdec"], w=[K("qdec")])
    ident = p.sb([128, 128]); p.dma("sp", ident[:, :], d["ident"], w=[K("ident")])
    R = p.sb([128, 128]); Rb = p.sb([128, 128], BF16)
    p.dve(lambda e: e.memset(R[:, :], 0.0), w=[K("R")])
    p.dve(lambda e: e.memset(Rb[:, :], 0.0), w=[K("Rb")])

    def mk(shape, dt=F32, n=2):
        return [p.sb(shape, dt) for _ in range(n)]
    fm = {n: mk([128, TB]) for n in ("q", "qs", "k", "ks", "cos", "sin")}
    tm = {n: mk([128, 4, 128]) for n in ("k", "v", "g", "cos", "sin")}
    qr = mk([128, TB], BF16); kr = mk([128, TB], BF16); qd = mk([128, TB], BF16)
    t1 = p.sb([128, TB]); t2 = p.sb([128, TB])
    krt = mk([128, 4, 128], BF16); vb = mk([128, 4, 128], BF16); sgt = mk([128, 4, 128])
    u1 = p.sb([128, 4, 128]); u2 = p.sb([128, 4, 128])
    AT = mk([128, 128], BF16)
    o_sb = mk([128, 128]); cen = mk([128, 128]); sq = mk([128, 128])
    st = mk([128, 4])
    yo = mk([128, TB])
    NPS = 6
    pss = [p.ps([128, 512]) for _ in range(NPS)]
    psn = [0]

    def nps():
        i = psn[0] % NPS
        psn[0] += 1
        return pss[i], K("ps", i)

    def blk(b):
        q = b % 2
        t0 = b * TB
        kq = lambda *n: K(*n, q)
        p.dma("sp", fm["q"][q][:, :], d["qT"][:, t0:t0 + TB], w=[kq("fq")])
        p.dma("sp", fm["qs"][q][0:64, :], d["qT"][64:128, t0:t0 + TB], w=[kq("fqs0")])
        p.dma("sp", fm["qs"][q][64:128, :], d["qT"][0:64, t0:t0 + TB], w=[kq("fqs1")])
        p.dma("sp", fm["k"][q][:, :], d["kT"][:, t0:t0 + TB], w=[kq("fk")])
        p.dma("sp", fm["ks"][q][0:64, :], d["kT"][64:128, t0:t0 + TB], w=[kq("fks0")])
        p.dma("sp", fm["ks"][q][64:128, :], d["kT"][0:64, t0:t0 + TB], w=[kq("fks1")])
        p.dma("sp", fm["cos"][q][:, :], d["cosF"][:, t0:t0 + TB], w=[kq("fcos")])
        p.dma("sp", fm["sin"][q][:, :], d["sinF"][:, t0:t0 + TB], w=[kq("fsin")])
        for n in ("k", "v", "g", "cos", "sin"):
            src = {"k": "k", "v": "v", "g": "g", "cos": "cosT", "sin": "sinT"}[n]
            p.dma("sp", tm[n][q][:, :, :], d[src][t0:t0 + TB, :].rearrange("(c p) x -> p c x", p=128), w=[kq("t" + n)])
        for (a, as_, o, on) in (("q", "qs", qr, "qr"), ("k", "ks", kr, "kr")):
            p.dve(lambda e, a=a: e.tensor_mul(out=t1[:, :], in0=fm[a][q][:, :], in1=fm["cos"][q][:, :]), r=[kq("f" + a), kq("fcos")], w=[K("t1")])
            p.pool(lambda e, as_=as_: e.tensor_mul(out=t2[:, :], in0=fm[as_][q][:, :], in1=fm["sin"][q][:, :]), r=[kq("f" + as_ + "0"), kq("f" + as_ + "1"), kq("fsin")], w=[K("t2")])
            p.dve(lambda e, o=o: e.tensor_add(out=o[q][:, :], in0=t1[:, :], in1=t2[:, :]), r=[K("t1"), K("t2")], w=[kq(on)])
        p.pool(lambda e: e.tensor_mul(out=qd[q][:, :].rearrange("p (c x) -> p c x", c=4), in0=qr[q][:, :].rearrange("p (c x) -> p c x", c=4),
                                      in1=qdec[:, :].unsqueeze(1).to_broadcast([128, 4, 128])), r=[kq("qr"), K("qdec")], w=[kq("qd")])
        p.dve(lambda e: e.tensor_mul(out=u1[:, :, :], in0=tm["k"][q][:, :, :], in1=tm["cos"][q][:, :, :]), r=[kq("tk"), kq("tcos")], w=[K("u1")])
        p.pool(lambda e: e.tensor_mul(out=u2[:, :, 0:64], in0=tm["k"][q][:, :, 64:128], in1=tm["sin"][q][:, :, 0:64]), r=[kq("tk"), kq("tsin")], w=[K("u2", 0)])
        p.pool(lambda e: e.tensor_mul(out=u2[:, :, 64:128], in0=tm["k"][q][:, :, 0:64], in1=tm["sin"][q][:, :, 64:128]), r=[kq("tk"), kq("tsin")], w=[K("u2", 1)])
        p.dve(lambda e: e.tensor_add(out=u1[:, :, :], in0=u1[:, :, :], in1=u2[:, :, :]), r=[K("u1"), K("u2", 0), K("u2", 1)], w=[K("u1")])
        p.dve(lambda e: e.tensor_scalar(out=krt[q][:, :, :], in0=u1[:, :, :], scalar1=cvr[:, 0:1], scalar2=None, op0=ALU.mult), r=[K("u1"), K("cvr")], w=[kq("krt")])
        p.act(lambda e: e.copy(out=vb[q][:, :, :], in_=tm["v"][q][:, :, :]), r=[kq("tv")], w=[kq("vb")])
        p.act(lambda e: e.activation(out=sgt[q][:, :, :], in_=tm["g"][q][:, :, :], func=AF.Silu), r=[kq("tg")], w=[kq("sg")])
        for ci in range(4):
            cq = (b * 4 + ci) % 2
            cs = slice(ci * 128, (ci + 1) * 128)
            ps, pk_ = nps()
            p.pe(lambda e, ps=ps, cs=cs: e.matmul(ps[:, 0:128], lhsT=kr[q][:, cs], rhs=qr[q][:, cs], start=True, stop=True), r=[kq("kr"), kq("qr")], w=[pk_])
            p.dve(lambda e, ps=ps, cq=cq: e.tensor_mul(out=AT[cq][:, :], in0=ps[:, 0:128], in1=dec[:, :]), r=[pk_, K("dec")], w=[K("AT", cq)])
            ps2, pk2 = nps()
            p.pe(lambda e, ps2=ps2, cq=cq, ci=ci: e.matmul(ps2[:, 0:128], lhsT=AT[cq][:, :], rhs=vb[q][:, ci, :], start=True, stop=False), r=[K("AT", cq), kq("vb")], w=[pk2])
            p.pe(lambda e, ps2=ps2, cs=cs: e.matmul(ps2[:, 0:128], lhsT=qd[q][:, cs], rhs=Rb[:, :], start=False, stop=True), r=[kq("qd"), K("Rb")], w=[pk2])
            ps3, pk3 = nps()
            p.pe(lambda e, ps3=ps3, ci=ci: e.matmul(ps3[:, 0:128], lhsT=krt[q][:, ci, :], rhs=vb[q][:, ci, :], start=True, stop=True), r=[kq("krt"), kq("vb")], w=[pk3])
            p.dve(lambda e, ps3=ps3: e.scalar_tensor_tensor(out=R[:, :], in0=R[:, :], scalar=cvr[:, 1:2], in1=ps3[:, 0:128], op0=ALU.mult, op1=ALU.add),
                  r=[K("R"), pk3, K("cvr")], w=[K("R")])
            p.act(lambda e: e.copy(out=Rb[:, :], in_=R[:, :]), r=[K("R")], w=[K("Rb")])
            o = o_sb[cq]; ce = cen[cq]; s_ = sq[cq]; stt_ = st[cq]
            p.act(lambda e, ps2=ps2, o=o: e.copy(out=o[:, :], in_=ps2[:, 0:128]), r=[pk2], w=[K("o", cq)])
            if "dbg_o" in d:
                p.dma("sp", d["dbg_o"][t0 + ci * 128:t0 + (ci + 1) * 128, :], o[:, :], r=[K("o", cq)], w=[K("dbgo", b, ci)])
                p.dma("sp", d["dbg_sg"][t0 + ci * 128:t0 + (ci + 1) * 128, :], sgt[q][:, ci, :], r=[kq("sg")], w=[K("dbgsg", b, ci)])
            p.dve(lambda e, o=o, stt_=stt_: e.tensor_reduce(out=stt_[:, 0:1], in_=o[:, :], axis=AX.X, op=ALU.add), r=[K("o", cq)], w=[K("st0", cq)])
            p.dve(lambda e, stt_=stt_: e.tensor_scalar(out=stt_[:, 1:2], in0=stt_[:, 0:1], scalar1=-1.0 / 128, scalar2=None, op0=ALU.mult), r=[K("st0", cq)], w=[K("st1", cq)])
            p.dve(lambda e, o=o, ce=ce, stt_=stt_: e.tensor_scalar(out=ce[:, :], in0=o[:, :], scalar1=stt_[:, 1:2], scalar2=None, op0=ALU.add), r=[K("o", cq), K("st1", cq)], w=[K("cen", cq)])
            p.dve(lambda e, ce=ce, s_=s_: e.tensor_mul(out=s_[:, :], in0=ce[:, :], in1=ce[:, :]), r=[K("cen", cq)], w=[K("sq", cq)])
            p.dve(lambda e, s_=s_, stt_=stt_: e.tensor_reduce(out=stt_[:, 2:3], in_=s_[:, :], axis=AX.X, op=ALU.add), r=[K("sq", cq)], w=[K("st2", cq)])
            p.act(lambda e, stt_=stt_: e.activation(out=stt_[:, 3:4], in_=stt_[:, 2:3], func=AF.Sqrt, bias=cvr[:, 2:3], scale=1.0 / 128), r=[K("st2", cq), K("cvr")], w=[K("st3", cq)])
            p.dve(lambda e, stt_=stt_: e.reciprocal(out=stt_[:, 3:4], in_=stt_[:, 3:4]), r=[K("st3", cq)], w=[K("st3", cq)])
            p.dve(lambda e, ce=ce, stt_=stt_, ci=ci: e.scalar_tensor_tensor(out=ce[:, :], in0=ce[:, :], scalar=stt_[:, 3:4], in1=sgt[q][:, ci, :], op0=ALU.mult, op1=ALU.mult),
                  r=[K("cen", cq), K("st3", cq), kq("sg")], w=[K("cen", cq)])
            ps4, pk4 = nps()
            p.pe(lambda e, ps4=ps4, ce=ce: e.transpose(out=ps4[:, 0:128], in_=ce[:, :], identity=ident[:, :]), r=[K("cen", cq), K("ident")], w=[pk4])
            p.act(lambda e, ps4=ps4, cs=cs: e.copy(out=yo[q][:, cs], in_=ps4[:, 0:128]), r=[pk4], w=[kq("yo", ci)])
        p.dma("sp", d["y"][:, t0:t0 + TB], yo[q][:, :], r=[kq("yo", ci) for ci in range(4)], w=[K("ydram", b)])

    for b in range(NB):
        blk(b)


def ret_tables(S):
    DK = 128
    inv_freq = (1.0 / (np.float32(10000.0) ** np.linspace(0.0, 1.0, DK // 2, dtype=np.float32))).astype(np.float32)
    ang = (np.arange(S, dtype=np.float32)[:, None] * inv_freq[None, :]).astype(np.float32)
    cos = np.cos(ang.astype(np.float64)).astype(np.float32); sin = np.sin(ang.astype(np.float64)).astype(np.float32)
    cosT = np.concatenate([cos, cos], 1); sinT = np.concatenate([-sin, sin], 1)
    return dict(cosT=np.ascontiguousarray(cosT), sinT=np.ascontiguousarray(sinT),
                cosF=np.ascontiguousarray(cosT.T), sinF=np.ascontiguousarray(sinT.T))


def ret_head_consts(h):
    C = 128
    log_g = np.log1p(-np.exp2(-5.0 - h))
    i = np.arange(C, dtype=np.float64)
    diff = i[None, :] - i[:, None]
    s = 128 ** -0.5
    dec = np.where(diff >= 0, np.exp(log_g * np.maximum(diff, 0.0)), 0.0) * s
    qdec = np.tile(np.exp(log_g * (i + 1.0))[None, :], (128, 1))
    cvr = np.zeros((128, 4), np.float32)
    cvr[:, 0] = np.exp(log_g * (C - 1.0 - i)) * s
    cvr[:, 1] = np.exp(log_g * C)
    cvr[:, 2] = RET_GN_EPS
    return dict(dec=dec.astype(np.float32), qdec=qdec.astype(np.float32), cvr=cvr, ident=np.eye(128, dtype=np.float32))

DN_ALPHA = 4 ** 0.25
LN_EPS = 1e-5
NEXP = 32


def emit_stageb(p, T, d, pfx="sb"):
    TB = 512
    NP = T // TB
    K = lambda *a: (pfx,) + a
    lnp = p.sb([128, 16, 4]); p.dma("sp", lnp[:, :, :], d["lnp"].rearrange("p (m c) -> p m c", c=4), w=[K("lnp")])
    wr = p.sb([128, 16, 36]); p.dma("sp", wr[:, :, :], d["wrS"].rearrange("p (m c) -> p m c", c=36), w=[K("wr")])
    brb = p.sb([128, 36]); p.dma("sp", brb[:, :], d["brb"], w=[K("brb")])
    selE = p.sb([32, 32, 128], BF16); p.dma("pool", selE[:, :, :], d["selE"].rearrange("k (e m) -> k e m", m=128), w=[K("selE")])
    ones = p.sb([128, 128]); p.dma("sp", ones[:, :], d["ones"], w=[K("ones")])
    ident = p.sb([128, 128]); p.dma("sp", ident[:, :], d["ident"], w=[K("ident")])
    epsc = p.sb([128, 1]); p.dve(lambda e: e.memset(epsc[:, :], LN_EPS), w=[K("epsc")])

    xf = p.sb([128, 16, TB])
    xb = p.sb([128, 16, TB], BF16)
    arena = p.sb([128, 55296], BF16)
    def carve(off, shape):
        n = int(np.prod(shape))
        v = arena[:, off:off + n]
        if len(shape) == 2:
            return v.rearrange("p (a b) -> p a b", a=shape[0])
        if len(shape) == 3:
            return v.rearrange("p (a b c) -> p a b c", a=shape[0], b=shape[1])
        return v
    yb = carve(0, [3, 8, TB])
    merb = carve(12288, [16, TB])
    wgs = [carve(20480, [16, 3, 128]), carve(26624, [16, 3, 128])]
    wbs = [carve(32768, [8, 3, 128]), carve(35840, [8, 3, 128])]
    wos = [carve(38912, [16, 128]), carve(40960, [16, 128])]
    wge = [carve(0, [16, 512]), carve(8192, [16, 512])]
    wue = [carve(16384, [16, 512]), carve(24576, [16, 512])]
    wde = [carve(32768, [4, 2048]), carve(40960, [4, 2048])]
    hT = [carve(49152, [4, TB]), carve(51200, [4, TB])]
    A1 = [K("phB1")]
    A2 = [K("phMoE")]
    mer = p.sb([128, TB]); sg = p.sb([128, TB]); tmp = p.sb([128, TB])
    mean = p.sb([128, TB]); rstd = p.sb([128, TB])
    gwe = [p.sb([128, TB]) for _ in range(2)]
    gwT = p.sb([32, TB], BF16)
    lg = p.sb([128, 36]); rt = p.sb([128, 64]); gw = p.sb([128, 4, 8]); t48 = p.sb([128, 4, 8])
    NPS = 8
    pss = [p.ps([128, 512]) for _ in range(NPS)]
    psn = [0]

    def nps():
        i = psn[0] % NPS
        psn[0] += 1
        return pss[i], K("ps", i)

    def layernorm(which, pi):
        ps_s, ks = nps()
        for m in range(16):
            p.pe(lambda e, m=m: e.matmul(ps_s[:, :], lhsT=ones[:, :], rhs=xf[:, m, :], start=(m == 0), stop=(m == 15)), r=[K("xf", m), K("ones")], w=[ks])
        ps_q, kq_ = nps()
        for m in range(16):
            p.dve(lambda e, m=m: e.tensor_mul(out=tmp[:, :], in0=xf[:, m, :], in1=xf[:, m, :]), r=[K("xf", m)], w=[K("tmp")])
            p.pe(lambda e, m=m: e.matmul(ps_q[:, :], lhsT=ones[:, :], rhs=tmp[:, :], start=(m == 0), stop=(m == 15)), r=[K("tmp"), K("ones")], w=[kq_])
        p.act(lambda e: e.mul(out=mean[:, :], in_=ps_s[:, :], mul=1.0 / 2048), r=[ks], w=[K("mean")])
        p.dve(lambda e: e.tensor_mul(out=tmp[:, :], in0=mean[:, :], in1=mean[:, :]), r=[K("mean")], w=[K("tmp")])
        p.dve(lambda e: e.scalar_tensor_tensor(out=rstd[:, :], in0=ps_q[:, :], scalar=1.0 / 2048, in1=tmp[:, :], op0=ALU.mult, op1=ALU.subtract), r=[kq_, K("tmp")], w=[K("rstd")])
        p.act(lambda e: e.activation(out=rstd[:, :], in_=rstd[:, :], func=AF.Sqrt, bias=epsc[:, 0:1], scale=1.0), r=[K("rstd"), K("epsc")], w=[K("rstd")])
        p.dve(lambda e: e.reciprocal(out=rstd[:, :], in_=rstd[:, :]), r=[K("rstd")], w=[K("rstd")])
        for m in range(16):
            p.dve(lambda e, m=m: e.tensor_sub(out=xf[:, m, :], in0=xf[:, m, :], in1=mean[:, :]), r=[K("xf", m), K("mean")], w=[K("xf", m)])
            p.dve(lambda e, m=m: e.tensor_mul(out=xf[:, m, :], in0=xf[:, m, :], in1=rstd[:, :]), r=[K("xf", m), K("rstd")], w=[K("xf", m)])
            p.dve(lambda e, m=m: e.tensor_scalar(out=xf[:, m, :], in0=xf[:, m, :], scalar1=lnp[:, m, 2 * which:2 * which + 1], scalar2=lnp[:, m, 2 * which + 1:2 * which + 2],
                                                 op0=ALU.mult, op1=ALU.add), r=[K("xf", m), K("lnp")], w=[K("xf", m)])

    def tpass(pi):
        t0 = pi * TB
        p.dma("sp", xf[:, :, :], d["xT"][:, t0:t0 + TB].rearrange("(m p) t -> p m t", p=128), r=[("xTc_dram",)], w=[K("xf", m) for m in range(16)])
        for m in range(16):
            p.act(lambda e, m=m: e.copy(out=xb[:, m, :], in_=xf[:, m, :]), r=[K("xf", m)], w=[K("xb", m)])
        for b in range(3):
            p.dma("pool", yb[:, b, :, :], d["yT"][b, :, t0:t0 + TB].rearrange("(kc p) t -> p kc t", p=128), r=[("yT_dram", b)], w=[K("yb", b)] + (A2 if b == 0 else []))
        for m in range(16):
            q = m % 2
            p.dma("pool", wgs[q][:, :, :, :], d["wgS"][m].rearrange("p (kc b j) -> p kc b j", kc=16, b=3), w=[K("wgs", q)])
            p.dma("pool", wbs[q][:, :, :, :], d["wbrS"][m].rearrange("p (kc b j) -> p kc b j", kc=8, b=3), w=[K("wbs", q)])
            for b in range(3):
                psG, kG = nps()
                for kc in range(16):
                    p.pe(lambda e, psG=psG, kc=kc, b=b, q=q: e.matmul(psG[:, :], lhsT=wgs[q][:, kc, b, :], rhs=xb[:, kc, :], start=(kc == 0), stop=(kc == 15)),
                         r=[K("wgs", q), K("xb", kc)] + A1, w=[kG])
                psY, kY = nps()
                for kc in range(8):
                    p.pe(lambda e, psY=psY, kc=kc, b=b, q=q: e.matmul(psY[:, :], lhsT=wbs[q][:, kc, b, :], rhs=yb[:, b, kc, :], start=(kc == 0), stop=(kc == 7)),
                         r=[K("wbs", q), K("yb", b)] + A1, w=[kY])
                p.act(lambda e, psG=psG: e.activation(out=sg[:, :], in_=psG[:, :], func=AF.Sigmoid), r=[kG], w=[K("sg")])
                if b == 0:
                    p.dve(lambda e, psY=psY: e.tensor_mul(out=mer[:, :], in0=psY[:, :], in1=sg[:, :]), r=[kY, K("sg")], w=[K("mer")])
                else:
                    p.dve(lambda e, psY=psY: e.tensor_mul(out=tmp[:, :], in0=psY[:, :], in1=sg[:, :]), r=[kY, K("sg")], w=[K("tmp")])
                    p.dve(lambda e: e.tensor_add(out=mer[:, :], in0=mer[:, :], in1=tmp[:, :]), r=[K("mer"), K("tmp")], w=[K("mer")])
            p.act(lambda e, m=m: e.copy(out=merb[:, m, :], in_=mer[:, :]), r=[K("mer")], w=[K("merb", m)])
        for m2 in range(16):
            q = m2 % 2
            p.dma("pool", wos[q][:, :, :], d["woutS"][m2].rearrange("p (kc j) -> p kc j", kc=16), w=[K("wos", q)])
            ps, kp = nps()
            for kc in range(16):
                p.pe(lambda e, ps=ps, kc=kc, q=q: e.matmul(ps[:, :], lhsT=wos[q][:, kc, :], rhs=merb[:, kc, :], start=(kc == 0), stop=(kc == 15)),
                     r=[K("wos", q), K("merb", kc)] + A1, w=[kp])
            p.dve(lambda e, ps=ps, m2=m2: e.scalar_tensor_tensor(out=xf[:, m2, :], in0=xf[:, m2, :], scalar=DN_ALPHA, in1=ps[:, :], op0=ALU.mult, op1=ALU.add),
                  r=[K("xf", m2), kp], w=[K("xf", m2)])
        layernorm(0, pi)
        for m in range(16):
            p.act(lambda e, m=m: e.copy(out=xb[:, m, :], in_=xf[:, m, :]), r=[K("xf", m)], w=[K("xb", m)])
        for j in range(TB // 128):
            js = slice(j * 128, (j + 1) * 128)
            ps, kp = nps()
            for m in range(16):
                p.pe(lambda e, ps=ps, m=m, js=js: e.matmul(ps[:, 0:36], lhsT=xf[:, m, js], rhs=wr[:, m, :], start=(m == 0), stop=(m == 15)), r=[K("xf", m), K("wr")], w=[kp])
            R = lambda *a: K("rt", *a)
            p.dve(lambda e, ps=ps: e.tensor_add(out=lg[:, :], in0=ps[:, 0:36], in1=brb[:, :]), r=[kp, K("brb")], w=[K("lg")])
            p.dve(lambda e: e.tensor_reduce(out=rt[:, 0:1], in_=lg[:, 0:4], axis=AX.X, op=ALU.max), r=[K("lg")], w=[R(0)])
            p.dve(lambda e: e.tensor_scalar(out=rt[:, 1:2], in0=rt[:, 0:1], scalar1=-1.0, scalar2=None, op0=ALU.mult), r=[R(0)], w=[R(1)])
            p.dve(lambda e: e.tensor_scalar(out=rt[:, 4:8], in0=lg[:, 0:4], scalar1=rt[:, 0:1], scalar2=None, op0=ALU.is_ge), r=[K("lg"), R(0)], w=[R(4)])
            p.act(lambda e: e.activation(out=rt[:, 8:12], in_=lg[:, 0:4], func=AF.Exp, bias=rt[:, 1:2], scale=1.0), r=[K("lg"), R(1)], w=[R(8)])
            p.dve(lambda e: e.tensor_reduce(out=rt[:, 2:3], in_=rt[:, 8:12], axis=AX.X, op=ALU.add), r=[R(8)], w=[R(2)])
            p.dve(lambda e: e.reciprocal(out=rt[:, 3:4], in_=rt[:, 2:3]), r=[R(2)], w=[R(3)])
            p.dve(lambda e: e.tensor_mul(out=t48[:, :, :], in0=lg[:, 4:36].rearrange("p (g x) -> p g x", g=4), in1=rt[:, 4:8].unsqueeze(2).to_broadcast([128, 4, 8])),
                  r=[K("lg"), R(4)], w=[K("t48")])
            p.dve(lambda e: e.tensor_reduce(out=rt[:, 20:28], in_=t48[:, :, :].rearrange("p g x -> p x g"), axis=AX.X, op=ALU.add), r=[K("t48")], w=[R(20)])
            p.dve(lambda e: e.tensor_reduce(out=rt[:, 12:13], in_=rt[:, 20:28], axis=AX.X, op=ALU.max), r=[R(20)], w=[R(12)])
            p.dve(lambda e: e.tensor_scalar(out=rt[:, 28:36], in0=rt[:, 20:28], scalar1=rt[:, 12:13], scalar2=None, op0=ALU.is_ge), r=[R(20), R(12)], w=[R(28)])
            p.dve(lambda e: e.scalar_tensor_tensor(out=rt[:, 36:44], in0=rt[:, 28:36], scalar=-1e30, in1=rt[:, 20:28], op0=ALU.mult, op1=ALU.add), r=[R(28), R(20)], w=[R(36)])
            p.dve(lambda e: e.tensor_reduce(out=rt[:, 13:14], in_=rt[:, 36:44], axis=AX.X, op=ALU.max), r=[R(36)], w=[R(13)])
            p.dve(lambda e: e.tensor_scalar(out=rt[:, 44:52], in0=rt[:, 36:44], scalar1=rt[:, 13:14], scalar2=None, op0=ALU.is_ge), r=[R(36), R(13)], w=[R(44)])
            p.dve(lambda e: e.tensor_sub(out=rt[:, 14:15], in0=rt[:, 13:14], in1=rt[:, 12:13]), r=[R(12), R(13)], w=[R(14)])
            p.act(lambda e: e.activation(out=rt[:, 15:16], in_=rt[:, 14:15], func=AF.Exp), r=[R(14)], w=[R(15)])
            p.dve(lambda e: e.tensor_scalar(out=rt[:, 16:17], in0=rt[:, 15:16], scalar1=1.0, scalar2=None, op0=ALU.add), r=[R(15)], w=[R(16)])
            p.dve(lambda e: e.reciprocal(out=rt[:, 16:17], in_=rt[:, 16:17]), r=[R(16)], w=[R(16)])
            p.dve(lambda e: e.tensor_mul(out=rt[:, 17:18], in0=rt[:, 15:16], in1=rt[:, 16:17]), r=[R(15), R(16)], w=[R(17)])
            p.dve(lambda e: e.tensor_scalar(out=rt[:, 16:18], in0=rt[:, 16:18], scalar1=rt[:, 3:4], scalar2=None, op0=ALU.mult), r=[R(16), R(17), R(3)], w=[R(16), R(17)])
            p.dve(lambda e: e.tensor_scalar(out=rt[:, 52:60], in0=rt[:, 28:36], scalar1=rt[:, 16:17], scalar2=None, op0=ALU.mult), r=[R(28), R(16)], w=[R(52)])
            p.dve(lambda e: e.scalar_tensor_tensor(out=rt[:, 52:60], in0=rt[:, 44:52], scalar=rt[:, 17:18], in1=rt[:, 52:60], op0=ALU.mult, op1=ALU.add), r=[R(44), R(17), R(52)], w=[R(52)])
            p.dve(lambda e: e.tensor_copy(out=gw[:, :, :], in_=rt[:, 52:60].unsqueeze(1).to_broadcast([128, 4, 8])), r=[R(52)], w=[K("gw")])
            p.dve(lambda e: e.tensor_mul(out=gw[:, :, :], in0=gw[:, :, :], in1=rt[:, 4:8].unsqueeze(2).to_broadcast([128, 4, 8])), r=[K("gw"), R(4)], w=[K("gw")])
            ps2, kp2 = nps()
            p.pe(lambda e, ps2=ps2: e.transpose(out=ps2[0:32, 0:128], in_=gw[:, :, :].rearrange("p g x -> p (g x)"), identity=ident[:, :]), r=[K("gw"), K("ident")], w=[kp2])
            p.act(lambda e, ps2=ps2, js=js: e.copy(out=gwT[:, js], in_=ps2[0:32, 0:128]), r=[kp2], w=[K("gwT", j)])
        if "dbg_gw" in d:
            p.dma("sp", d["dbg_gw"][:, t0:t0 + TB], gwT[:, :], r=[K("gwT", j) for j in range(4)], w=[K("dbggw", pi)])
            p.dma("sp", d["dbg_x1"][:, t0:t0 + TB].rearrange("(m p) t -> p m t", p=128), xf[:, :, :], r=[K("xf", m) for m in range(16)], w=[K("dbgx1", pi)])
        for m in range(16):
            p.act(lambda e, m=m: e.mul(out=xf[:, m, :], in_=xf[:, m, :], mul=DN_ALPHA), r=[K("xf", m)], w=[K("xf", m)])
        for ex in range(NEXP):
            q = ex % 2
            p.dma("pool", wge[q][:, :, :], d["w_gate"][ex].rearrange("(kc p) f -> p kc f", p=128), w=[K("wge", q)] + (A1 if ex == 0 else []))
            p.dma("pool", wue[q][:, :, :], d["w_up"][ex].rearrange("(kc p) f -> p kc f", p=128), w=[K("wue", q)])
            p.dma("pool", wde[q][:, :, :], d["w_down"][ex].rearrange("(f p) n -> p f n", p=128), w=[K("wde", q)])
            ps, kp = nps()
            p.pe(lambda e, ps=ps, ex=ex: e.matmul(ps[:, :], lhsT=selE[:, ex, :], rhs=gwT[:, :], start=True, stop=True), r=[K("selE")] + [K("gwT", j) for j in range(4)], w=[kp])
            p.act(lambda e, ps=ps, q=q: e.copy(out=gwe[q][:, :], in_=ps[:, :]), r=[kp], w=[K("gwe", q)])
            for f in range(4):
                psG, kG = nps()
                for kc in range(16):
                    p.pe(lambda e, psG=psG, kc=kc, f=f, q=q: e.matmul(psG[:, :], lhsT=wge[q][:, kc, f * 128:(f + 1) * 128], rhs=xb[:, kc, :], start=(kc == 0), stop=(kc == 15)),
                         r=[K("wge", q), K("xb", kc)] + A2, w=[kG])
                psU, kU = nps()
                for kc in range(16):
                    p.pe(lambda e, psU=psU, kc=kc, f=f, q=q: e.matmul(psU[:, :], lhsT=wue[q][:, kc, f * 128:(f + 1) * 128], rhs=xb[:, kc, :], start=(kc == 0), stop=(kc == 15)),
                         r=[K("wue", q), K("xb", kc)] + A2, w=[kU])
                p.act(lambda e, psG=psG: e.activation(out=sg[:, :], in_=psG[:, :], func=AF.Silu), r=[kG], w=[K("sg")])
                p.dve(lambda e, psU=psU: e.tensor_mul(out=tmp[:, :], in0=psU[:, :], in1=sg[:, :]), r=[kU, K("sg")], w=[K("tmp")])
                p.dve(lambda e, f=f, q=q: e.tensor_mul(out=hT[q][:, f, :], in0=tmp[:, :], in1=gwe[q][:, :]), r=[K("tmp"), K("gwe", q)], w=[K("hT", q, f)])
            for m in range(16):
                psD, kD = nps()
                for f in range(4):
                    p.pe(lambda e, psD=psD, f=f, m=m, q=q: e.matmul(psD[:, :], lhsT=wde[q][:, f, m * 128:(m + 1) * 128], rhs=hT[q][:, f, :], start=(f == 0), stop=(f == 3)),
                         r=[K("wde", q), K("hT", q, f)] + A2, w=[kD])
                p.dve(lambda e, psD=psD, m=m: e.tensor_add(out=xf[:, m, :], in0=xf[:, m, :], in1=psD[:, :]), r=[K("xf", m), kD], w=[K("xf", m)])
        layernorm(1, pi)
        for m in range(16):
            p.dma("sp", d["out"][m * 128:(m + 1) * 128, t0:t0 + TB], xf[:, m, :], r=[K("xf", m)], w=[("out_dram", pi, m)])

    for pi in range(NP):
        tpass(pi)


def stageb_host(l, inp, first):
    W = inp["w_in_first"] if first else inp["w_in_deep"][l - 1]
    g0 = W.shape[1] - 3 * 2048
    Wg = W[:, g0:].reshape(16, 128, 3, 16, 128)
    wgS = np.ascontiguousarray(Wg.transpose(3, 1, 0, 2, 4)).reshape(16, 128, 16 * 3 * 128)
    wbr = np.stack([inp["w_br_rw"][l], inp["w_br_nsa"][l], inp["w_br_ret"][l]])
    wbrS = np.ascontiguousarray(wbr.reshape(3, 8, 128, 16, 128).transpose(3, 2, 1, 0, 4)).reshape(16, 128, 8 * 3 * 128)
    woutS = np.ascontiguousarray(inp["w_out"][l].reshape(16, 128, 16, 128).transpose(2, 1, 0, 3)).reshape(16, 128, 16 * 128)
    lnp = np.stack([inp["ln1_g"][l], inp["ln1_b"][l], inp["ln2_g"][l], inp["ln2_b"][l]], -1).reshape(16, 128, 4).transpose(1, 0, 2)
    Wr = np.concatenate([inp["moe_w_grp"][l], inp["moe_w_exp"][l]], 1)
    wrS = Wr.reshape(16, 128, 36).transpose(1, 0, 2)
    br = np.concatenate([inp["moe_b_grp"][l], inp["moe_b_exp"][l]])
    selE = np.zeros((32, 32, 128), np.float32)
    for e in range(32):
        selE[e, e, :] = 1.0
    return dict(wgS=wgS, wbrS=wbrS, woutS=woutS, lnp=np.ascontiguousarray(lnp).reshape(128, 64), wrS=np.ascontiguousarray(wrS).reshape(128, 16 * 36),
                brb=np.ascontiguousarray(np.broadcast_to(br[None], (128, 36))), selE=selE.reshape(32, 32 * 128),
                ones=np.ones((128, 128), np.float32), ident=np.eye(128, dtype=np.float32),
                w_gate=inp["moe_w_gate"][l], w_up=inp["moe_w_up"][l], w_down=inp["moe_w_down"][l])

NF_A = 1664
NT_A = 518
_PROG_CACHE = {}


def build_stage_a(S):
    p = Prog()
    E = lambda n, sh, dt=F32: p.dram(n, sh, dt, kind="ExternalInput")
    O = lambda n, sh, dt=F32: p.dram(n, sh, dt, kind="ExternalOutput")
    NSEL = S // 64
    NC = S // 16
    d = dict(xT=E("xT", [2048, S]), wfm=E("wfm", [2048, NF_A]), wtm=E("wtm", [2048, NT_A]),
             ofm=p.dram("ofm", [NF_A, S]), otm=p.dram("otm", [S, NT_A]))
    emit_proj(p, S, NF_A, NT_A, d)
    p.phase()
    ofm, otm = d["ofm"], d["otm"]
    ident = E("ident", [128, 128])
    rw = dict(pr=ofm[0:128, :], pk=ofm[128:256, :], pv=ofm[256:384, :], pxw=ofm[384:480, :], pxa=ofm[480:576, :], pxg=ofm[576:832, :], pxv=ofm[832:896, :],
              cv=E("rw_cv", [128, len(CV_NAMES)]), ident=ident, bones=E("rw_bones", [128, 128]), w_up=E("rw_w_up", [96, 128]), a_up=E("rw_a_up", [96, 128]),
              g_up=E("rw_g_up", [256, 128]), v_up=E("rw_v_up", [64, 128]), vf_in=E("vf_in", [128, S]), vf_out=O("vf_out", [128, S]), y=O("y_rw", [128, S]),
              scr=p.dram("rw_scr", [2, S, 5, 64]))
    emit_rwkv(p, S, rw)
    p.phase()
    nsa = dict(qT=ofm[896:1152, :].rearrange("(h x) s -> h x s", h=4), kcT=ofm[1152:1216, :], vcT=ofm[1216:1280, :], ksT=ofm[1280:1344, :], kwT=ofm[1344:1408, :],
               vs=otm[:, 0:64], vw=otm[:, 64:128], gate=otm[:, 128:134],
               w1=E("nsa_w1", [2, 64, 32, 128]), peT=E("nsa_peT", [2, 64, 32]), b1=E("nsa_b1", [2, 128, 1]), w2=E("nsa_w2", [2, 128, 64]),
               ovl=E("nsa_ovl", [NC, NSEL]), ptab=E("nsa_ptab", [128, 512]), maskc=E("nsa_maskc", [128, 16, 128]), maskp=E("nsa_maskp", [128, 128]),
               tri=E("nsa_tri", [128, 128]), triu=E("nsa_triu", [128, 128]), ex=E("nsa_ex", [128, 64, 128]), ident=ident, y=O("y_nsa", [128, S]))
    emit_nsa(p, S, nsa)
    p.phase()
    ret = dict(qT=ofm[1408:1536, :], kT=ofm[1536:1664, :], k=otm[:, 134:262], v=otm[:, 262:390], g=otm[:, 390:518],
               cosF=E("ret_cosF", [128, S]), sinF=E("ret_sinF", [128, S]), cosT=E("ret_cosT", [S, 128]), sinT=E("ret_sinT", [S, 128]),
               dec=E("ret_dec", [128, 128]), qdec=E("ret_qdec", [128, 128]), cvr=E("ret_cvr", [128, 4]), ident=ident, y=O("y_ret", [128, S]))
    emit_ret(p, S, ret)
    return p.finalize()


def build_stage_b(T):
    p = Prog()
    E = lambda n, sh, dt=F32: p.dram(n, sh, dt, kind="ExternalInput")
    d = dict(xT=E("xTc", [2048, T]), yT=E("yT", [3, 1024, T]), wgS=E("wgS", [16, 128, 6144]), wbrS=E("wbrS", [16, 128, 3072]), woutS=E("woutS", [16, 128, 2048]),
             lnp=E("lnp", [128, 64]), wrS=E("wrS", [128, 576]), brb=E("brb", [128, 36]), selE=E("selE", [32, 4096]), ones=E("ones", [128, 128]), ident=E("ident", [128, 128]),
             w_gate=E("w_gate", [32, 2048, 512]), w_up=E("w_up", [32, 2048, 512]), w_down=E("w_down", [32, 512, 2048]),
             out=p.dram("out", [2048, T], kind="ExternalOutput"))
    emit_stageb(p, T, d)
    return p.finalize()


def stage_a_weights(c, l, inp):
    first = l == 0
    W = inp["w_in_first"] if first else inp["w_in_deep"][l - 1]
    rwc = 3520 if first else 3584
    n0 = rwc
    r0 = rwc + 2608
    hc = slice(c * 128, c * 128 + 128)
    g, hp = c // 2, c % 2
    heads = [g * 4 + 2 * hp, g * 4 + 2 * hp + 1, g * 4 + 2 * (1 - hp), g * 4 + 2 * (1 - hp) + 1]
    gs = slice(g * 64, (g + 1) * 64)
    fm = [W[:, 0:1024][:, hc], W[:, 1024:2048][:, hc], W[:, 2048:3072][:, hc], W[:, 3072:3168], W[:, 3168:3264], W[:, 3264:3520],
          (W[:, 3520:3584] if not first else np.zeros((2048, 64), np.float32))]
    q = W[:, n0:n0 + 1024]
    fm += [q[:, h * 64:(h + 1) * 64] for h in heads]
    kv = [W[:, n0 + 1024 + i * 256:n0 + 1024 + (i + 1) * 256] for i in range(6)]
    fm += [kv[0][:, gs], kv[1][:, gs], kv[2][:, gs], kv[4][:, gs]]
    rq, rk, rv, rg = [W[:, r0 + i * 1024:r0 + (i + 1) * 1024][:, hc] for i in range(4)]
    fm += [rq, rk]
    gate = W[:, n0 + 1024 + 1536:n0 + 1024 + 1536 + 48]
    gc = g * 12 + 2 * hp * 3
    tm = [kv[3][:, gs], kv[5][:, gs], gate[:, gc:gc + 6], rk, rv, rg]
    wfm = np.ascontiguousarray(np.concatenate(fm, 1)); wtm = np.ascontiguousarray(np.concatenate(tm, 1))
    assert wfm.shape[1] == NF_A and wtm.shape[1] == NT_A
    return wfm, wtm


def forward(inp, S):
    n = 8
    T = S // n
    inp = {k: np.asarray(v) for k, v in inp.items()}
    x = inp["x"][0, :S]
    xT = np.ascontiguousarray(x.T)
    if ("A", S) not in _PROG_CACHE:
        _PROG_CACHE[("A", S)] = build_stage_a(S)
        _PROG_CACHE[("B", T)] = build_stage_b(T)
    ncA, ncB = _PROG_CACHE[("A", S)], _PROG_CACHE[("B", T)]
    ntab = nsa_tables(S)
    rtab = ret_tables(S)
    ident = np.eye(128, dtype=np.float32)
    vf = [np.zeros((128, S), np.float32) for _ in range(n)]
    for l in range(2):
        nw = nsa_weights(l, inp)
        maps = []
        for c in range(n):
            wfm, wtm = stage_a_weights(c, l, inp)
            m = dict(xT=xT, wfm=wfm, wtm=wtm, ident=ident, vf_in=vf[c])
            m.update(rwkv_host_inputs(c, l, inp))
            m.update({"nsa_" + k: v for k, v in nw.items()})
            m.update({"nsa_" + k: v for k, v in ntab.items() if k != "ident"})
            m.update({"ret_" + k: v for k, v in rtab.items()})
            m.update({"ret_" + k: v for k, v in ret_head_consts(c).items() if k != "ident"})
            maps.append(m)
        resA = run_bass_kernel_spmd(ncA, maps, core_ids=list(range(n))).results
        vf = [r["vf_out"] for r in resA]
        yT = np.stack([np.concatenate([r[k] for r in resA], 0) for k in ("y_rw", "y_nsa", "y_ret")])
        hb = stageb_host(l, inp, l == 0)
        maps = []
        for c in range(n):
            m = dict(hb)
            m["xTc"] = np.ascontiguousarray(xT[:, c * T:(c + 1) * T])
            m["yT"] = np.ascontiguousarray(yT[:, :, c * T:(c + 1) * T])
            maps.append(m)
        resB = run_bass_kernel_spmd(ncB, maps, core_ids=list(range(n))).results
        xT = np.ascontiguousarray(np.concatenate([r["out"] for r in resB], 1))
    return np.ascontiguousarray(xT.T)[None].astype(np.float32)


def kernel(**inputs):
    return forward(inputs, 16384)
```
